# Optimizing a Trainium2 kernel written in Bass

```python
import math
import jax
import jax.numpy as jnp
from jax import lax
import numpy as np

D_MODEL = 1024
BATCH = 4
SEQ = 8192
DEPTH = 2

GRID_W = 64
CTX_LEN = 256
EPS = 1e-6

N_BRANCH = 4
W_MIX = D_MODEL // N_BRANCH

GLA_HEADS = 4
GLA_DK = W_MIX // GLA_HEADS
GLA_DV = W_MIX // GLA_HEADS
GLA_RANK = 16
GLA_TAU = 16.0
GLA_CHUNK = 64

S5_GROUP = 16
S5_GROUPS = W_MIX // S5_GROUP
S5_STATE = 64
S5_MAX_RE = -1e-4

HY_BANDS = 16
HY_EMB = 1 + 2 * HY_BANDS
HY_FFN = 64
HY_SHORT = 3
HY_DECAY_SHORT = 0.3
HY_DECAY_LONG = 1.5
HY_TARGET = 1e-2

RG_BLOCKS = 4
RG_BLOCK = W_MIX // RG_BLOCKS
RG_CONV = 4
RG_C = 8.0

D_FF = 2816
N_EXPERTS = 8
TOP_K = 2
D_EXPERT = 3584
N_DENSE = (DEPTH + 1) // 2
N_MOE = DEPTH // 2

IN_SIZES = (GLA_HEADS * GLA_DK, GLA_HEADS * GLA_DK, GLA_HEADS * GLA_DV, GLA_HEADS * GLA_DV,
            2 * GLA_RANK, W_MIX, 3 * W_MIX, W_MIX, W_MIX, N_BRANCH * D_MODEL)
IN_COLS = sum(IN_SIZES)

kernel_name = "hybrid_dit_gla_s5_hyena_rglru_moe"


def rmsnorm(x, g):
    x32 = x.astype(jnp.float32)
    y = x32 * lax.rsqrt(jnp.mean(x32 * x32, axis=-1, keepdims=True) + EPS)
    return (y * g.astype(jnp.float32)).astype(x.dtype)


def modulate(h, shift, scale):
    return (h * (1 + scale) + shift).astype(h.dtype)


def adaln(cond, w, b):
    return jax.nn.silu(cond) @ w + b


def split_cols(p):
    out, start = [], 0
    for n in IN_SIZES:
        out.append(p[..., start:start + n])
        start += n
    return out


def grid_pos_embed(n_tokens, dim):
    rows = n_tokens // GRID_W
    q = dim // 4
    omega = 1.0 / (10000.0 ** (jnp.arange(q, dtype=jnp.float32) / q))
    r = jnp.arange(rows, dtype=jnp.float32)[:, None] * omega
    cc = jnp.arange(GRID_W, dtype=jnp.float32)[:, None] * omega
    er = jnp.concatenate([jnp.sin(r), jnp.cos(r)], axis=-1)
    ec = jnp.concatenate([jnp.sin(cc), jnp.cos(cc)], axis=-1)
    emb = jnp.concatenate([jnp.broadcast_to(er[:, None], (rows, GRID_W, dim // 2)),
                           jnp.broadcast_to(ec[None], (rows, GRID_W, dim // 2))], axis=-1)
    return emb.reshape(rows * GRID_W, dim)


def dwconv(x, w, pad_l, pad_r):
    n = x.shape[1]
    xp = jnp.pad(x, ((0, 0), (pad_l, pad_r), (0, 0)))
    return sum(xp[:, k:k + n] * w[k] for k in range(w.shape[0]))


def to_heads(t, n):
    b, n_tok, w = t.shape
    return t.reshape(b, n_tok, n, w // n).transpose(0, 2, 1, 3)


def _lin_combine(e1, e2):
    a1, b1 = e1
    a2, b2 = e2
    return a1 * a2, a2 * b1 + b2


def linear_scan(a, b, h0):
    b = b.at[:, 0].add(a[:, 0] * h0)
    _, h = lax.associative_scan(_lin_combine, (a, b), axis=1)
    return h


def gla_chunked(q, k, v, la, s0):
    b_, h_, n_tok, dk = q.shape
    dv = v.shape[-1]
    n_ch = n_tok // GLA_CHUNK
    q = q.reshape(b_, h_, n_ch, GLA_CHUNK, dk)
    k = k.reshape(b_, h_, n_ch, GLA_CHUNK, dk)
    v = v.reshape(b_, h_, n_ch, GLA_CHUNK, dv)
    cum = jnp.cumsum(la.reshape(b_, h_, n_ch, GLA_CHUNK, dk), axis=3)
    cum_last = cum[:, :, :, -1:, :]
    q_in = q * jnp.exp(cum)
    k_in = k * jnp.exp(-cum)
    k_out = k * jnp.exp(cum_last - cum)
    mask = jnp.tril(jnp.ones((GLA_CHUNK, GLA_CHUNK), dtype=bool))
    att = jnp.where(mask, jnp.einsum('bhnid,bhnjd->bhnij', q_in, k_in), 0.0)
    o_intra = jnp.einsum('bhnij,bhnjv->bhniv', att, v)
    d_state = jnp.einsum('bhnjd,bhnjv->bhndv', k_out, v)
    decay = jnp.exp(cum_last[:, :, :, 0, :])

    def step(s, inp):
        dec, ds = inp
        return dec[..., None] * s + ds, s

    s_fin, s_in = lax.scan(step, s0, (jnp.moveaxis(decay, 2, 0), jnp.moveaxis(d_state, 2, 0)))
    s_in = jnp.moveaxis(s_in, 0, 2)
    o_inter = jnp.einsum('bhnid,bhndv->bhniv', q_in, s_in)
    return (o_intra + o_inter).reshape(b_, h_, n_tok, dv), s_fin


def gla_mixer(q, k, v, og, gdn, w_up, b_up, s0):
    f32 = jnp.float32
    b_, n_tok, _ = q.shape
    qh = to_heads(q.astype(f32), GLA_HEADS) * (GLA_DK ** -0.5)
    kh = to_heads(k.astype(f32), GLA_HEADS)
    vh = to_heads(v.astype(f32), GLA_HEADS)
    g_dirs = jnp.split(gdn.astype(f32), 2, axis=-1)
    o = 0.0
    finals = []
    for d in range(2):
        la = jax.nn.log_sigmoid(g_dirs[d] @ w_up[d].astype(f32) + b_up[d].astype(f32)) / GLA_TAU
        la = to_heads(la, GLA_HEADS)
        if d == 0:
            od, sd = gla_chunked(qh, kh, vh, la, s0[d])
        else:
            od, sd = gla_chunked(*(jnp.flip(t, 2) for t in (qh, kh, vh, la)), s0[d])
            od = jnp.flip(od, 2)
        o = o + od
        finals.append(sd)
    o = o * lax.rsqrt(jnp.mean(o * o, axis=-1, keepdims=True) + EPS)
    o = o.transpose(0, 2, 1, 3).reshape(b_, n_tok, GLA_HEADS * GLA_DV)
    out = o * jax.nn.silu(og.astype(f32))
    return out.astype(q.dtype), jnp.stack(finals)


def s5_mixer(u, lam_re, lam_im, log_dt, b_re, b_im, c_re, c_im, d_skip, w_glu, b_glu, s0):
    f32 = jnp.float32
    b_, n_tok, _ = u.shape
    ug = u.astype(f32).reshape(b_, n_tok, S5_GROUPS, S5_GROUP)
    y = ug * d_skip.astype(f32)
    finals = []
    for d in range(2):
        lam = lax.complex(jnp.minimum(lam_re[d].astype(f32), S5_MAX_RE), lam_im[d].astype(f32))
        dt = jnp.exp(log_dt[d].astype(f32))[:, None]
        lbar = jnp.exp(lam * dt)
        bbar = ((lbar - 1.0) / lam)[..., None] * lax.complex(b_re[d].astype(f32), b_im[d].astype(f32))
        cmat = lax.complex(c_re[d].astype(f32), c_im[d].astype(f32))
        src = ug if d == 0 else jnp.flip(ug, 1)
        bu = jnp.einsum('blgh,gph->blgp', src.astype(jnp.complex64), bbar)
        a = jnp.broadcast_to(lbar[None, None], (1, n_tok) + lbar.shape)
        h = linear_scan(a, bu, s0[d])
        finals.append(h[:, -1])
        yd = jnp.einsum('blgp,ghp->blgh', h, cmat).real
        y = y + (yd if d == 0 else jnp.flip(yd, 1))
    y = y.reshape(b_, n_tok, W_MIX)
    g = jax.nn.gelu(y)
    out = g * jax.nn.sigmoid(g @ w_glu.astype(f32) + b_glu.astype(f32))
    return out.astype(u.dtype), jnp.stack(finals)


def hyena_filters(n_tok, w1, b1, w2, b2, w3, freq):
    f32 = jnp.float32
    t = jnp.arange(n_tok, dtype=f32)[:, None]
    bands = jnp.linspace(1e-4, HY_BANDS - 1, HY_BANDS, dtype=f32)[None]
    ang = 2.0 * math.pi * bands * t / n_tok
    z = jnp.concatenate([t / n_tok, jnp.cos(ang), jnp.sin(ang)], axis=-1)
    fr = freq.astype(f32)
    h = jnp.sin(fr * (z @ w1.astype(f32) + b1.astype(f32)))
    h = jnp.sin(fr * (h @ w2.astype(f32) + b2.astype(f32)))
    h = h @ w3.astype(f32)
    t01 = t / max(n_tok - 1, 1)
    deltas = jnp.abs(jnp.linspace(math.log(HY_TARGET) / HY_DECAY_SHORT,
                                  math.log(HY_TARGET) / HY_DECAY_LONG, W_MIX, dtype=f32))
    h = h * jnp.exp(-t01 * jnp.tile(deltas, 2))
    return h / (jnp.sum(jnp.abs(h), axis=0, keepdims=True) + EPS)


def hyena_mixer(p, w_short, w1, b1, w2, b2, w3, freq, bias):
    f32 = jnp.float32
    n_tok = p.shape[1]
    pc = dwconv(p.astype(f32), w_short.astype(f32), 1, 1)
    v, x0, x1 = jnp.split(pc, 3, axis=-1)
    hf, hb = jnp.split(hyena_filters(n_tok, w1, b1, w2, b2, w3, freq), 2, axis=-1)
    n_fft = 2 * n_tok
    h_freq = jnp.fft.rfft(hf, n=n_fft, axis=0) + jnp.conj(jnp.fft.rfft(hb, n=n_fft, axis=0))
    z = x1 * v
    conv = jnp.fft.irfft(jnp.fft.rfft(z, n=n_fft, axis=1) * h_freq, n=n_fft, axis=1)[:, :n_tok]
    y = x0 * (conv + z * bias.astype(f32))
    return y.astype(p.dtype)


def rglru_mixer(xr, gate, w_conv, w_a, b_a, w_x, b_x, lam, s0):
    f32 = jnp.float32
    b_, n_tok, _ = xr.shape
    xc = dwconv(xr.astype(f32), w_conv.astype(f32), 2, 1)
    xb = xc.reshape(b_, n_tok, RG_BLOCKS, RG_BLOCK)
    y = 0.0
    finals = []
    for d in range(2):
        src = xb if d == 0 else jnp.flip(xb, 1)
        r = jax.nn.sigmoid(jnp.einsum('blhi,hij->blhj', src, w_a[d].astype(f32)) + b_a[d].astype(f32))
        i = jax.nn.sigmoid(jnp.einsum('blhi,hij->blhj', src, w_x[d].astype(f32)) + b_x[d].astype(f32))
        log_a = -RG_C * r * jax.nn.softplus(-lam[d].astype(f32).reshape(RG_BLOCKS, RG_BLOCK))
        a = jnp.exp(log_a)
        bt = jnp.sqrt(-jnp.expm1(2.0 * log_a)) * (i * src)
        h = linear_scan(a, bt, s0[d])
        finals.append(h[:, -1])
        y = y + (h if d == 0 else jnp.flip(h, 1))
    y = y.reshape(b_, n_tok, W_MIX) * jax.nn.gelu(gate.astype(f32))
    return y.astype(xr.dtype), jnp.stack(finals)


def merge_branches(branches, gates, w_branch, w_out):
    gk = jnp.split(gates, N_BRANCH, axis=-1)
    y = 0.0
    for k in range(N_BRANCH):
        y = y + jax.nn.sigmoid(gk[k]) * (branches[k] @ w_branch[k])
    return y @ w_out


def swiglu(h, w_gu, w_down):
    g, u = jnp.split(h @ w_gu, 2, axis=-1)
    return (jax.nn.silu(g) * u) @ w_down


def moe_swiglu(h, router, router_b, w_gu, w_down):
    shp = h.shape
    ht = h.reshape(-1, shp[-1])
    logits = ht.astype(jnp.float32) @ router.astype(jnp.float32) + router_b.astype(jnp.float32)
    top_v, top_i = lax.top_k(logits, TOP_K)
    w = jax.nn.softmax(top_v, axis=-1)
    gates = jnp.sum(jax.nn.one_hot(top_i, N_EXPERTS, dtype=jnp.float32) * w[..., None], axis=1)
    y = jnp.zeros_like(ht)
    for e in range(N_EXPERTS):
        y = y + gates[:, e:e + 1].astype(ht.dtype) * swiglu(ht, w_gu[e], w_down[e])
    return y.reshape(shp)


def channel_mix(h, layer, ffn_w_gu, ffn_w_down, moe_router, moe_router_b, moe_w_gu, moe_w_down):
    j = layer // 2
    if layer % 2 == 0:
        return swiglu(h, ffn_w_gu[j], ffn_w_down[j])
    return moe_swiglu(h, moe_router[j], moe_router_b[j], moe_w_gu[j], moe_w_down[j])


def setup_inputs(seed: int = 0) -> dict:
    key = jax.random.key(seed)
    ks = iter(jax.random.split(key, 64))
    f32 = jnp.float32

    def nrm(shape, scale):
        return jax.random.normal(next(ks), shape, f32) * scale

    def uni(shape, lo, hi):
        return jax.random.uniform(next(ks), shape, f32, lo, hi)

    l2 = (DEPTH, 2)
    x = nrm((BATCH, SEQ, D_MODEL), 1.0)
    c = nrm((BATCH, D_MODEL), 1.0)
    ctx = nrm((BATCH, CTX_LEN, D_MODEL), 1.0)
    c_ctx = nrm((D_MODEL,), 1.0)
    mod_w = nrm((DEPTH, D_MODEL, 6 * D_MODEL), 0.5 * D_MODEL ** -0.5)
    mod_b = nrm((DEPTH, 6 * D_MODEL), 0.02)
    norm1_g = 1.0 + nrm((DEPTH, D_MODEL), 0.02)
    norm2_g = 1.0 + nrm((DEPTH, D_MODEL), 0.02)
    w_in = nrm((DEPTH, D_MODEL, IN_COLS), D_MODEL ** -0.5)
    gla_w_up = nrm(l2 + (GLA_RANK, W_MIX), GLA_RANK ** -0.5)
    gla_b_up = nrm(l2 + (W_MIX,), 0.1)
    n_idx = jnp.arange(S5_STATE, dtype=f32)
    s5_lam_re = -0.5 + nrm(l2 + (S5_GROUPS, S5_STATE), 0.01)
    s5_lam_im = math.pi * n_idx + nrm(l2 + (S5_GROUPS, S5_STATE), 0.01)
    s5_log_dt = uni(l2 + (S5_GROUPS,), math.log(1e-3), math.log(1e-1))
    s5_b_re = nrm(l2 + (S5_GROUPS, S5_STATE, S5_GROUP), (2 * S5_GROUP) ** -0.5)
    s5_b_im = nrm(l2 + (S5_GROUPS, S5_STATE, S5_GROUP), (2 * S5_GROUP) ** -0.5)
    s5_c_re = nrm(l2 + (S5_GROUPS, S5_GROUP, S5_STATE), (2 * S5_STATE) ** -0.5)
    s5_c_im = nrm(l2 + (S5_GROUPS, S5_GROUP, S5_STATE), (2 * S5_STATE) ** -0.5)
    s5_d = nrm((DEPTH, S5_GROUPS, S5_GROUP), 0.5)
    s5_w_glu = nrm((DEPTH, W_MIX, W_MIX), W_MIX ** -0.5)
    s5_b_glu = nrm((DEPTH, W_MIX), 0.02)
    hy_w_short = nrm((DEPTH, HY_SHORT, 3 * W_MIX), HY_SHORT ** -0.5)
    hy_w1 = nrm((DEPTH, HY_EMB, HY_FFN), HY_EMB ** -0.5)
    hy_b1 = nrm((DEPTH, HY_FFN), 0.1)
    hy_w2 = nrm((DEPTH, HY_FFN, HY_FFN), HY_FFN ** -0.5)
    hy_b2 = nrm((DEPTH, HY_FFN), 0.1)
    hy_w3 = nrm((DEPTH, HY_FFN, 2 * W_MIX), HY_FFN ** -0.5)
    hy_freq = 1.0 + nrm((DEPTH, HY_FFN), 0.05)
    hy_bias = nrm((DEPTH, W_MIX), 0.1)
    rg_w_conv = nrm((DEPTH, RG_CONV, W_MIX), RG_CONV ** -0.5)
    rg_w_a = nrm(l2 + (RG_BLOCKS, RG_BLOCK, RG_BLOCK), RG_BLOCK ** -0.5)
    rg_b_a = nrm(l2 + (RG_BLOCKS, RG_BLOCK), 0.1)
    rg_w_x = nrm(l2 + (RG_BLOCKS, RG_BLOCK, RG_BLOCK), RG_BLOCK ** -0.5)
    rg_b_x = nrm(l2 + (RG_BLOCKS, RG_BLOCK), 0.1)
    a0 = uni(l2 + (W_MIX,), 0.9, 0.999)
    s_root = a0 ** (1.0 / RG_C)
    rg_lam = jnp.log(s_root) - jnp.log1p(-s_root)
    w_branch = nrm((DEPTH, N_BRANCH, W_MIX, D_MODEL), W_MIX ** -0.5)
    w_out = nrm((DEPTH, D_MODEL, D_MODEL), D_MODEL ** -0.5)
    ffn_w_gu = nrm((N_DENSE, D_MODEL, 2 * D_FF), D_MODEL ** -0.5)
    ffn_w_down = nrm((N_DENSE, D_FF, D_MODEL), D_FF ** -0.5)
    moe_router = nrm((N_MOE, D_MODEL, N_EXPERTS), D_MODEL ** -0.5)
    moe_router_b = nrm((N_MOE, N_EXPERTS), 0.01)
    moe_w_gu = nrm((N_MOE, N_EXPERTS, D_MODEL, 2 * D_EXPERT), D_MODEL ** -0.5)
    moe_w_down = nrm((N_MOE, N_EXPERTS, D_EXPERT, D_MODEL), D_EXPERT ** -0.5)
    final_g = 1.0 + nrm((D_MODEL,), 0.02)
    return {"x": x, "c": c, "ctx": ctx, "c_ctx": c_ctx, "mod_w": mod_w, "mod_b": mod_b,
            "norm1_g": norm1_g, "norm2_g": norm2_g, "w_in": w_in,
            "gla_w_up": gla_w_up, "gla_b_up": gla_b_up,
            "s5_lam_re": s5_lam_re, "s5_lam_im": s5_lam_im, "s5_log_dt": s5_log_dt,
            "s5_b_re": s5_b_re, "s5_b_im": s5_b_im, "s5_c_re": s5_c_re, "s5_c_im": s5_c_im,
            "s5_d": s5_d, "s5_w_glu": s5_w_glu, "s5_b_glu": s5_b_glu,
            "hy_w_short": hy_w_short, "hy_w1": hy_w1, "hy_b1": hy_b1, "hy_w2": hy_w2,
            "hy_b2": hy_b2, "hy_w3": hy_w3, "hy_freq": hy_freq, "hy_bias": hy_bias,
            "rg_w_conv": rg_w_conv, "rg_w_a": rg_w_a, "rg_b_a": rg_b_a, "rg_w_x": rg_w_x,
            "rg_b_x": rg_b_x, "rg_lam": rg_lam, "w_branch": w_branch, "w_out": w_out,
            "ffn_w_gu": ffn_w_gu, "ffn_w_down": ffn_w_down, "moe_router": moe_router,
            "moe_router_b": moe_router_b, "moe_w_gu": moe_w_gu, "moe_w_down": moe_w_down,
            "final_g": final_g}


def reference(x, c, ctx, c_ctx, mod_w, mod_b, norm1_g, norm2_g, w_in, gla_w_up, gla_b_up,
              s5_lam_re, s5_lam_im, s5_log_dt, s5_b_re, s5_b_im, s5_c_re, s5_c_im, s5_d,
              s5_w_glu, s5_b_glu, hy_w_short, hy_w1, hy_b1, hy_w2, hy_b2, hy_w3, hy_freq,
              hy_bias, rg_w_conv, rg_w_a, rg_b_a, rg_w_x, rg_b_x, rg_lam, w_branch, w_out,
              ffn_w_gu, ffn_w_down, moe_router, moe_router_b, moe_w_gu, moe_w_down, final_g):
    f32 = jnp.float32
    n_b, n_lat, _ = x.shape
    x = x + grid_pos_embed(n_lat, D_MODEL).astype(x.dtype)[None]
    y_ctx = ctx
    for l in range(DEPTH):
        last = l == DEPTH - 1
        m_lat = jnp.split(adaln(c, mod_w[l], mod_b[l])[:, None, :], 6, axis=-1)
        m_ctx = jnp.split(adaln(c_ctx, mod_w[l], mod_b[l])[None, None, :], 6, axis=-1)

        h_lat = modulate(rmsnorm(x, norm1_g[l]), m_lat[0], m_lat[1])
        h_ctx = modulate(rmsnorm(y_ctx, norm1_g[l]), m_ctx[0], m_ctx[1])
        p_lat = split_cols(h_lat @ w_in[l])
        p_ctx = split_cols(h_ctx @ w_in[l])
        gla_p = (gla_w_up[l], gla_b_up[l])
        s5_p = (s5_lam_re[l], s5_lam_im[l], s5_log_dt[l], s5_b_re[l], s5_b_im[l],
                s5_c_re[l], s5_c_im[l], s5_d[l], s5_w_glu[l], s5_b_glu[l])
        hy_p = (hy_w_short[l], hy_w1[l], hy_b1[l], hy_w2[l], hy_b2[l], hy_w3[l], hy_freq[l], hy_bias[l])
        rg_p = (rg_w_conv[l], rg_w_a[l], rg_b_a[l], rg_w_x[l], rg_b_x[l], rg_lam[l])

        gla_c, gla_state = gla_mixer(*p_ctx[0:5], *gla_p,
                                     jnp.zeros((2, n_b, GLA_HEADS, GLA_DK, GLA_DV), f32))
        gla_l, _ = gla_mixer(*p_lat[0:5], *gla_p, gla_state)
        s5_c, s5_state = s5_mixer(p_ctx[5], *s5_p,
                                  jnp.zeros((2, n_b, S5_GROUPS, S5_STATE), jnp.complex64))
        s5_l, _ = s5_mixer(p_lat[5], *s5_p, s5_state)
        rg_c, rg_state = rglru_mixer(p_ctx[7], p_ctx[8], *rg_p,
                                     jnp.zeros((2, n_b, RG_BLOCKS, RG_BLOCK), f32))
        rg_l, _ = rglru_mixer(p_lat[7], p_lat[8], *rg_p, rg_state)
        hy_l = hyena_mixer(p_lat[6], *hy_p)

        x = x + m_lat[2] * merge_branches((gla_l, s5_l, hy_l, rg_l), p_lat[9], w_branch[l], w_out[l])
        if not last:
            hy_c = hyena_mixer(p_ctx[6], *hy_p)
            y_ctx = y_ctx + m_ctx[2] * merge_branches((gla_c, s5_c, hy_c, rg_c), p_ctx[9],
                                                      w_branch[l], w_out[l])

        h2_lat = modulate(rmsnorm(x, norm2_g[l]), m_lat[3], m_lat[4])
        x = x + m_lat[5] * channel_mix(h2_lat, l, ffn_w_gu, ffn_w_down, moe_router,
                                       moe_router_b, moe_w_gu, moe_w_down)
        if not last:
            h2_ctx = modulate(rmsnorm(y_ctx, norm2_g[l]), m_ctx[3], m_ctx[4])
            y_ctx = y_ctx + m_ctx[5] * channel_mix(h2_ctx, l, ffn_w_gu, ffn_w_down, moe_router,
                                                   moe_router_b, moe_w_gu, moe_w_down)
    return rmsnorm(x, final_g)
```

```python
import os
import math
import contextlib
import numpy as np
import concourse.bass as bass
import concourse.mybir as mybir
from concourse.bass_utils import run_bass_kernel_spmd

F32 = mybir.dt.float32
BF16 = mybir.dt.bfloat16
I32 = mybir.dt.int32
AF = mybir.ActivationFunctionType
ALU = mybir.AluOpType
AX = mybir.AxisListType

D = 1024
KT = 8
NCTX = 256
NLAT = 8192
S = NCTX + NLAT + NCTX
NBLK = S // 512
LAT0, LAT1 = NCTX, NCTX + NLAT
DEPTH = 2
IN_COLS = 6688
MIXC = 2592
EPS = 1e-6
D_FF = 2816
D_EXP = 3584
NEXP = 8
NOUT = NLAT // 2

ENGS = ("pe", "act", "dve", "pool", "sp")
EPOCH = 4000
NDMASEM = 8
DEPOCH = 200


class Prog:
    def __init__(self, nc):
        self.nc = nc
        self.ops = []
        self.deps = []
        self.last_w = {}
        self.readers = {}
        self.bar = set()
        self.bar_done = set(ENGS)
        self.last_c = {}
        self.last_d = {e: [] for e in ENGS}

    def barrier(self):
        b = set(self.last_c.values())
        for e in ENGS:
            b.update(self.last_d[e])
        self.bar = b
        self.bar_done = set()

    def op(self, eng, fn, reads=(), writes=(), dma=False):
        i = len(self.ops)
        d = set()
        if eng not in self.bar_done:
            d.update(self.bar)
            self.bar_done.add(eng)
        if dma:
            self.last_d[eng] = (self.last_d[eng] + [i])[-NDMASEM:]
        else:
            self.last_c[eng] = i
        for r in reads:
            if r in self.last_w:
                d.add(self.last_w[r])
        for w in writes:
            if w in self.last_w:
                d.add(self.last_w[w])
            lastr = {}
            for rr in self.readers.get(w, ()):
                if self.ops[rr][2]:
                    d.add(rr)
                else:
                    lastr[self.ops[rr][0]] = rr
            d.update(lastr.values())
        for w in writes:
            self.last_w[w] = i
            self.readers[w] = []
        for r in reads:
            self.readers.setdefault(r, []).append(i)
        d.discard(i)
        self.ops.append((eng, fn, dma))
        self.deps.append(sorted(d))
        return i

    def emit(self, final_wait_ops=()):
        nc = self.nc
        ops, deps = self.ops, self.deps
        n = len(ops)
        waited = [False] * n
        for i in range(n):
            for j in deps[i]:
                if ops[j][0] == "pe" and ops[i][0] == "pe" and not ops[j][2] and not ops[i][2]:
                    continue
                waited[j] = True
        for j in final_wait_ops:
            waited[j] = True
        ms = [None] * n
        cnt = {e: 0 for e in ENGS}
        dcnt = {e: 0 for e in ENGS}
        dnum = [None] * n
        for i in range(n):
            e, _, isd = ops[i]
            if isd:
                dnum[i] = dcnt[e]
                dcnt[e] += 1
            elif waited[i]:
                ms[i] = cnt[e]
                cnt[e] += 1
        stack = contextlib.ExitStack()
        with stack:
            csem = {}
            for e in ENGS:
                for ep in range((cnt[e] + EPOCH - 1) // EPOCH):
                    csem[(e, ep)] = stack.enter_context(nc.semaphore(f"c_{e}_{ep}"))
            dsem = {}
            for e in ENGS:
                if dcnt[e]:
                    nep = (dcnt[e] + NDMASEM * DEPOCH - 1) // (NDMASEM * DEPOCH)
                    for k in range(NDMASEM * nep):
                        dsem[(e, k)] = stack.enter_context(nc.semaphore(f"d_{e}_{k}"))

            def dslot(k):
                ep = k // (NDMASEM * DEPOCH)
                kk = k % (NDMASEM * DEPOCH)
                return ep * NDMASEM + (kk % NDMASEM), kk // NDMASEM

            block = stack.enter_context(nc.Block())
            per_eng = {e: [i for i in range(n) if ops[i][0] == e] for e in ENGS}
            engobj = {"pe": "tensor", "act": "scalar", "dve": "vector", "pool": "gpsimd", "sp": "sync"}

            def make(e):
                def body(eng):
                    seen_c = {}
                    seen_d = set()

                    def wait_for(j):
                        ej, _, isd = ops[j]
                        if isd:
                            if j in seen_d:
                                return
                            seen_d.add(j)
                            sl, rnd = dslot(dnum[j])
                            eng.wait_ge(dsem[(ej, sl)], 16 * (rnd + 1))
                        else:
                            m = ms[j]
                            if m is None:
                                return
                            if seen_c.get(ej, -1) >= m:
                                return
                            seen_c[ej] = m
                            eng.wait_ge(csem[(ej, m // EPOCH)], (m % EPOCH) + 1)

                    for i in per_eng[e]:
                        _, fn, isd = ops[i]
                        for j in deps[i]:
                            if ops[j][0] == "pe" and e == "pe" and not ops[j][2] and not isd:
                                continue
                            wait_for(j)
                        if isd:
                            sl, rnd = dslot(dnum[i])
                            if rnd > 0:
                                eng.wait_ge(dsem[(e, sl)], 16 * rnd)
                            ins = fn(eng)
                            ins.then_inc(dsem[(e, sl)], 16)
                        else:
                            ins = fn(eng)
                            if ms[i] is not None:
                                ins.then_inc(csem[(e, ms[i] // EPOCH)], 1)
                    if e == "sp":
                        for j in final_wait_ops:
                            wait_for(j)
                return body

            for e in ENGS:
                if per_eng[e] or (e == "sp" and final_wait_ops):
                    getattr(block, engobj[e])(make(e))
        return n


class KB:
    def __init__(self, nc, debug_outs=()):
        self.nc = nc
        self.P = Prog(nc)
        self.st = contextlib.ExitStack()
        self.debug_outs = set(debug_outs)
        self.ps_i = 0
        self.psum = []
        self.dq = 0
        self.ev = 0

    def sb(self, name, shape, dt=F32):
        self.uid = getattr(self, "uid", 0) + 1
        return self.st.enter_context(self.nc.sbuf_tensor(f"s{self.uid}_{name}", list(shape), dt))

    @contextlib.contextmanager
    def scope(self):
        old = self.st
        with contextlib.ExitStack() as sst:
            self.st = sst
            try:
                yield
            finally:
                self.st = old
                self.P.barrier()

    def ring(self, name, n, shape, dt=F32):
        return [self.sb(f"{name}{i}", shape, dt) for i in range(n)]

    def dram(self, name, shape, dt=F32):
        kind = "ExternalOutput" if name in self.debug_outs else "Internal"
        return self.nc.dram_tensor(name, list(shape), dt, kind=kind)

    def init_psum(self):
        for i in range(8):
            self.psum.append(self.st.enter_context(self.nc.psum_tensor(f"ps{i}", [128, 512], F32)))

    def ps(self):
        i = self.ps_i % 8
        self.ps_i += 1
        return self.psum[i], f"ps{i}"

    def dma(self, out, in_, r=(), w=(), q=None):
        if q is None:
            q = ("sp", "pool")[self.dq % 2]
            self.dq += 1
        return self.P.op(q, lambda e: e.dma_start(out=out, in_=in_), r, w, dma=True)

    def mm(self, out, lhsT, rhs, start, stop, r, w):
        return self.P.op("pe", lambda e: e.matmul(out, lhsT=lhsT, rhs=rhs, start=start, stop=stop), r, w)

    def tr(self, out, in_, ident, r, w):
        return self.P.op("pe", lambda e: e.transpose(out=out, in_=in_, identity=ident), r, w)

    def act(self, out, in_, func, r, w, bias=None, scale=None):
        kw = {}
        if bias is not None:
            kw["bias"] = bias
        if scale is not None:
            kw["scale"] = scale
        return self.P.op("act", lambda e: e.activation(out=out, in_=in_, func=func, **kw), r, w)

    def tt(self, eng, out, in0, in1, op, r, w):
        return self.P.op(eng, lambda e: e.tensor_tensor(out=out, in0=in0, in1=in1, op=op), r, w)

    def ts(self, eng, out, in0, s1, s2, op0, op1, r, w):
        if s2 is None:
            return self.P.op(eng, lambda e: e.tensor_scalar(out=out, in0=in0, scalar1=s1, scalar2=None, op0=op0), r, w)
        return self.P.op(eng, lambda e: e.tensor_scalar(out=out, in0=in0, scalar1=s1, scalar2=s2, op0=op0, op1=op1), r, w)

    def stt(self, eng, out, in0, scalar, in1, op0, op1, r, w):
        return self.P.op(eng, lambda e: e.scalar_tensor_tensor(out=out, in0=in0, scalar=scalar, in1=in1, op0=op0, op1=op1), r, w)

    def cp(self, eng, out, in_, r, w):
        if eng == "act":
            return self.P.op("act", lambda e: e.copy(out=out, in_=in_), r, w)
        return self.P.op(eng, lambda e: e.tensor_copy(out=out, in_=in_), r, w)

    def evac(self, out, in_, r, w):
        self.ev += 1
        return self.cp(("act", "dve")[self.ev % 2], out, in_, r, w)

    def memset(self, eng, ap, val, w):
        return self.P.op(eng, lambda e: e.memset(ap, val), (), w)

    def scan(self, eng, out, a, b, init, r, w):
        return self.P.op(eng, lambda e: e.tensor_tensor_scan(out=out, data0=a, data1=b, initial=init, op0=ALU.mult, op1=ALU.add), r, w)

    def recip(self, out, in_, r, w):
        return self.P.op("dve", lambda e: e.reciprocal(out=out, in_=in_), r, w)


def segs(t0, n):
    out = []
    for (a, b, c) in ((0, LAT0, 1), (LAT0, LAT1, 0), (LAT1, S, 1)):
        lo, hi = max(a, t0), min(b, t0 + n)
        if hi > lo:
            out.append((lo, hi - lo, c))
    return out


def in_tiles():
    tl = []
    c = 0
    for sz in (256, 256, 256, 256, 32, 256, 768, 256, 256):
        k = 0
        while k < sz:
            w = min(128, sz - k)
            tl.append((c + k, w))
            k += w
        c += sz
    return tl


GELU_C = 2.0 * math.sqrt(2.0 / math.pi)


def gelu_tanh(kb, out, x, tmp, rk, wk, tk):
    kb.act(tmp, x, AF.Square, rk, [tk])
    kb.ts("dve", tmp, tmp, 0.044715, 1.0, ALU.mult, ALU.add, [tk], [tk])
    kb.tt("dve", tmp, tmp, x, ALU.mult, [tk] + list(rk), [tk])
    kb.act(tmp, tmp, AF.Sigmoid, [tk], [tk], scale=GELU_C)
    kb.tt("dve", out, tmp, x, ALU.mult, [tk] + list(rk), wk)


FWD_R = [(t0, min(512, LAT1 - t0)) for t0 in range(0, LAT1, 512)]
BWD_R = [(t0, 512) for t0 in range(S - 512, LAT0, -512)] + [(LAT0, 256)]


def stage_rglru(kb, A, l, pT, brT):
    for ct in range(2):
        with kb.scope():
            xy = kb.sb("rg_xy", [128, S], F32)
            xc = kb.sb("rg_xc", [128, S], F32)
            xcb = kb.sb("rg_xcb", [128, S], BF16)
            taps = kb.sb("rg_taps", [128, 5], F32)
            wbd = kb.sb("rg_wbd", [128, 2, 2, 128], BF16)
            bias = kb.sb("rg_bias", [128, 2, 2], F32)
            lam = kb.sb("rg_lam", [128, 2], F32)
            n8 = kb.sb("rg_n8", [128, 2], F32)
            n16 = kb.sb("rg_n16", [128, 2], F32)
            kb.dma(xy[:], pT.ap()[2080 + 128 * ct:2080 + 128 * (ct + 1), :], r=["pT"], w=["rg_xy"], q="sp")
            kb.dma(taps[:], A["rg_taps"][l, ct], w=["rg_taps"], q="sp")
            kb.dma(wbd[:], A["rg_wbd"][l, ct], w=["rg_wbd"], q="pool")
            kb.dma(bias[:], A["rg_bias"][l, ct], w=["rg_bias"], q="sp")
            kb.dma(lam[:], A["rg_lam"][l, ct], w=["rg_lam"], q="sp")
            kb.act(n8[:], lam[:], AF.Exp, ["rg_lam"], ["rg_n8"], scale=-1.0)
            kb.ts("dve", n8[:], n8[:], 1.0, None, ALU.add, None, ["rg_n8"], ["rg_n8"])
            kb.act(n8[:], n8[:], AF.Ln, ["rg_n8"], ["rg_n8"])
            kb.ts("dve", n16[:], n8[:], -16.0, None, ALU.mult, None, ["rg_n8"], ["rg_n16"])
            kb.ts("dve", n8[:], n8[:], -8.0, None, ALU.mult, None, ["rg_n8", "rg_n16"], ["rg_n8"])
            for (a0, a1) in ((0, LAT0), (LAT0, LAT1), (LAT1, S)):
                kb.ts("dve", xc[:, a0:a1], xy[:, a0:a1], taps[:, 2:3], None, ALU.mult, None,
                      ["rg_xy", "rg_taps"], ["rg_xc"])
                for o in (0, 1, 3, 4):
                    off = o - 2
                    lo, hi = max(a0, a0 - off), min(a1, a1 - off)
                    kb.stt("dve", xc[:, lo:hi], xy[:, lo + off:hi + off], taps[:, o:o + 1], xc[:, lo:hi],
                           ALU.mult, ALU.add, ["rg_xy", "rg_taps", "rg_xc"], ["rg_xc"])
            kb.cp("act", xcb[:], xc[:], ["rg_xc"], ["rg_xcb"])
            y = xy
            kb.memset("pool", y[:, LAT1:S], 0.0, ["rg_xy"])
            tr = kb.ring("rg_r", 2, [128, 512], F32)
            ti = kb.ring("rg_i", 2, [128, 512], F32)
            ta = kb.ring("rg_a", 2, [128, 512], F32)
            tq = kb.ring("rg_q", 2, [128, 512], F32)
            hb = kb.ring("rg_hb", 2, [128, 512], F32)
            it = 0
            for d in range(2):
                prev = None
                for (t0, n) in (FWD_R if d == 0 else BWD_R):
                    k = it % 2
                    it += 1
                    r_, i_, a_, q_ = tr[k], ti[k], ta[k], tq[k]
                    rk_, ik_, ak_, qk_ = f"rg_r{k}", f"rg_i{k}", f"rg_a{k}", f"rg_q{k}"
                    p1, pk1 = kb.ps()
                    kb.mm(p1[:, 0:n], wbd[:, d, 0, :], xcb[:, t0:t0 + n], True, True, ["rg_wbd", "rg_xcb"], [pk1])
                    p2, pk2 = kb.ps()
                    kb.mm(p2[:, 0:n], wbd[:, d, 1, :], xcb[:, t0:t0 + n], True, True, ["rg_wbd", "rg_xcb"], [pk2])
                    kb.act(r_[:, 0:n], p1[:, 0:n], AF.Sigmoid, [pk1, "rg_bias"], [rk_], bias=bias[:, d, 0:1])
                    kb.act(i_[:, 0:n], p2[:, 0:n], AF.Sigmoid, [pk2, "rg_bias"], [ik_], bias=bias[:, d, 1:2])
                    kb.act(a_[:, 0:n], r_[:, 0:n], AF.Exp, [rk_, "rg_n8"], [ak_], scale=n8[:, d:d + 1])
                    kb.act(q_[:, 0:n], r_[:, 0:n], AF.Exp, [rk_, "rg_n16"], [qk_], scale=n16[:, d:d + 1])
                    kb.ts("pool", q_[:, 0:n], q_[:, 0:n], -1.0, 1.0, ALU.mult, ALU.add, [qk_], [qk_])
                    kb.act(q_[:, 0:n], q_[:, 0:n], AF.Sqrt, [qk_], [qk_])
                    kb.tt("pool", i_[:, 0:n], i_[:, 0:n], xc[:, t0:t0 + n], ALU.mult, [ik_, "rg_xc"], [ik_])
                    kb.tt("dve", q_[:, 0:n], q_[:, 0:n], i_[:, 0:n], ALU.mult, [qk_, ik_], [qk_])
                    if d == 0:
                        init = 0.0 if prev is None else y[:, t0 - 1:t0]
                        kb.scan("dve", y[:, t0:t0 + n], a_[:, 0:n], q_[:, 0:n], init, [ak_, qk_, "rg_xy"], ["rg_xy"])
                    else:
                        h_, hk_ = hb[k], f"rg_hb{k}"
                        init = 0.0 if prev is None else prev[0][:, 0:1]
                        rr = [ak_, qk_] + ([] if prev is None else [prev[1]])
                        kb.scan("dve", h_[:, 0:n][:, ::-1], a_[:, 0:n][:, ::-1], q_[:, 0:n][:, ::-1], init, rr, [hk_])
                        kb.tt("pool", y[:, t0:t0 + n], y[:, t0:t0 + n], h_[:, 0:n], ALU.add, ["rg_xy", hk_], ["rg_xy"])
                        prev = (h_, hk_)
                    if d == 0:
                        prev = True
            kb.tt("dve", y[:, 0:LAT0], y[:, 0:LAT0], y[:, LAT1:S], ALU.add, ["rg_xy"], ["rg_xy"])
            kb.cp("dve", y[:, LAT1:S], y[:, 0:LAT0], ["rg_xy"], ["rg_xy"])
            gt = kb.ring("rg_g", 2, [128, 512], F32)
            ob = kb.ring("rg_o", 2, [128, 512], BF16)
            for b in range(NBLK):
                k = b % 2
                t0 = b * 512
                kb.dma(gt[k][:], pT.ap()[2336 + 128 * ct:2336 + 128 * (ct + 1), t0:t0 + 512], r=["pT"], w=[f"rg_g{k}"])
                gelu_tanh(kb, gt[k][:], gt[k][:], tr[k][:], [f"rg_g{k}"], [f"rg_g{k}"], f"rg_r{k}")
                kb.tt("dve", ob[k][:], gt[k][:], y[:, t0:t0 + 512], ALU.mult, [f"rg_g{k}", "rg_xy"], [f"rg_o{k}"])
                kb.dma(brT.ap()[768 + 128 * ct:768 + 128 * (ct + 1), t0:t0 + 512], ob[k][:], r=[f"rg_o{k}"], w=["brT"])


NCH = S // 128


def stage_gla(kb, A, l, pT, brT, C):
    ident, a01 = C["ident"], C["a01"]
    for hp in range(2):
        with kb.scope():
            vt = [kb.sb(f"gl_vtok{h}", [128, NCH, 128], BF16) for h in range(2)]
            oacc = kb.sb("gl_oacc", [128, S], F32)
            wup = kb.sb("gl_wup", [32, 2, 128], F32)
            nb = kb.sb("gl_nb", [128, 2], F32)
            kb.dma(wup[:], A["gla_wup"][l, hp], w=["gl_wup"], q="sp")
            kb.dma(nb[:], A["gla_bup"][l, hp], w=["gl_nb"], q="sp")
            kb.ts("dve", nb[:], nb[:], -1.0, None, ALU.mult, None, ["gl_nb"], ["gl_nb"])
            kb.memset("pool", oacc[:, LAT1:S], 0.0, ["gl_oacc"])
            with kb.scope():
                vb = kb.ring("gl_vb", 2, [128, 512], BF16)
                vfull = kb.ring("gl_vf", 2, [128, 512], BF16)
                for b in range(NBLK):
                    k = b % 2
                    t0 = b * 512
                    kb.dma(vb[k][:], pT.ap()[512 + 128 * hp:512 + 128 * (hp + 1), t0:t0 + 512], r=["pT"], w=[f"gl_vb{k}"], q="pool")
                    pst, pk = kb.ps()
                    pb = pst[:].bitcast(BF16)
                    for j in range(4):
                        kb.tr(pb[:, j * 128:(j + 1) * 128], vb[k][:, j * 128:(j + 1) * 128], ident[:], [f"gl_vb{k}", "ident"], [pk])
                    vf = vfull[b % 2]
                    kb.evac(vf[:], pb[:, 0:512], [pk], [f"gl_vf{b % 2}"])
                    for h in range(2):
                        kb.tt(("pool", "dve")[h], vt[h][:, 4 * b:4 * b + 4, :], vf[:].rearrange("p (c j) -> p c j", j=128),
                              C["hmask"][h][:].unsqueeze(1).to_broadcast([128, 4, 128]), ALU.mult,
                              [f"gl_vf{b % 2}", "hmask"], [f"gl_vtok{h}"])
            for d in range(2):
                with kb.scope():
                    qin = kb.sb("gl_qin", [128, S], BF16)
                    qm = [kb.sb(f"gl_qm{h}", [128, S], BF16) for h in range(2)]
                    kin = kb.sb("gl_kin", [128, S], BF16)
                    kotok = kb.sb("gl_kotok", [128, NCH, 128], BF16)
                    decay = kb.sb("gl_decay", [128, NCH], F32)
                    Sf = kb.sb("gl_S", [128, 128], F32)
                    Sbd = kb.sb("gl_Sb", [128, 128], BF16)
                    kb.memset("pool", Sf[:], 0.0, ["gl_S"])
                    kb.memset("pool", Sbd[:], 0.0, ["gl_Sb"])
                    with kb.scope():
                        qb = kb.ring("gl_qb", 2, [128, 512], BF16)
                        kbf = kb.ring("gl_kb", 2, [128, 512], BF16)
                        gd = kb.ring("gl_gd", 2, [32, 512], F32)
                        cl = kb.ring("gl_cl", 2, [128, 512], F32)
                        e1 = kb.ring("gl_e1", 2, [128, 512], F32)
                        e2 = kb.ring("gl_e2", 2, [128, 512], F32)
                        kob = kb.ring("gl_kob", 2, [128, 512], BF16)
                        for b in range(NBLK):
                            k = b % 2
                            t0 = b * 512
                            kb.dma(qb[k][:], pT.ap()[128 * hp:128 * (hp + 1), t0:t0 + 512], r=["pT"], w=[f"gl_qb{k}"], q="pool")
                            kb.dma(kbf[k][:], pT.ap()[256 + 128 * hp:256 + 128 * (hp + 1), t0:t0 + 512], r=["pT"], w=[f"gl_kb{k}"], q="pool")
                            kb.dma(gd[k][:], pT.ap()[1024:1056, t0:t0 + 512], r=["pT"], w=[f"gl_gd{k}"], q="sp")
                            pst, pk = kb.ps()
                            kb.mm(pst[:], wup[:, d, :], gd[k][:], True, True, ["gl_wup", f"gl_gd{k}"], [pk])
                            c_, ck = cl[k], f"gl_cl{k}"
                            kb.act(c_[:], pst[:], AF.Exp, [pk, "gl_nb"], [ck], bias=nb[:, d:d + 1], scale=-1.0)
                            kb.act(c_[:], c_[:], AF.Ln, [ck, "one_t"], [ck], bias=C["one"][:, 0:1])
                            if d == 0:
                                kb.scan("dve", c_[:], a01[:], c_[:], 0.0, ["a01", ck], [ck])
                                last = c_[:].rearrange("p (c j) -> p c j", j=128)[:, :, 127]
                            else:
                                kb.scan("dve", c_[:, ::-1], a01[:], c_[:, ::-1], 0.0, ["a01", ck], [ck])
                                last = c_[:].rearrange("p (c j) -> p c j", j=128)[:, :, 0]
                            kb.act(e1[k][:], c_[:], AF.Exp, [ck], [f"gl_e1{k}"], scale=-1.0 / 16)
                            kb.act(e2[k][:], c_[:], AF.Exp, [ck], [f"gl_e2{k}"], scale=1.0 / 16)
                            kb.act(decay[:, 4 * b:4 * b + 4], last, AF.Exp, [ck], ["gl_decay"], scale=-1.0 / 16)
                            kb.stt("dve", qin[:, t0:t0 + 512], qb[k][:], 0.125, e1[k][:], ALU.mult, ALU.mult,
                                   [f"gl_qb{k}", f"gl_e1{k}"], ["gl_qin"])
                            for h in range(2):
                                kb.ts("pool", qm[h][:, t0:t0 + 512], qin[:, t0:t0 + 512], C["blk_mask"][:, 64 * h:64 * h + 1], None,
                                      ALU.mult, None, ["gl_qin", "blk_mask"], [f"gl_qm{h}"])
                            kb.tt("pool", kin[:, t0:t0 + 512], kbf[k][:], e2[k][:], ALU.mult, [f"gl_kb{k}", f"gl_e2{k}"], ["gl_kin"])
                            kb.tt("pool", kob[k][:].rearrange("p (c j) -> p c j", j=128),
                                  kin[:, t0:t0 + 512].rearrange("p (c j) -> p c j", j=128),
                                  decay[:, 4 * b:4 * b + 4].unsqueeze(2).to_broadcast([128, 4, 128]), ALU.mult,
                                  ["gl_kin", "gl_decay"], [f"gl_kob{k}"])
                            pst, pk = kb.ps()
                            pb = pst[:].bitcast(BF16)
                            for j in range(4):
                                kb.tr(pb[:, j * 128:(j + 1) * 128], kob[k][:, j * 128:(j + 1) * 128], ident[:], [f"gl_kob{k}", "ident"], [pk])
                            kb.evac(kotok[:, 4 * b:4 * b + 4, :], pb[:, 0:512].rearrange("p (c j) -> p c j", j=128), [pk], ["gl_kotok"])
                    if os.environ.get("GLA_CUT") == "prep":
                        continue
                    with kb.scope():
                        att = kb.ring("gl_att", 2, [128, 2, 128], BF16)
                        mask = C["mask_f"] if d == 0 else C["mask_b"]
                        mkey = "mask_f" if d == 0 else "mask_b"
                        chunks = list(range(0, 66)) if d == 0 else list(range(67, 1, -1))
                        def emit_att(ci):
                            c = chunks[ci]
                            k = ci % 2
                            tok = slice(c * 128, (c + 1) * 128)
                            pa, pka = kb.ps()
                            for h in range(2):
                                kb.mm(pa[:, h * 128:(h + 1) * 128], kin[:, tok], qm[h][:, tok], True, True, ["gl_kin", f"gl_qm{h}"], [pka])
                            kb.tt("dve", att[k][:], pa[:, 0:256].rearrange("p (h i) -> p h i", h=2),
                                  mask[:].unsqueeze(1).to_broadcast([128, 2, 128]), ALU.mult, [pka, mkey], [f"gl_att{k}"])

                        emit_att(0)
                        for ci, c in enumerate(chunks):
                            k = ci % 2
                            tok = slice(c * 128, (c + 1) * 128)
                            if ci + 1 < len(chunks):
                                emit_att(ci + 1)
                            pS, pkS = kb.ps()
                            kb.mm(pS[:, 0:128], kotok[:, c, :], vt[0][:, c, :], True, False, ["gl_kotok", "gl_vtok0"], [pkS])
                            kb.mm(pS[:, 0:128], kotok[:, c, :], vt[1][:, c, :], False, True, ["gl_kotok", "gl_vtok1"], [pkS])
                            po, pko = kb.ps()
                            kb.mm(po[:, 0:128], vt[0][:, c, :], att[k][:, 0, :], True, False, ["gl_vtok0", f"gl_att{k}"], [pko])
                            kb.mm(po[:, 0:128], vt[1][:, c, :], att[k][:, 1, :], False, False, ["gl_vtok1", f"gl_att{k}"], [pko])
                            kb.mm(po[:, 0:128], Sbd[:], qin[:, tok], False, True, ["gl_Sb", "gl_qin"], [pko])
                            if d == 0:
                                kb.cp("act", oacc[:, tok], po[:, 0:128], [pko], ["gl_oacc"])
                            else:
                                kb.tt("dve", oacc[:, tok], oacc[:, tok], po[:, 0:128], ALU.add, [pko, "gl_oacc"], ["gl_oacc"])
                            kb.stt("dve", Sf[:], Sf[:], decay[:, c:c + 1], pS[:, 0:128], ALU.mult, ALU.add,
                                   ["gl_S", "gl_decay", pkS], ["gl_S"])
                            kb.tt("pool", Sbd[:], Sf[:], C["blk_mask"][:], ALU.mult, ["gl_S", "blk_mask"], ["gl_Sb"])
            kb.tt("dve", oacc[:, 0:LAT0], oacc[:, 0:LAT0], oacc[:, LAT1:S], ALU.add, ["gl_oacc"], ["gl_oacc"])
            kb.cp("dve", oacc[:, LAT1:S], oacc[:, 0:LAT0], ["gl_oacc"], ["gl_oacc"])
            with kb.scope():
                sq = kb.ring("gl_sq", 2, [128, 512], F32)
                og = kb.ring("gl_og", 2, [128, 512], F32)
                ob = kb.ring("gl_ob", 2, [128, 512], BF16)
                for b in range(NBLK):
                    k = b % 2
                    t0 = b * 512
                    kb.dma(og[k][:], pT.ap()[768 + 128 * hp:768 + 128 * (hp + 1), t0:t0 + 512], r=["pT"], w=[f"gl_og{k}"], q="sp")
                    kb.act(sq[k][:], oacc[:, t0:t0 + 512], AF.Square, ["gl_oacc"], [f"gl_sq{k}"])
                    pst, pk = kb.ps()
                    kb.mm(pst[:], C["blk_ones"][:], sq[k][:], True, True, ["blk_ones", f"gl_sq{k}"], [pk])
                    kb.act(sq[k][:], pst[:], AF.Sqrt, [pk, "eps_t"], [f"gl_sq{k}"], bias=C["eps"][:, 0:1])
                    kb.recip(sq[k][:], sq[k][:], [f"gl_sq{k}"], [f"gl_sq{k}"])
                    kb.act(og[k][:], og[k][:], AF.Silu, [f"gl_og{k}"], [f"gl_og{k}"])
                    kb.tt("pool", sq[k][:], sq[k][:], oacc[:, t0:t0 + 512], ALU.mult, [f"gl_sq{k}", "gl_oacc"], [f"gl_sq{k}"])
                    kb.tt("dve", ob[k][:], sq[k][:], og[k][:], ALU.mult, [f"gl_sq{k}", f"gl_og{k}"], [f"gl_ob{k}"])
                    kb.dma(brT.ap()[128 * hp:128 * (hp + 1), t0:t0 + 512], ob[k][:], r=[f"gl_ob{k}"], w=["brT"])


PI = math.pi
NCS = 66
NS5 = NCS * 128


def stage_s5(kb, A, l, pT, brT, C):
    gsT = kb.dram(f"gsT{l}", [256, S], BF16)
    for ct in range(2):
        with kb.scope():
            yacc = kb.sb("s5_y", [128, S], F32)
            ub = kb.sb("s5_u", [128, S], BF16)
            dsk = kb.sb("s5_d", [128, 1], F32)
            jt = kb.sb("s5_jt", [128, 128], F32)
            kb.dma(dsk[:], A["s5_dskip"][l, ct], w=["s5_d"], q="sp")
            kb.dma(jt[:], A["jtab"][:, :], w=["s5_jt"], q="sp")
            with kb.scope():
                uf = kb.ring("s5_uf", 2, [128, 2176], F32)
                for q4 in range(4):
                    k = q4 % 2
                    sl = slice(2176 * q4, 2176 * (q4 + 1))
                    kb.dma(uf[k][:], pT.ap()[1056 + 128 * ct:1056 + 128 * (ct + 1), sl], r=["pT"], w=[f"s5_uf{k}"], q="sp")
                    kb.cp("act", ub[:, sl], uf[k][:], [f"s5_uf{k}"], ["s5_u"])
                    kb.ts("dve", yacc[:, sl], uf[k][:], dsk[:, 0:1], None, ALU.mult, None, [f"s5_uf{k}", "s5_d"], ["s5_y"])
            kb.memset("dve", yacc[:, LAT1:S], 0.0, ["s5_y"])
            for tl in range(4):
                for d in range(2):
                    s5_tile_dir(kb, A, l, ct, tl, d, yacc, ub, jt, C)
            kb.tt("dve", yacc[:, 0:LAT0], yacc[:, 0:LAT0], yacc[:, LAT1:S], ALU.add, ["s5_y"], ["s5_y"])
            kb.cp("dve", yacc[:, LAT1:S], yacc[:, 0:LAT0], ["s5_y"], ["s5_y"])
            with kb.scope():
                tmp = kb.ring("s5_gt", 2, [128, 512], F32)
                gb = kb.ring("s5_gb", 2, [128, 512], BF16)
                for b in range(NBLK):
                    k = b % 2
                    t0 = b * 512
                    gelu_tanh(kb, gb[k][:], yacc[:, t0:t0 + 512], tmp[k][:], ["s5_y"], [f"s5_gb{k}"], f"s5_gt{k}")
                    kb.dma(gsT.ap()[128 * ct:128 * (ct + 1), t0:t0 + 512], gb[k][:], r=[f"s5_gb{k}"], w=["gsT"])
    with kb.scope():
        wg = kb.sb("s5_wg", [128, 2, 256], BF16)
        bg = kb.sb("s5_bg", [128, 2], F32)
        kb.dma(wg[:], A["s5_w_glu"][l].rearrange("(kt p) c -> p kt c", p=128), w=["s5_wg"], q="pool")
        kb.dma(bg[:], A["s5_bglu"][l], w=["s5_bg"], q="sp")
        gin = kb.ring("s5_gin", 2, [128, 2, 512], BF16)
        sg = kb.ring("s5_sg", 2, [128, 512], F32)
        ob = kb.ring("s5_ob", 2, [128, 512], BF16)
        it = 0
        for b in range(NBLK):
            k = b % 2
            t0 = b * 512
            kb.dma(gin[k][:], gsT.ap()[:, t0:t0 + 512].rearrange("(kt p) t -> p kt t", p=128), r=["gsT"], w=[f"s5_gin{k}"], q="sp")
            for mt in range(2):
                kk = it % 2
                it += 1
                pst, pk = kb.ps()
                for kt in range(2):
                    kb.mm(pst[:], wg[:, kt, 128 * mt:128 * (mt + 1)], gin[k][:, kt, :], kt == 0, kt == 1, ["s5_wg", f"s5_gin{k}"], [pk])
                kb.act(sg[kk][:], pst[:], AF.Sigmoid, [pk, "s5_bg"], [f"s5_sg{kk}"], bias=bg[:, mt:mt + 1])
                kb.tt("dve", ob[kk][:], sg[kk][:], gin[k][:, mt, :], ALU.mult, [f"s5_sg{kk}", f"s5_gin{k}"], [f"s5_ob{kk}"])
                kb.dma(brT.ap()[256 + 128 * mt:256 + 128 * (mt + 1), t0:t0 + 512], ob[kk][:], r=[f"s5_ob{kk}"], w=["brT"])


def s5_tile_dir(kb, A, l, ct, tl, d, yacc, ub, jt, C):
    o0 = 0 if d == 0 else LAT0
    rng_list = FWD_R if d == 0 else BWD_R
    first, last = (0, 127) if d == 0 else (127, 0)
    with kb.scope():
        prm = kb.sb("s5_prm", [128, 3], F32)
        Bz = kb.sb("s5_Bz", [128, 2, 128], BF16)
        Cz = kb.sb("s5_Cz", [128, 2, 128], F32)
        Wz = kb.sb("s5_Wz", [128, 2, 128], BF16)
        kb.dma(prm[:], A["s5_prm"][l, d, ct, tl], w=["s5_prm"], q="sp")
        kb.dma(Bz[:], A["s5_Bz"][l, d, ct, tl], w=["s5_Bz"], q="pool")
        kb.dma(Cz[:], A["s5_Cz"][l, d, ct, tl], w=["s5_Cz"], q="sp")
        sc = kb.sb("s5_sc", [128, 24], F32)
        K_ = "s5_sc"
        col = lambda i: sc[:, i:i + 1]
        kb.act(col(0), prm[:, 2:3], AF.Exp, ["s5_prm"], [K_])
        kb.memset("dve", col(20), -1e-4, [K_])
        kb.act(col(1), prm[:, 0:1], AF.Relu, ["s5_prm", K_], [K_], bias=col(20), scale=-1.0)
        kb.ts("dve", col(1), col(1), -1.0, -1e-4, ALU.mult, ALU.add, [K_], [K_])
        kb.tt("dve", col(2), col(1), col(0), ALU.mult, [K_], [K_])
        kb.tt("dve", col(3), prm[:, 1:2], col(0), ALU.mult, ["s5_prm", K_], [K_])
        ct_ = kb.sb("s5_c", [128, 128], F32)
        st_ = kb.sb("s5_s", [128, 128], F32)
        rp = kb.sb("s5_rp", [128, 128], F32)
        ph = kb.sb("s5_ph", [128, 128], F32)
        kb.ts("dve", ph[:], jt[:], col(3), None, ALU.mult, None, ["s5_jt", K_], ["s5_ph"])
        ti32 = kb.sb("s5_ti", [128, 128], I32)
        for (t_, k_, off) in ((st_, "s5_s", 0.0), (ct_, "s5_c", 0.5 * PI)):
            kb.ts("dve", t_[:], ph[:], off, 1.0 / (2 * PI), ALU.add, ALU.mult, ["s5_ph"], [k_])
            kb.cp("dve", ti32[:], t_[:], [k_], ["s5_ti"])
            kb.cp("dve", t_[:], ti32[:], ["s5_ti"], [k_])
            if off:
                kb.stt("dve", t_[:], t_[:], -2 * PI, ph[:], ALU.mult, ALU.add, [k_, "s5_ph"], [k_])
                kb.ts("dve", t_[:], t_[:], off, None, ALU.add, None, [k_], [k_])
            else:
                kb.stt("dve", t_[:], t_[:], -2 * PI, ph[:], ALU.mult, ALU.add, [k_, "s5_ph"], [k_])
            kb.act(t_[:], t_[:], AF.Sin, [k_], [k_])
        kb.act(rp[:], jt[:], AF.Exp, ["s5_jt", K_], ["s5_rp"], scale=col(2))
        kb.tt("dve", col(4), rp[:, 0:1], ct_[:, 0:1], ALU.mult, ["s5_rp", "s5_c", K_], [K_])
        kb.ts("dve", col(4), col(4), -1.0, None, ALU.add, None, [K_], [K_])
        kb.tt("dve", col(5), rp[:, 0:1], st_[:, 0:1], ALU.mult, ["s5_rp", "s5_s", K_], [K_])
        kb.tt("dve", col(6), col(1), col(1), ALU.mult, [K_], [K_])
        kb.stt("dve", col(6), prm[:, 1:2], prm[:, 1:2], col(6), ALU.mult, ALU.add, ["s5_prm", K_], [K_])
        kb.recip(col(6), col(6), [K_], [K_])
        kb.tt("dve", col(9), col(5), prm[:, 1:2], ALU.mult, [K_, "s5_prm"], [K_])
        kb.stt("dve", col(7), col(4), col(1), col(9), ALU.mult, ALU.add, [K_], [K_])
        kb.tt("dve", col(7), col(7), col(6), ALU.mult, [K_], [K_])
        kb.tt("dve", col(9), col(4), prm[:, 1:2], ALU.mult, [K_, "s5_prm"], [K_])
        kb.stt("dve", col(8), col(5), col(1), col(9), ALU.mult, ALU.subtract, [K_], [K_])
        kb.tt("dve", col(8), col(8), col(6), ALU.mult, [K_], [K_])
        kb.ts("dve", col(10), col(8), -1.0, None, ALU.mult, None, [K_], [K_])
        wt = kb.sb("s5_wt", [128, 128], F32)
        kb.ts("dve", wt[:], Cz[:, 1, :], col(10), None, ALU.mult, None, ["s5_Cz", K_], ["s5_wt"])
        kb.stt("dve", Wz[:, 0, :], Cz[:, 0, :], col(7), wt[:], ALU.mult, ALU.add, ["s5_Cz", K_, "s5_wt"], ["s5_Wz"])
        kb.ts("dve", wt[:], Cz[:, 0, :], col(10), None, ALU.mult, None, ["s5_Cz", K_, "s5_Wz"], ["s5_wt"])
        kb.ts("dve", col(11), col(7), -1.0, None, ALU.mult, None, [K_], [K_])
        kb.stt("dve", Wz[:, 1, :], Cz[:, 1, :], col(11), wt[:], ALU.mult, ALU.add, ["s5_Cz", K_, "s5_wt"], ["s5_Wz"])
        kb.cp("dve", col(12), ct_[:, 127:128], ["s5_c", K_], [K_])
        kb.cp("dve", col(13), st_[:, 127:128], ["s5_s", K_], [K_])
        kb.ts("dve", col(14), col(13), -1.0, None, ALU.mult, None, [K_], [K_])
        if d == 1:
            for (t_, k_) in ((ct_, "s5_c"), (st_, "s5_s"), (rp, "s5_rp")):
                kb.cp("dve", ph[:], t_[:, ::-1], [k_, "s5_ph"], ["s5_ph"])
                kb.cp("dve", t_[:], ph[:], ["s5_ph"], [k_])
        atab = kb.sb("s5_atab", [128, NS5], F32)
        r1c = rp[:, 0:1] if d == 0 else rp[:, 127:128]
        for q4 in range(4):
            kb.act(atab[:, 2112 * q4:2112 * (q4 + 1)], C["one"][:, 0:1].to_broadcast([128, 2112]), AF.Identity,
                   ["one_t", "s5_rp"], ["s5_atab"], scale=r1c)
        kb.memset("dve", atab[:].rearrange("p (c j) -> p c j", j=128)[:, :, first:first + 1], 0.0, ["s5_atab"])
        wre = kb.sb("s5_wre", [128, NS5], F32)
        wim = kb.sb("s5_wim", [128, NS5], F32)
        c3 = lambda n: ct_[:].unsqueeze(1).to_broadcast([128, n // 128, 128])
        s3 = lambda n: st_[:].unsqueeze(1).to_broadcast([128, n // 128, 128])
        v3 = lambda ap: ap.rearrange("p (c j) -> p c j", j=128)
        tmps = kb.ring("s5_t", 4, [128, 512], F32)
        ti = [0]

        def tmp():
            i = ti[0] % 4
            ti[0] += 1
            return tmps[i], f"s5_t{i}"
        bu_re = kb.ring("s5_bur", 2, [128, 512], F32)
        bu_im = kb.ring("s5_bui", 2, [128, 512], F32)
        bu_i = [0]
        for (t0, n) in rng_list:
            w0 = t0 - o0
            p1, k1 = kb.ps()
            kb.mm(p1[:, 0:n], Bz[:, 0, :], ub[:, t0:t0 + n], True, True, ["s5_Bz", "s5_u"], [k1])
            p2, k2 = kb.ps()
            kb.mm(p2[:, 0:n], Bz[:, 1, :], ub[:, t0:t0 + n], True, True, ["s5_Bz", "s5_u"], [k2])
            bi_ = bu_i[0] % 2
            bu_i[0] += 1
            b1, b1k = bu_re[bi_], f"s5_bur{bi_}"
            b2, b2k = bu_im[bi_], f"s5_bui{bi_}"
            kb.cp("act", b1[:, 0:n], p1[:, 0:n], [k1], [b1k])
            kb.cp("act", b2[:, 0:n], p2[:, 0:n], [k2], [b2k])
            ta, ka = tmp()
            tb, kbk = tmp()
            kb.tt("dve", v3(ta[:, 0:n]), v3(b1[:, 0:n]), c3(n), ALU.mult, [b1k, "s5_c"], [ka])
            kb.tt("dve", v3(tb[:, 0:n]), v3(b2[:, 0:n]), s3(n), ALU.mult, [b2k, "s5_s"], [kbk])
            kb.tt("pool", wre[:, w0:w0 + n], ta[:, 0:n], tb[:, 0:n], ALU.add, [ka, kbk], ["s5_wre"])
            ta, ka = tmp()
            tb, kbk = tmp()
            kb.tt("dve", v3(ta[:, 0:n]), v3(b2[:, 0:n]), c3(n), ALU.mult, [b2k, "s5_c"], [ka])
            kb.tt("dve", v3(tb[:, 0:n]), v3(b1[:, 0:n]), s3(n), ALU.mult, [b1k, "s5_s"], [kbk])
            kb.tt("pool", wim[:, w0:w0 + n], ta[:, 0:n], tb[:, 0:n], ALU.subtract, [ka, kbk], ["s5_wim"])
        if d == 0:
            kb.scan("dve", wre[:], atab[:], wre[:], 0.0, ["s5_atab", "s5_wre"], ["s5_wre"])
            kb.scan("dve", wim[:], atab[:], wim[:], 0.0, ["s5_atab", "s5_wim"], ["s5_wim"])
        else:
            kb.scan("dve", wre[:, ::-1], atab[:, ::-1], wre[:, ::-1], 0.0, ["s5_atab", "s5_wre"], ["s5_wre"])
            kb.scan("dve", wim[:, ::-1], atab[:, ::-1], wim[:, ::-1], 0.0, ["s5_atab", "s5_wim"], ["s5_wim"])
        Ha = kb.sb("s5_Ha", [128, 2, NCS], F32)
        Hb = kb.sb("s5_Hb", [128, 2, NCS], F32)
        et = kb.sb("s5_et", [128, NCS], F32)
        ger = v3(wre[:])[:, :, last]
        gei = v3(wim[:])[:, :, last]
        kb.ts("dve", et[:], gei, col(14), None, ALU.mult, None, ["s5_wim", K_], ["s5_et"])
        kb.stt("dve", Ha[:, 0, :], ger, col(12), et[:], ALU.mult, ALU.add, ["s5_wre", K_, "s5_et"], ["s5_Ha"])
        kb.ts("dve", et[:], ger, col(13), None, ALU.mult, None, ["s5_wre", K_, "s5_Ha"], ["s5_et"])
        kb.stt("dve", Ha[:, 1, :], gei, col(12), et[:], ALU.mult, ALU.add, ["s5_wim", K_, "s5_et"], ["s5_Ha"])
        r128 = rp[:, 127:128] if d == 0 else rp[:, 0:1]
        kb.tt("dve", col(15), r128, col(12), ALU.mult, ["s5_rp", K_], [K_])
        kb.tt("dve", col(16), r128, col(13), ALU.mult, ["s5_rp", K_], [K_])
        cur, ck, nxt, nk = Ha, "s5_Ha", Hb, "s5_Hb"
        sft = 1
        while sft < NCS:
            kb.ts("dve", col(17), col(16), -1.0, None, ALU.mult, None, [K_], [K_])
            m = NCS - sft
            if d == 0:
                dst, src, keep = slice(sft, NCS), slice(0, m), slice(0, sft)
            else:
                dst, src, keep = slice(0, m), slice(sft, NCS), slice(m, NCS)
            kb.cp("dve", nxt[:, :, keep], cur[:, :, keep], [ck], [nk])
            kb.stt("dve", nxt[:, 0, dst], cur[:, 0, src], col(15), cur[:, 0, dst], ALU.mult, ALU.add, [ck, K_, nk], [nk])
            kb.stt("dve", nxt[:, 0, dst], cur[:, 1, src], col(17), nxt[:, 0, dst], ALU.mult, ALU.add, [ck, K_, nk], [nk])
            kb.stt("dve", nxt[:, 1, dst], cur[:, 1, src], col(15), cur[:, 1, dst], ALU.mult, ALU.add, [ck, K_, nk], [nk])
            kb.stt("dve", nxt[:, 1, dst], cur[:, 0, src], col(16), nxt[:, 1, dst], ALU.mult, ALU.add, [ck, K_, nk], [nk])
            kb.tt("dve", col(18), col(15), col(15), ALU.mult, [K_], [K_])
            kb.stt("dve", col(18), col(16), col(17), col(18), ALU.mult, ALU.add, [K_], [K_])
            kb.stt("dve", col(19), col(15), 2.0, col(16), ALU.mult, ALU.mult, [K_], [K_])
            kb.cp("dve", col(15), col(18), [K_], [K_])
            kb.cp("dve", col(16), col(19), [K_], [K_])
            cur, ck, nxt, nk = nxt, nk, cur, ck
            sft *= 2
        Hp, hpk = nxt, nk
        if d == 0:
            kb.memset("dve", Hp[:, :, 0:1], 0.0, [hpk])
            kb.cp("dve", Hp[:, :, 1:NCS], cur[:, :, 0:NCS - 1], [ck, hpk], [hpk])
        else:
            kb.memset("dve", Hp[:, :, NCS - 1:NCS], 0.0, [hpk])
            kb.cp("dve", Hp[:, :, 0:NCS - 1], cur[:, :, 1:NCS], [ck, hpk], [hpk])
        hb_re = kb.ring("s5_hre", 2, [128, 512], BF16)
        hb_im = kb.ring("s5_him", 2, [128, 512], BF16)
        for bi, (t0, n) in enumerate(rng_list):
            w0 = t0 - o0
            c0, nc_ = w0 // 128, n // 128
            rp3 = rp[:].unsqueeze(1).to_broadcast([128, nc_, 128])
            for (wbuf, wk, comp) in ((wre, "s5_wre", 0), (wim, "s5_wim", 1)):
                ta, ka = tmp()
                kb.tt("dve", v3(ta[:, 0:n]), rp3, Hp[:, comp, c0:c0 + nc_].unsqueeze(2).to_broadcast([128, nc_, 128]),
                      ALU.mult, ["s5_rp", hpk], [ka])
                kb.tt("pool", wbuf[:, w0:w0 + n], wbuf[:, w0:w0 + n], ta[:, 0:n], ALU.add, [wk, ka], [wk])
            k = bi % 2
            gr, gi = v3(wre[:, w0:w0 + n]), v3(wim[:, w0:w0 + n])
            ta, ka = tmp()
            tb, kbk = tmp()
            kb.tt("dve", v3(ta[:, 0:n]), gr, c3(n), ALU.mult, ["s5_wre", "s5_c"], [ka])
            kb.tt("dve", v3(tb[:, 0:n]), gi, s3(n), ALU.mult, ["s5_wim", "s5_s"], [kbk])
            kb.tt("pool", hb_re[k][:, 0:n], ta[:, 0:n], tb[:, 0:n], ALU.subtract, [ka, kbk], [f"s5_hre{k}"])
            ta, ka = tmp()
            tb, kbk = tmp()
            kb.tt("dve", v3(ta[:, 0:n]), gr, s3(n), ALU.mult, ["s5_wre", "s5_s"], [ka])
            kb.tt("dve", v3(tb[:, 0:n]), gi, c3(n), ALU.mult, ["s5_wim", "s5_c"], [kbk])
            kb.tt("pool", hb_im[k][:, 0:n], ta[:, 0:n], tb[:, 0:n], ALU.add, [ka, kbk], [f"s5_him{k}"])
            py, ky = kb.ps()
            kb.mm(py[:, 0:n], Wz[:, 0, :], hb_re[k][:, 0:n], True, False, ["s5_Wz", f"s5_hre{k}"], [ky])
            kb.mm(py[:, 0:n], Wz[:, 1, :], hb_im[k][:, 0:n], False, True, ["s5_Wz", f"s5_him{k}"], [ky])
            kb.tt("dve", yacc[:, t0:t0 + n], yacc[:, t0:t0 + n], py[:, 0:n], ALU.add, ["s5_y", ky], ["s5_y"])


FROW = NLAT + 128


def sin_rr(kb, t, k, ti32, tik, shape_ok=True):
    kb.ts("dve", ti32, t, 1.0 / (2 * PI), None, ALU.mult, None, [k], [tik])
    return ti32


def hy_filters(kb, A, l, n_tok, zT_ap, t01_ap, frow_t, brow_t, rowlen):
    nb = max(1, n_tok // 512)
    bw = min(512, n_tok)
    with kb.scope():
        w1 = kb.sb("hf_w1", [33, 64], F32)
        w2 = kb.sb("hf_w2", [64, 64], F32)
        w3 = kb.sb("hf_w3", [64, 512], F32)
        vec = kb.sb("hf_vec", [64, 5], F32)
        ndl = kb.sb("hf_ndl", [128, 4], F32)
        hyb = kb.sb("hf_eps", [128, 1], F32)
        kb.dma(w1[:], A["hy_w1"][l], w=["hf_w1"], q="sp")
        kb.dma(w2[:], A["hy_w2"][l], w=["hf_w2"], q="sp")
        kb.dma(w3[:], A["hy_w3"][l], w=["hf_w3"], q="sp")
        kb.dma(vec[:, 0:3], A["hy_vec"][l], w=["hf_vec"], q="sp")
        kb.dma(ndl[:], A["hy_ndelta"][:, :], w=["hf_ndl"], q="sp")
        kb.tt("dve", vec[:, 3:4], vec[:, 0:1], vec[:, 2:3], ALU.mult, ["hf_vec"], ["hf_vec"])
        kb.tt("dve", vec[:, 4:5], vec[:, 1:2], vec[:, 2:3], ALU.mult, ["hf_vec"], ["hf_vec"])
        h2 = kb.sb("hf_h2", [64, n_tok], F32)
        zb = kb.ring("hf_z", 2, [33, bw], F32)
        h1 = kb.ring("hf_h1", 2, [64, bw], F32)
        tf = kb.ring("hf_tf", 2, [64, bw], F32)
        ti = kb.ring("hf_ti", 2, [64, bw], I32)

        def sin_inplace(t, k, kk):
            kb.ts("dve", tf[kk][:], t, 1.0 / (2 * PI), None, ALU.mult, None, [k], [f"hf_tf{kk}"])
            kb.cp("dve", ti[kk][:], tf[kk][:], [f"hf_tf{kk}"], [f"hf_ti{kk}"])
            kb.cp("dve", tf[kk][:], ti[kk][:], [f"hf_ti{kk}"], [f"hf_tf{kk}"])
            kb.stt("dve", t, tf[kk][:], -2 * PI, t, ALU.mult, ALU.add, [f"hf_tf{kk}", k], [k])
            kb.act(t, t, AF.Sin, [k], [k])

        for b in range(nb):
            k = b % 2
            sl = slice(b * bw, (b + 1) * bw)
            kb.dma(zb[k][:], zT_ap[:, sl], w=[f"hf_z{k}"], q="sp")
            p1, k1 = kb.ps()
            kb.mm(p1[0:64, 0:bw], w1[:], zb[k][:], True, True, ["hf_w1", f"hf_z{k}"], [k1])
            kb.act(h1[k][:], p1[0:64, 0:bw], AF.Identity, [k1, "hf_vec"], [f"hf_h1{k}"], bias=vec[:, 3:4], scale=vec[:, 2:3])
            sin_inplace(h1[k][:], f"hf_h1{k}", k)
            p2, k2 = kb.ps()
            kb.mm(p2[0:64, 0:bw], w2[:], h1[k][:], True, True, ["hf_w2", f"hf_h1{k}"], [k2])
            kb.act(h2[:, sl], p2[0:64, 0:bw], AF.Identity, [k2, "hf_vec"], ["hf_h2"], bias=vec[:, 4:5], scale=vec[:, 2:3])
            sin_inplace(h2[:, sl], "hf_h2", k)
        filt = kb.sb("hf_filt", [128, n_tok], F32)
        row = kb.sb("hf_row", [128, rowlen], BF16)
        t01 = kb.ring("hf_t01", 2, [128, bw], F32)
        ab = kb.ring("hf_ab", 2, [128, bw], F32)
        sums = kb.sb("hf_sums", [128, nb + 2], F32)
        for ft in range(4):
            for b in range(nb):
                k = b % 2
                sl = slice(b * bw, (b + 1) * bw)
                kb.dma(t01[k][:], t01_ap[:, sl], w=[f"hf_t01{k}"], q="sp")
                kb.act(t01[k][:], t01[k][:], AF.Exp, [f"hf_t01{k}", "hf_ndl"], [f"hf_t01{k}"], scale=ndl[:, ft:ft + 1])
                p3, k3 = kb.ps()
                kb.mm(p3[:, 0:bw], w3[:, 128 * ft:128 * (ft + 1)], h2[:, sl], True, True, ["hf_w3", "hf_h2"], [k3])
                kb.tt("dve", filt[:, sl], p3[:, 0:bw], t01[k][:], ALU.mult, [k3, f"hf_t01{k}"], ["hf_filt"])
                kb.act(ab[k][:], filt[:, sl], AF.Abs, ["hf_filt"], [f"hf_ab{k}"])
                kb.P.op("dve", lambda e, o=sums[:, b:b + 1], i=ab[k][:]: e.reduce_sum(out=o, in_=i, axis=AX.X),
                        [f"hf_ab{k}"], ["hf_sums"])
            kb.P.op("dve", lambda e, o=sums[:, nb:nb + 1], i=sums[:, 0:nb]: e.reduce_sum(out=o, in_=i, axis=AX.X),
                    ["hf_sums"], ["hf_sums"])
            kb.ts("dve", sums[:, nb + 1:nb + 2], sums[:, nb:nb + 1], EPS, None, ALU.add, None, ["hf_sums"], ["hf_sums"])
            kb.recip(sums[:, nb + 1:nb + 2], sums[:, nb + 1:nb + 2], ["hf_sums"], ["hf_sums"])
            for q in range(0, rowlen, 2080):
                kb.memset("dve", row[:, q:min(rowlen, q + 2080)], 0.0, ["hf_row"])
            for b in range(nb):
                sl = slice(b * bw, (b + 1) * bw)
                if ft < 2:
                    dst = row[:, n_tok - (b + 1) * bw:n_tok - b * bw]
                    kb.ts("dve", dst, filt[:, sl][:, ::-1], sums[:, nb + 1:nb + 2], None, ALU.mult, None,
                          ["hf_filt", "hf_sums"], ["hf_row"])
                else:
                    dst = row[:, 127 + b * bw:127 + (b + 1) * bw]
                    kb.ts("dve", dst, filt[:, sl], sums[:, nb + 1:nb + 2], None, ALU.mult, None,
                          ["hf_filt", "hf_sums"], ["hf_row"])
            tgt = frow_t if ft < 2 else brow_t
            ctl = ft % 2
            kb.dma(tgt.ap()[128 * ctl:128 * (ctl + 1), :], row[:], r=["hf_row"], w=["hy_rows"], q="sp")


def hy_conv(kb, nblk, Zall, zkey, ctl, frow_t, brow_t, rowlen, convT, C):
    n_tok = nblk * 128
    with kb.scope():
        G = kb.ring("hc_G", 8, [128, n_tok], BF16)
        Zp = kb.ring("hc_Zp", 3, [128, 3 * nblk - 2], BF16)
        ob = kb.ring("hc_ob", 2, [64, 8, 128], F32)
        for i in range(3):
            kb.memset("dve", Zp[i][:], 0.0, [f"hc_Zp{i}"])
        qs = ("sp", "act", "pool")
        for c in range(128):
            ch = 128 * ctl + c
            gi = (2 * c) % 8
            Gf, gfk = G[gi], f"hc_G{gi}"
            Gb, gbk = G[gi + 1], f"hc_G{gi + 1}"
            kb.dma(Gf[:], bass.AP(frow_t, ch * rowlen, [[1, 128], [1, n_tok]]), r=["hy_rows"], w=[gfk], q=qs[(2 * c) % 3])
            kb.dma(Gb[:], bass.AP(brow_t, ch * rowlen, [[1, 128], [1, n_tok]]), r=["hy_rows"], w=[gbk], q=qs[(2 * c + 1) % 3])
            zp, zpk = Zp[c % 3], f"hc_Zp{c % 3}"
            kb.cp("pool", zp[:, nblk - 1:2 * nblk - 1], Zall[:, :, c], [zkey, zpk], [zpk])
            po, pk = kb.ps()
            nm = 2 * nblk
            mi = 0
            for k in range(nblk):
                kb.mm(po[0:nblk, 0:128], zp[:, nblk - 1 - k:2 * nblk - 1 - k],
                      Gf[:, n_tok - 128 * (k + 1):n_tok - 128 * k][:, ::-1], mi == 0, mi == nm - 1, [zpk, gfk], [pk])
                mi += 1
            for k in range(nblk):
                kb.mm(po[0:nblk, 0:128], zp[:, nblk - 1 + k:2 * nblk - 1 + k],
                      Gb[:, 128 * k:128 * (k + 1)][:, ::-1], mi == 0, mi == nm - 1, [zpk, gbk], [pk])
                mi += 1
            o_, ok_ = ob[(c // 8) % 2], f"hc_ob{(c // 8) % 2}"
            kb.evac(o_[0:nblk, c % 8, :], po[0:nblk, 0:128], [pk], [ok_])
            if c % 8 == 7:
                c0 = ch - 7
                kb.dma(convT.ap()[c0:c0 + 8, :].rearrange("c (I i) -> I c i", i=128), o_[0:nblk, :, :], r=[ok_], w=["hy_conv"], q="sp")


def stage_hyena(kb, A, l, pT, brT, C, do_ctx):
    hzT = kb.dram(f"hzT{l}", [256, S], F32)
    hx0T = kb.dram(f"hx0T{l}", [256, S], F32)
    convT = kb.dram(f"hconvT{l}", [256, NLAT], F32)
    convTc = kb.dram(f"hconvTc{l}", [256, NCTX], F32)
    frow = kb.dram(f"hfrow{l}", [256, FROW], BF16)
    brow = kb.dram(f"hbrow{l}", [256, FROW], BF16)
    frowc = kb.dram(f"hfrowc{l}", [256, NCTX + 128], BF16)
    browc = kb.dram(f"hbrowc{l}", [256, NCTX + 128], BF16)
    hy_filters(kb, A, l, NLAT, A["hy_zT"], A["hy_t01"], frow, brow, FROW)
    if do_ctx:
        hy_filters(kb, A, l, NCTX, A["hy_zTc"], A["hy_t01c"], frowc, browc, NCTX + 128)
    for ct in range(2):
        with kb.scope():
            Zall = kb.sb("hy_Zall", [128, 64, 128], BF16)
            Zc = kb.sb("hy_Zc", [128, 2, 128], BF16)
            with kb.scope():
                taps = kb.sb("hy_taps", [128, 3, 3], F32)
                kb.dma(taps[:], A["hy_taps"][l, ct], w=["hy_taps"], q="sp")
                raw = kb.sb("hy_raw", [128, S], F32)
                cv = [kb.sb(f"hy_cv{i}", [128, S], F32) for i in range(3)]
                for wi in range(3):
                    r0 = 1312 + 256 * wi + 128 * ct
                    kb.dma(raw[:], pT.ap()[r0:r0 + 128, :], r=["pT"], w=["hy_raw"], q="sp")
                    for (a0, a1) in ((0, LAT0), (LAT0, LAT1), (LAT1, S)):
                        kb.ts("dve", cv[wi][:, a0:a1], raw[:, a0:a1], taps[:, wi, 1:2], None, ALU.mult, None,
                              ["hy_raw", "hy_taps"], [f"hy_cv{wi}"])
                        for o in (0, 2):
                            off = o - 1
                            lo, hi = max(a0, a0 - off), min(a1, a1 - off)
                            kb.stt("dve", cv[wi][:, lo:hi], raw[:, lo + off:hi + off], taps[:, wi, o:o + 1], cv[wi][:, lo:hi],
                                   ALU.mult, ALU.add, ["hy_raw", "hy_taps", f"hy_cv{wi}"], [f"hy_cv{wi}"])
                kb.tt("dve", cv[2][:], cv[2][:], cv[0][:], ALU.mult, ["hy_cv2", "hy_cv0"], ["hy_cv2"])
                kb.dma(hzT.ap()[128 * ct:128 * (ct + 1), :], cv[2][:], r=["hy_cv2"], w=["hzT"], q="sp")
                kb.dma(hx0T.ap()[128 * ct:128 * (ct + 1), :], cv[1][:], r=["hy_cv1"], w=["hx0T"], q="sp")
                zb16 = kb.sb("hy_zb", [128, S], BF16)
                kb.cp("act", zb16[:], cv[2][:], ["hy_cv2"], ["hy_zb"])
                for blk in range(0, 66):
                    tok = slice(128 * blk, 128 * (blk + 1))
                    if blk % 4 == 0:
                        pst, pk = kb.ps()
                        pb = pst[:].bitcast(BF16)
                    j = blk % 4
                    kb.tr(pb[:, j * 128:(j + 1) * 128], zb16[:, tok], C["ident"][:], ["hy_zb", "ident"], [pk])
                    if blk == 1:
                        kb.evac(Zc[:, 0:2, :], pb[:, 0:256].rearrange("p (c j) -> p c j", j=128), [pk], ["hy_Zc"])
                    if blk >= 2 and (blk % 4 == 3 or blk == 65):
                        b0 = (blk // 4) * 4
                        lo = max(b0, 2)
                        kb.evac(Zall[:, lo - 2:blk - 1, :],
                                pb[:, (lo - b0) * 128:(blk - b0 + 1) * 128].rearrange("p (c j) -> p c j", j=128), [pk], ["hy_Zall"])
            hy_conv(kb, 64, Zall, "hy_Zall", ct, frow, brow, FROW, convT, C)
            if do_ctx:
                hy_conv(kb, 2, Zc, "hy_Zc", ct, frowc, browc, NCTX + 128, convTc, C)
            with kb.scope():
                hb_ = kb.sb("hy_bias", [128, 1], F32)
                kb.dma(hb_[:], A["hy_bias"][l, ct], w=["hy_bias"], q="sp")
                zz = kb.ring("hy_zz", 2, [128, 512], F32)
                x0 = kb.ring("hy_x0", 2, [128, 512], F32)
                cc = kb.ring("hy_cc", 2, [128, 512], F32)
                ob = kb.ring("hy_ob", 2, [128, 512], BF16)
                rows = slice(128 * ct, 128 * (ct + 1))
                it = 0
                for (t0, n, isc) in [(t0, 512, 0) for t0 in range(LAT0, LAT1, 512)] + ([(0, 256, 1)] if do_ctx else []):
                    k = it % 2
                    it += 1
                    kb.dma(zz[k][:, 0:n], hzT.ap()[rows, t0:t0 + n], r=["hzT"], w=[f"hy_zz{k}"], q="sp")
                    kb.dma(x0[k][:, 0:n], hx0T.ap()[rows, t0:t0 + n], r=["hx0T"], w=[f"hy_x0{k}"], q="sp")
                    src = convTc.ap()[rows, 0:n] if isc else convT.ap()[rows, t0 - LAT0:t0 - LAT0 + n]
                    kb.dma(cc[k][:, 0:n], src, r=["hy_conv"], w=[f"hy_cc{k}"], q="sp")
                    kb.stt("dve", cc[k][:, 0:n], zz[k][:, 0:n], hb_[:, 0:1], cc[k][:, 0:n], ALU.mult, ALU.add,
                           [f"hy_zz{k}", "hy_bias", f"hy_cc{k}"], [f"hy_cc{k}"])
                    kb.tt("dve", ob[k][:, 0:n], cc[k][:, 0:n], x0[k][:, 0:n], ALU.mult, [f"hy_cc{k}", f"hy_x0{k}"], [f"hy_ob{k}"])
                    kb.dma(brT.ap()[512 + 128 * ct:512 + 128 * (ct + 1), t0:t0 + n], ob[k][:, 0:n], r=[f"hy_ob{k}"], w=["brT"], q="sp")
                    if isc:
                        kb.dma(brT.ap()[512 + 128 * ct:512 + 128 * (ct + 1), LAT1:S], ob[k][:, 0:n], r=[f"hy_ob{k}"], w=["brT"], q="sp")


def rms_rstd(kb, C, sq, sqk, x, xk, rstd, rk):
    kb.act(sq, x, AF.Square, [xk], [sqk])
    pst, pk = kb.ps()
    n = rstd.shape[-1]
    for kt in range(KT):
        kb.mm(pst[:, 0:n], C["ones_f"][:], sq[:, kt, :], kt == 0, kt == KT - 1, ["ones_f", sqk], [pk])
    kb.act(rstd, pst[:, 0:n], AF.Sqrt, [pk, "eps_t"], [rk], bias=C["eps"][:, 0:1])
    kb.recip(rstd, rstd, [rk], [rk])


def stage_f1(kb, A, l, xres_v, brT, sgT, h2T, gT, C, A1, A2, MV, tok_lo, tok_hi, moe):
    with kb.scope():
        wbr = kb.sb("f1_wbr", [128, 8, D], BF16)
        wo = kb.sb("f1_wo", [128, 8, D], BF16)
        for k4 in range(4):
            kb.dma(wbr[:, 2 * k4:2 * k4 + 2, :], A["w_branch"][l, k4].rearrange("(kt p) c -> p kt c", p=128), w=["f1_wbr"], q="pool")
        for kt in range(0, KT, 2):
            kb.dma(wo[:, kt:kt + 2, :], A["w_out"][l].rearrange("(kt p) c -> p kt c", p=128)[:, kt:kt + 2, :], w=["f1_wo"], q="pool")
        if moe:
            rw = kb.sb("f1_rw", [128, KT, 8], F32)
            rb = kb.sb("f1_rb", [128, 8], F32)
            kb.dma(rw[:], A["moe_router"][0].rearrange("(kt p) e -> p kt e", p=128), w=["f1_rw"], q="sp")
            kb.dma(rb[:], A["moe_rb"][:, :], w=["f1_rb"], q="sp")
            lg = kb.ring("f1_lg", 2, [128, 8], F32)
            e1 = kb.ring("f1_e1", 2, [128, 8], F32)
            e2 = kb.ring("f1_e2", 2, [128, 8], F32)
            mm_ = kb.ring("f1_mm", 2, [128, 4], F32)
            gts = kb.sb("f1_gts", [8, 512], F32)
        br = kb.ring("f1_br", 2, [128, 8, 512], BF16)
        sg = kb.ring("f1_sg", 2, [128, 8, 512], BF16)
        ym = kb.sb("f1_ym", [128, 8, 512], F32)
        ymb = kb.sb("f1_ymb", [128, 8, 512], BF16)
        xs = kb.ring("f1_x", 2, [128, 8, 512], F32)
        sq = kb.sb("f1_sq", [128, 8, 512], F32)
        h2b = kb.ring("f1_h2b", 2, [128, 8, 512], BF16)
        rstd = kb.sb("f1_rstd", [128, 512], F32)
        tmp = kb.ring("f1_tmp", 3, [128, 512], F32)
        ti = 0
        sgi = 0
        for bi, t0 in enumerate(range(tok_lo, tok_hi, 512)):
            kk = bi % 2
            brb, brk = br[kk], f"f1_br{kk}"
            x_, xk = xs[kk], f"f1_x{kk}"
            kb.dma(brb[:], brT.ap()[:, t0:t0 + 512].rearrange("(kt p) t -> p kt t", p=128), r=["brT"], w=[brk], q="sp")
            kb.dma(x_[:], xres_v[:, :, t0:t0 + 512], r=["xres"], w=[xk], q="sp")
            for k4 in range(4):
                sgb, sgk = sg[sgi % 2], f"f1_sg{sgi % 2}"
                sgi += 1
                kb.dma(sgb[:], sgT.ap()[1024 * k4:1024 * (k4 + 1), t0:t0 + 512].rearrange("(kt p) t -> p kt t", p=128),
                       r=["sgT"], w=[sgk], q="sp")
                for mt in range(KT):
                    pst, pk = kb.ps()
                    for kt in range(2):
                        kb.mm(pst[:], wbr[:, 2 * k4 + kt, 128 * mt:128 * (mt + 1)], brb[:, 2 * k4 + kt, :], kt == 0, kt == 1,
                              ["f1_wbr", brk], [pk])
                    if k4 == 0:
                        kb.tt("dve", ym[:, mt, :], pst[:], sgb[:, mt, :], ALU.mult, [pk, sgk], ["f1_ym"])
                    else:
                        t_, tk = tmp[ti % 3], f"f1_tmp{ti % 3}"
                        ti += 1
                        kb.tt("dve", t_[:], pst[:], sgb[:, mt, :], ALU.mult, [pk, sgk], [tk])
                        kb.tt("pool", ym[:, mt, :], ym[:, mt, :], t_[:], ALU.add, ["f1_ym", tk], ["f1_ym"])
            kb.cp("act", ymb[:], ym[:], ["f1_ym"], ["f1_ymb"])
            for mt in range(KT):
                pst, pk = kb.ps()
                for kt in range(KT):
                    kb.mm(pst[:], wo[:, kt, 128 * mt:128 * (mt + 1)], ymb[:, kt, :], kt == 0, kt == KT - 1, ["f1_wo", "f1_ymb"], [pk])
                for (s0, n, isc) in segs(t0, 512):
                    o0 = s0 - t0
                    kb.stt("dve", x_[:, mt, o0:o0 + n], pst[:, o0:o0 + n], MV[:, 2, mt, isc:isc + 1], x_[:, mt, o0:o0 + n],
                           ALU.mult, ALU.add, [pk, f"MV{l}", xk], [xk])
            kb.dma(xres_v[:, :, t0:t0 + 512], x_[:], r=[xk], w=["xres"], q="sp")
            rms_rstd(kb, C, sq[:], "f1_sq", x_[:], xk, rstd[:], "f1_rstd")
            kb.tt("dve", sq[:], x_[:], rstd[:].unsqueeze(1).to_broadcast([128, KT, 512]), ALU.mult, [xk, "f1_rstd"], ["f1_sq"])
            hb, hk = h2b[kk], f"f1_h2b{kk}"
            for (s0, n, isc) in segs(t0, 512):
                o0 = s0 - t0
                for kt in range(KT):
                    if moe:
                        kb.ts("pool", sq[:, kt, o0:o0 + n], sq[:, kt, o0:o0 + n], A2[:, kt, isc:isc + 1], MV[:, 3, kt, isc:isc + 1],
                              ALU.mult, ALU.add, ["f1_sq", f"A2{l}", f"MV{l}"], ["f1_sq"])
                    else:
                        kb.ts("pool", hb[:, kt, o0:o0 + n], sq[:, kt, o0:o0 + n], A2[:, kt, isc:isc + 1], MV[:, 3, kt, isc:isc + 1],
                              ALU.mult, ALU.add, ["f1_sq", f"A2{l}", f"MV{l}"], [hk])
            if moe:
                kb.cp("act", hb[:], sq[:], ["f1_sq"], [hk])
                for g4 in range(4):
                    k2 = g4 % 2
                    tk128 = slice(128 * g4, 128 * (g4 + 1))
                    pl, pkl = kb.ps()
                    for kt in range(KT):
                        kb.mm(pl[:, 0:8], sq[:, kt, tk128], rw[:, kt, :], kt == 0, kt == KT - 1, ["f1_sq", "f1_rw"], [pkl])
                    L, Lk = lg[k2], f"f1_lg{k2}"
                    E1, E1k = e1[k2], f"f1_e1{k2}"
                    E2, E2k = e2[k2], f"f1_e2{k2}"
                    M, Mk = mm_[k2], f"f1_mm{k2}"
                    kb.tt("dve", L[:], pl[:, 0:8], rb[:], ALU.add, [pkl, "f1_rb"], [Lk])
                    kb.P.op("dve", lambda e, o=M[:, 0:1], i=L[:]: e.reduce_max(out=o, in_=i, axis=AX.X), [Lk], [Mk])
                    kb.ts("dve", E1[:], L[:], M[:, 0:1], None, ALU.is_equal, None, [Lk, Mk], [E1k])
                    kb.stt("dve", E2[:], E1[:], -1e30, L[:], ALU.mult, ALU.add, [E1k, Lk], [E2k])
                    kb.P.op("dve", lambda e, o=M[:, 1:2], i=E2[:]: e.reduce_max(out=o, in_=i, axis=AX.X), [E2k], [Mk])
                    kb.ts("dve", E2[:], E2[:], M[:, 1:2], None, ALU.is_equal, None, [E2k, Mk], [E2k])
                    kb.ts("dve", M[:, 2:3], M[:, 0:1], -1.0, None, ALU.mult, None, [Mk], [Mk])
                    kb.act(M[:, 3:4], M[:, 1:2], AF.Sigmoid, [Mk], [Mk], bias=M[:, 2:3])
                    kb.ts("dve", M[:, 2:3], M[:, 3:4], -1.0, 1.0, ALU.mult, ALU.add, [Mk], [Mk])
                    kb.ts("dve", E1[:], E1[:], M[:, 2:3], None, ALU.mult, None, [E1k, Mk], [E1k])
                    kb.stt("dve", E1[:], E2[:], M[:, 3:4], E1[:], ALU.mult, ALU.add, [E2k, Mk, E1k], [E1k])
                    pt_, pkt = kb.ps()
                    kb.tr(pt_[0:8, 0:128], E1[:], C["identf"][:], [E1k, "identf"], [pkt])
                    kb.cp("act", gts[:, tk128], pt_[0:8, 0:128], [pkt], ["f1_gts"])
                kb.dma(gT.ap()[:, t0:t0 + 512], gts[:], r=["f1_gts"], w=["gT"], q="sp")
            kb.dma(h2T.ap()[:, t0:t0 + 512].rearrange("(kt p) t -> p kt t", p=128), hb[:], r=[hk], w=["h2T"], q="sp")


def stage_f2(kb, A, l, xres_v, h2T, gT, C, MV, tok_lo, tok_hi, moe):
    if moe:
        n_exp, n_ht, wgu_of, wd_of, d_h = NEXP, D_EXP // 128, (lambda e: A["moe_w_gu"][0, e]), (lambda e: A["moe_w_down"][0, e]), D_EXP
    else:
        n_exp, n_ht, wgu_of, wd_of, d_h = 1, D_FF // 128, (lambda e: A["ffn_w_gu"][0]), (lambda e: A["ffn_w_down"][0]), D_FF
    CH = 4 if n_ht % 4 == 0 else 2
    NW = 3
    sblocks = [(t0, min(2048, tok_hi - t0)) for t0 in range(tok_lo, tok_hi, 2048)]
    with kb.scope():
        h2b = kb.sb("f2_h2", [128, KT, 2048], BF16)
        yacc = kb.sb("f2_y", [128, KT, 2048], F32)
        wgu = kb.ring("f2_wgu", 2, [128, KT, 2, 128 * CH], BF16)
        wd = kb.ring("f2_wd", NW, [128, CH, D], BF16)
        actc = kb.ring("f2_act", 3, [128, CH, 512], BF16)
        sl_ = kb.ring("f2_sl", 2, [128, 512], F32)
        xb = kb.ring("f2_x", 2, [128, 512], F32)
        if moe:
            gts = kb.sb("f2_gts", [8, 2048], F32)
            sel = kb.sb("f2_sel", [8, 8, 128], F32)
            kb.cp("dve", sel[:], C["identf"][0:8, 0:8].unsqueeze(2).to_broadcast([8, 8, 128]), ["identf"], ["f2_sel"])
            Ge = kb.ring("f2_Ge", 2, [128, 2048], BF16)
            mt_ = kb.ring("f2_mt", 2, [128, 512], F32)
        visits = [(e, h0) for e in range(n_exp) for h0 in range(0, n_ht, CH)]
        wcount = [0]

        def load_w(vi):
            e, h0 = visits[vi]
            k = wcount[0] % NW
            kg = wcount[0] % 2
            wcount[0] += 1
            gu_v = wgu_of(e).rearrange("(kt p) c -> p kt c", p=128)
            wd_v = wd_of(e).rearrange("(ht p) c -> p ht c", p=128)
            for kt in range(0, KT, 4):
                kb.dma(wgu[kg][:, kt:kt + 4, 0, :], gu_v[:, kt:kt + 4, 128 * h0:128 * (h0 + CH)], w=[f"f2_wgu{kg}"], q="pool")
                kb.dma(wgu[kg][:, kt:kt + 4, 1, :], gu_v[:, kt:kt + 4, d_h + 128 * h0:d_h + 128 * (h0 + CH)], w=[f"f2_wgu{kg}"], q="pool")
            kb.dma(wd[k][:], wd_v[:, h0:h0 + CH, :], w=[f"f2_wd{k}"], q="pool")
            return (kg, k)

        for (t0, nt) in sblocks:
            nsb = nt // 512
            for kt in range(KT):
                kb.dma(h2b[:, kt, 0:nt], h2T.ap()[128 * kt:128 * (kt + 1), t0:t0 + nt], r=["h2T"], w=["f2_h2"], q="sp")
            if moe:
                kb.dma(gts[:, 0:nt], gT.ap()[:, t0:t0 + nt], r=["gT"], w=["f2_gts"], q="sp")
            wslot = {0: load_w(0)}
            pend = None
            si = 0

            def emit_down(p):
                ac, ack, wd_, wdk, tsl, first = p
                for mt in range(KT):
                    pd, pkd = kb.ps()
                    for hh in range(CH):
                        kb.mm(pd[:], wd_[:, hh, 128 * mt:128 * (mt + 1)], ac[:, hh, :], hh == 0, hh == CH - 1, [wdk, ack], [pkd])
                    if first:
                        kb.cp("act", yacc[:, mt, tsl], pd[:], [pkd], ["f2_y"])
                    else:
                        kb.tt("dve", yacc[:, mt, tsl], yacc[:, mt, tsl], pd[:], ALU.add, [pkd, "f2_y"], ["f2_y"])

            for vi, (e, h0) in enumerate(visits):
                if vi + 1 < len(visits):
                    wslot[vi + 1] = load_w(vi + 1)
                kg, k = wslot[vi]
                wg_, wgk, wd_, wdk = wgu[kg], f"f2_wgu{kg}", wd[k], f"f2_wd{k}"
                if moe and h0 == 0:
                    G_, Gk = Ge[e % 2], f"f2_Ge{e % 2}"
                    for sb_ in range(nsb):
                        pg, pkg = kb.ps()
                        kb.mm(pg[:], sel[:, e, :], gts[:, 512 * sb_:512 * (sb_ + 1)], True, True, ["f2_sel", "f2_gts"], [pkg])
                        kb.cp("act", G_[:, 512 * sb_:512 * (sb_ + 1)], pg[:], [pkg], [Gk])
                for sb_ in range(nsb):
                    tsl = slice(512 * sb_, 512 * (sb_ + 1))
                    ac, ack = actc[si % 3], f"f2_act{si % 3}"
                    si += 1
                    for hh in range(CH):
                        pgt, pkg = kb.ps()
                        for kt in range(KT):
                            kb.mm(pgt[:], wg_[:, kt, 0, 128 * hh:128 * (hh + 1)], h2b[:, kt, tsl], kt == 0, kt == KT - 1, [wgk, "f2_h2"], [pkg])
                        put, pku = kb.ps()
                        for kt in range(KT):
                            kb.mm(put[:], wg_[:, kt, 1, 128 * hh:128 * (hh + 1)], h2b[:, kt, tsl], kt == 0, kt == KT - 1, [wgk, "f2_h2"], [pku])
                        s_, sk = sl_[hh % 2], f"f2_sl{hh % 2}"
                        kb.act(s_[:], pgt[:], AF.Silu, [pkg], [sk])
                        if moe:
                            m_, mk = mt_[hh % 2], f"f2_mt{hh % 2}"
                            kb.tt("dve", m_[:], s_[:], put[:], ALU.mult, [sk, pku], [mk])
                            kb.tt("pool", ac[:, hh, :], m_[:], G_[:, tsl], ALU.mult, [mk, Gk], [ack])
                        else:
                            kb.tt("dve", ac[:, hh, :], s_[:], put[:], ALU.mult, [sk, pku], [ack])
                    if pend is not None:
                        emit_down(pend)
                    pend = (ac, ack, wd_, wdk, tsl, vi == 0)
            emit_down(pend)
            xi = 0
            for sb_ in range(nsb):
                tb = t0 + 512 * sb_
                for mt in range(KT):
                    x_, xk = xb[xi % 2], f"f2_x{xi % 2}"
                    xi += 1
                    dst = xres_v[:, mt, tb:tb + 512]
                    kb.dma(x_[:], dst, r=["xres"], w=[xk], q="sp")
                    for (s0, n, isc) in segs(tb, 512):
                        o0 = s0 - tb
                        kb.stt("dve", x_[:, o0:o0 + n], yacc[:, mt, 512 * sb_ + o0:512 * sb_ + o0 + n], MV[:, 5, mt, isc:isc + 1],
                               x_[:, o0:o0 + n], ALU.mult, ALU.add, ["f2_y", f"MV{l}", xk], [xk])
                    kb.dma(dst, x_[:], r=[xk], w=["xres"], q="sp")


def stage_final(kb, A, xres_v, C):
    with kb.scope():
        fg = kb.sb("fn_g", [128, KT], F32)
        kb.dma(fg[:], A["final_g"][:, :], w=["fn_g"], q="sp")
        xs = kb.ring("fn_x", 2, [128, KT, 512], F32)
        sq = kb.ring("fn_sq", 2, [128, KT, 512], F32)
        rs = kb.ring("fn_rs", 2, [128, 512], F32)
        out_v = A["out"].rearrange("(kt p) t -> p kt t", p=128)
        for bi, t0 in enumerate(range(LAT0, LAT0 + NOUT, 512)):
            k = bi % 2
            kb.dma(xs[k][:], xres_v[:, :, t0:t0 + 512], r=["xres"], w=[f"fn_x{k}"], q="sp")
            rms_rstd(kb, C, sq[k][:], f"fn_sq{k}", xs[k][:], f"fn_x{k}", rs[k][:], f"fn_rs{k}")
            kb.tt("dve", sq[k][:], xs[k][:], rs[k][:].unsqueeze(1).to_broadcast([128, KT, 512]), ALU.mult,
                  [f"fn_x{k}", f"fn_rs{k}"], [f"fn_sq{k}"])
            kb.tt("pool", sq[k][:], sq[k][:], fg[:].unsqueeze(2).to_broadcast([128, KT, 512]), ALU.mult,
                  [f"fn_sq{k}", "fn_g"], [f"fn_sq{k}"])
            kb.dma(out_v[:, :, t0 - LAT0:t0 - LAT0 + 512], sq[k][:], r=[f"fn_sq{k}"], w=["out"], q="sp")


def build(nc, A, stop_after=None, debug_outs=()):
    kb = KB(nc, debug_outs)
    P = kb.P
    final_ops = []
    with kb.st:
        kb.init_psum()
        xres = kb.dram("xres", [D, S], F32)
        pT = kb.dram("pT", [MIXC, S], F32)
        sgT = kb.dram("sgT", [4 * D, S], BF16)
        brT = kb.dram("brT", [D, S], BF16)
        h2T = kb.dram("h2T", [D, S], BF16)
        gT = kb.dram("gT", [8, S], F32)
        xres_v = xres.ap().rearrange("(kt p) t -> p kt t", p=128)

        ones_f = kb.sb("ones_f", [128, 128], F32)
        kb.memset("pool", ones_f[:], 1.0 / D, ["ones_f"])
        eps_t = kb.sb("eps_t", [128, 1], F32)
        kb.memset("pool", eps_t[:], EPS, ["eps_t"])
        C = {}
        identf = kb.sb("identf", [128, 128], F32)
        kb.memset("pool", identf[:], 0.0, ["identf"])
        P.op("pool", lambda e: e.affine_select(out=identf[:], in_=identf[:], pattern=[[-1, 128]], compare_op=ALU.not_equal,
                                               fill=1.0, base=0, channel_multiplier=1), ["identf"], ["identf"])
        C["ident"] = kb.sb("ident", [128, 128], BF16)
        kb.cp("dve", C["ident"][:], identf[:], ["identf"], ["ident"])
        C["identf"] = identf
        for nm, sg in (("mask_f", -1), ("mask_b", 1)):
            mk = kb.sb(nm, [128, 128], F32)
            kb.memset("pool", mk[:], 1.0, [nm])
            P.op("pool", lambda e, mk=mk, sg=sg: e.affine_select(out=mk[:], in_=mk[:], pattern=[[-sg, 128]], compare_op=ALU.is_ge,
                                                                fill=0.0, base=0, channel_multiplier=sg), [nm], [nm])
            C[nm] = mk
        bo = kb.sb("blk_ones", [128, 128], F32)
        kb.memset("pool", bo[:], 0.0, ["blk_ones"])
        kb.memset("pool", bo[0:64, 0:64], 1.0 / 64, ["blk_ones"])
        kb.memset("pool", bo[64:128, 64:128], 1.0 / 64, ["blk_ones"])
        C["blk_ones"] = bo
        bm = kb.sb("blk_mask", [128, 128], F32)
        kb.ts("dve", bm[:], bo[:], 64.0, None, ALU.mult, None, ["blk_ones"], ["blk_mask"])
        C["blk_mask"] = bm
        C["hmask"] = []
        for h in range(2):
            hm = kb.sb(f"hmask{h}", [128, 128], BF16)
            kb.memset("dve", hm[:], 0.0, ["hmask"])
            kb.memset("dve", hm[:, 64 * h:64 * h + 64], 1.0, ["hmask"])
            C["hmask"].append(hm)
        a01 = kb.sb("a01", [128, 512], F32)
        kb.memset("pool", a01[:], 1.0, ["a01"])
        kb.memset("pool", a01[:].rearrange("p (c j) -> p c j", j=128)[:, :, 0:1], 0.0, ["a01"])
        C["a01"] = a01
        C["eps"] = eps_t
        C["ones_f"] = ones_f
        one_t = kb.sb("one_t", [128, 1], F32)
        kb.memset("pool", one_t[:], 1.0, ["one_t"])
        C["one"] = one_t
        c2 = kb.sb("c2", [128, KT, 2], F32)
        sc2 = kb.sb("sc2", [128, KT, 2], F32)
        kb.dma(c2[:], A["c2"][:, :, :], w=["c2"])
        kb.act(sc2[:], c2[:], AF.Silu, ["c2"], ["sc2"])

        xin_v = A["xT"].rearrange("(kt p) t -> p kt t", p=128)
        pos_v = A["posT"].rearrange("(kt p) t -> p kt t", p=128)
        with kb.scope():
            xr = kb.ring("px", 2, [128, KT, 512], F32)
            pr = kb.ring("pp", 2, [128, KT, 512], F32)
            for b in range(NBLK):
                t0 = b * 512
                xs, xk = xr[b % 2], f"px{b % 2}"
                ps_, pk = pr[b % 2], f"pp{b % 2}"
                kb.dma(xs[:], xin_v[:, :, t0:t0 + 512], w=[xk])
                for (s0, n, isc) in segs(t0, 512):
                    if isc:
                        continue
                    kb.dma(ps_[:, :, s0 - t0:s0 - t0 + n], pos_v[:, :, s0 - LAT0:s0 - LAT0 + n], w=[pk])
                    kb.tt("dve", xs[:, :, s0 - t0:s0 - t0 + n], xs[:, :, s0 - t0:s0 - t0 + n],
                          ps_[:, :, s0 - t0:s0 - t0 + n], ALU.add, [xk, pk], [xk])
                kb.dma(xres_v[:, :, t0:t0 + 512], xs[:], r=[xk], w=["xres"])

        A1s = [kb.sb(f"A1_{l}", [128, KT, 2], F32) for l in range(DEPTH)]
        A2s = [kb.sb(f"A2_{l}", [128, KT, 2], F32) for l in range(DEPTH)]
        MVs = [kb.sb(f"MV_{l}", [128, 6, KT, 2], F32) for l in range(DEPTH)]
        for l in range(DEPTH):
            with kb.scope():
                modv = kb.sb(f"modv{l}", [128, 48, 2], F32)
                modb = kb.sb(f"modb{l}", [128, 48], F32)
                g12 = kb.sb(f"g12{l}", [128, 2, KT], F32)
                kb.dma(modb[:], A["mod_b"][l], w=[f"modb{l}"])
                kb.dma(g12[:, 0, :], A["norm1_g"][l], w=[f"g12{l}"])
                kb.dma(g12[:, 1, :], A["norm2_g"][l], w=[f"g12{l}"])
                wm = kb.ring("wmod", 2, [128, KT, 512], F32)
                mw_v = A["mod_w"][l].rearrange("(kt p) c -> p kt c", p=128)
                for j in range(12):
                    wt, wk = wm[j % 2], f"wmod{j % 2}"
                    kb.dma(wt[:], mw_v[:, :, j * 512:(j + 1) * 512], w=[wk])
                    for sub in range(4):
                        ct = j * 4 + sub
                        pst, pk = kb.ps()
                        for kt in range(KT):
                            kb.mm(pst[:, 0:2], wt[:, kt, sub * 128:(sub + 1) * 128], sc2[:, kt, :],
                                  kt == 0, kt == KT - 1, [wk, "sc2"], [pk])
                        kb.ts("dve", modv[:, ct, :], pst[:, 0:2], modb[:, ct:ct + 1], None, ALU.add, None,
                              [pk, f"modb{l}"], [f"modv{l}"])
                A1, A2, MV = A1s[l], A2s[l], MVs[l]
                kb.cp("dve", MV[:].rearrange("p a k s -> p (a k) s"), modv[:], [f"modv{l}"], [f"MV{l}"])
                kb.stt("dve", A1[:], MV[:, 1, :, :], 1.0, g12[:, 0, :].unsqueeze(2).to_broadcast([128, KT, 2]),
                       ALU.add, ALU.mult, [f"MV{l}", f"g12{l}"], [f"A1{l}"])
                kb.stt("dve", A2[:], MV[:, 4, :, :], 1.0, g12[:, 1, :].unsqueeze(2).to_broadcast([128, KT, 2]),
                       ALU.add, ALU.mult, [f"MV{l}", f"g12{l}"], [f"A2{l}"])

            with kb.scope():
                win = kb.sb("win", [128, KT, IN_COLS], BF16)
                wi_v = A["w_in"][l].rearrange("(kt p) c -> p kt c", p=128)
                WQ = 8
                wq_w = IN_COLS // WQ
                for ch in range(WQ):
                    c0, c1 = ch * wq_w, (ch + 1) * wq_w
                    for kt in range(0, KT, 4):
                        kb.dma(win[:, kt:kt + 4, c0:c1], wi_v[:, kt:kt + 4, c0:c1], w=[f"win{ch}"], q="pool")

                def wkeys(c0, w):
                    return [f"win{q}" for q in range(c0 // wq_w, (c0 + w - 1) // wq_w + 1)]
                xr = kb.ring("ax", 2, [128, KT, 512], F32)
                sr = kb.ring("asq", 1, [128, KT, 512], F32)
                hr = kb.ring("ahb", 2, [128, KT, 512], BF16)
                rs = kb.ring("arstd", 2, [128, 512], F32)
                so = kb.ring("aso", 4, [128, 512], F32)
                go = kb.ring("ago", 2, [128, 4, 512], BF16)
                tiles = in_tiles()
                soi = 0
                for b in range(NBLK):
                    t0 = b * 512
                    xs, xk = xr[b % 2], f"ax{b % 2}"
                    sq, sk = sr[0], "asq0"
                    hb, hk = hr[b % 2], f"ahb{b % 2}"
                    rstd, rk = rs[b % 2], f"arstd{b % 2}"
                    kb.dma(xs[:], xres_v[:, :, t0:t0 + 512], r=["xres"], w=[xk], q="sp")
                    kb.act(sq[:], xs[:], AF.Square, [xk], [sk])
                    pst, pk = kb.ps()
                    for kt in range(KT):
                        kb.mm(pst[:], ones_f[:], sq[:, kt, :], kt == 0, kt == KT - 1, ["ones_f", sk], [pk])
                    kb.act(rstd[:], pst[:], AF.Sqrt, [pk, "eps_t"], [rk], bias=eps_t[:, 0:1])
                    kb.recip(rstd[:], rstd[:], [rk], [rk])
                    kb.tt("dve", sq[:], xs[:], rstd[:].unsqueeze(1).to_broadcast([128, KT, 512]), ALU.mult,
                          [xk, rk], [sk])
                    for (s0, n, isc) in segs(t0, 512):
                        o0 = s0 - t0
                        for kt in range(KT):
                            kb.ts("pool", hb[:, kt, o0:o0 + n], sq[:, kt, o0:o0 + n], A1[:, kt, isc:isc + 1],
                                  MV[:, 0, kt, isc:isc + 1], ALU.mult, ALU.add, [sk, f"A1{l}", f"MV{l}"], [hk])
                    if "hdbg" in kb.debug_outs:
                        if b == 0:
                            hdbg = kb.dram("hdbg", [D, S], BF16)
                            mvdbg = kb.dram("mvdbg", [128, 96], F32)
                            final_ops.append(kb.dma(mvdbg.ap()[:, :], MV[:].rearrange("p a k s -> p (a k s)"), r=[f"MV{l}"], w=["mvdbg"]))
                        final_ops.append(kb.dma(hdbg.ap().rearrange("(kt p) t -> p kt t", p=128)[:, :, t0:t0 + 512], hb[:], r=[hk], w=["hdbg"]))
                    for (c0, w) in tiles:
                        pst, pk = kb.ps()
                        for kt in range(KT):
                            kb.mm(pst[0:w, :], win[:, kt, c0:c0 + w], hb[:, kt, :], kt == 0, kt == KT - 1,
                                  wkeys(c0, w) + [hk], [pk])
                        sbuf, sbk = so[soi % 4], f"aso{soi % 4}"
                        soi += 1
                        kb.evac(sbuf[0:w, :], pst[0:w, :], [pk], [sbk])
                        kb.dma(pT.ap()[c0:c0 + w, t0:t0 + 512], sbuf[0:w, :], r=[sbk], w=["pT"])
                    for g4 in range(8):
                        gbuf, gk = go[g4 % 2], f"ago{g4 % 2}"
                        for j in range(4):
                            c0 = MIXC + (g4 * 4 + j) * 128
                            pst, pk = kb.ps()
                            for kt in range(KT):
                                kb.mm(pst[:], win[:, kt, c0:c0 + 128], hb[:, kt, :], kt == 0, kt == KT - 1,
                                      wkeys(c0, 128) + [hk], [pk])
                            kb.act(gbuf[:, j, :], pst[:], AF.Sigmoid, [pk], [gk])
                        r0 = g4 * 512
                        kb.dma(sgT.ap()[r0:r0 + 512, t0:t0 + 512].rearrange("(j p) t -> p j t", p=128), gbuf[:],
                               r=[gk], w=["sgT"])
            if stop_after == f"A{l}":
                break
            stage_rglru(kb, A, l, pT, brT)
            if stop_after == f"D{l}":
                break
            stage_gla(kb, A, l, pT, brT, C)
            if stop_after == f"B{l}":
                break
            stage_s5(kb, A, l, pT, brT, C)
            if stop_after == f"C{l}":
                break
            stage_hyena(kb, A, l, pT, brT, C, do_ctx=(l < DEPTH - 1))
            if stop_after == f"E{l}":
                break
            last = (l == DEPTH - 1)
            tok_lo, tok_hi = (LAT0, LAT0 + NOUT) if last else (0, S)
            stage_f1(kb, A, l, xres_v, brT, sgT, h2T, gT, C, A1s[l], A2s[l], MVs[l], tok_lo, tok_hi, moe=(l % 2 == 1))
            if stop_after == f"F{l}":
                break
            stage_f2(kb, A, l, xres_v, h2T, gT, C, MVs[l], tok_lo, tok_hi, moe=(l % 2 == 1))
            if stop_after == f"G{l}":
                break
            continue
            if stop_after == f"E{l}":
                break
            if stop_after == f"C{l}":
                break
            if stop_after == f"B{l}":
                break
            if stop_after == f"D{l}":
                break

        if stop_after is None:
            stage_final(kb, A, xres_v, C)
        fin = []
        for key in ("pT", "sgT", "xres", "brT", "out", "hy_conv", "hy_rows", "h2T", "gT"):
            if key in P.last_w:
                fin.append(P.last_w[key])
        P.emit(final_wait_ops=sorted(set(fin + final_ops)))
    return nc


def host_prep(inputs, b, rev=False):
    f = np.float32
    m = {}
    x = np.asarray(inputs["x"][b], f)
    ctx = np.asarray(inputs["ctx"][b], f)
    if rev:
        x = x[::-1]
        ctx = ctx[::-1]
    xT = np.empty((D, S), f)
    xT[:, 0:LAT0] = ctx.T
    xT[:, LAT0:LAT1] = x.T
    xT[:, LAT1:S] = ctx.T
    m["xT"] = xT
    c2 = np.stack([np.asarray(inputs["c"][b], f), np.asarray(inputs["c_ctx"], f)], -1)
    m["c2"] = np.ascontiguousarray(c2.reshape(KT, 128, 2).transpose(1, 0, 2))
    m["mod_w"] = np.asarray(inputs["mod_w"], f)
    m["mod_b"] = np.ascontiguousarray(np.asarray(inputs["mod_b"], f).reshape(DEPTH, 48, 128).transpose(0, 2, 1))
    m["norm1_g"] = np.ascontiguousarray(np.asarray(inputs["norm1_g"], f).reshape(DEPTH, KT, 128).transpose(0, 2, 1))
    m["norm2_g"] = np.ascontiguousarray(np.asarray(inputs["norm2_g"], f).reshape(DEPTH, KT, 128).transpose(0, 2, 1))
    m["w_in"] = np.asarray(inputs["w_in"], f)
    wc = np.asarray(inputs["rg_w_conv"], f)
    taps = np.zeros((DEPTH, 2, 128, 5), f)
    for ct in range(2):
        taps[:, ct, :, 0:4] = wc[:, :, ct * 128:(ct + 1) * 128].transpose(0, 2, 1)
    m["rg_taps"] = taps
    wbd = np.zeros((DEPTH, 2, 128, 2, 2, 128), f)
    wa = np.asarray(inputs["rg_w_a"], f)
    wx = np.asarray(inputs["rg_w_x"], f)
    for ct in range(2):
        for bb in range(2):
            sl = slice(64 * bb, 64 * bb + 64)
            for d in range(2):
                wbd[:, ct, sl, d, 0, sl] = wa[:, d, 2 * ct + bb]
                wbd[:, ct, sl, d, 1, sl] = wx[:, d, 2 * ct + bb]
    m["rg_wbd"] = wbd
    ba = np.asarray(inputs["rg_b_a"], f).reshape(DEPTH, 2, 2, 128)
    bx = np.asarray(inputs["rg_b_x"], f).reshape(DEPTH, 2, 2, 128)
    m["rg_bias"] = np.ascontiguousarray(np.stack([ba, bx], -1).transpose(0, 2, 3, 1, 4))
    lre = np.asarray(inputs["s5_lam_re"], f); lim = np.asarray(inputs["s5_lam_im"], f); ldt = np.asarray(inputs["s5_log_dt"], f)
    bre = np.asarray(inputs["s5_b_re"], f); bim = np.asarray(inputs["s5_b_im"], f)
    cre = np.asarray(inputs["s5_c_re"], f); cim = np.asarray(inputs["s5_c_im"], f)
    prm = np.zeros((DEPTH, 2, 2, 4, 128, 3), f)
    Bz = np.zeros((DEPTH, 2, 2, 4, 128, 2, 128), f)
    Cz = np.zeros((DEPTH, 2, 2, 4, 128, 2, 128), f)
    for ct in range(2):
        for tl in range(4):
            for gl in range(2):
                g = ct * 8 + tl * 2 + gl
                ps_ = slice(64 * gl, 64 * gl + 64)
                cs_ = slice(32 * tl + 16 * gl, 32 * tl + 16 * gl + 16)
                prm[:, :, ct, tl, ps_, 0] = lre[:, :, g, :]
                prm[:, :, ct, tl, ps_, 1] = lim[:, :, g, :]
                prm[:, :, ct, tl, ps_, 2] = ldt[:, :, g, None]
                Bz[:, :, ct, tl, cs_, 0, ps_] = bre[:, :, g].transpose(0, 1, 3, 2)
                Bz[:, :, ct, tl, cs_, 1, ps_] = bim[:, :, g].transpose(0, 1, 3, 2)
                Cz[:, :, ct, tl, ps_, 0, cs_] = cre[:, :, g].transpose(0, 1, 3, 2)
                Cz[:, :, ct, tl, ps_, 1, cs_] = cim[:, :, g].transpose(0, 1, 3, 2)
    m["s5_prm"], m["s5_Bz"], m["s5_Cz"] = prm, Bz, Cz
    m["s5_dskip"] = np.ascontiguousarray(np.asarray(inputs["s5_d"], f).reshape(DEPTH, 2, 128, 1))
    m["s5_w_glu"] = np.asarray(inputs["s5_w_glu"], f)
    m["s5_bglu"] = np.ascontiguousarray(np.asarray(inputs["s5_b_glu"], f).reshape(DEPTH, 2, 128).transpose(0, 2, 1))
    if rev:
        m["rg_taps"] = m["rg_taps"][..., ::-1]
        m["rg_wbd"] = m["rg_wbd"][:, :, :, ::-1]
        m["rg_bias"] = m["rg_bias"][:, :, :, ::-1]
        m["s5_prm"], m["s5_Bz"], m["s5_Cz"] = m["s5_prm"][:, ::-1], m["s5_Bz"][:, ::-1], m["s5_Cz"][:, ::-1]
    m["jtab"] = np.ascontiguousarray(np.broadcast_to(np.arange(1, 129, dtype=f)[None, :], (128, 128)))
    for k_ in ("w_branch", "w_out", "ffn_w_gu", "ffn_w_down", "moe_router", "moe_w_gu", "moe_w_down"):
        m[k_] = np.asarray(inputs[k_], f)
    m["moe_rb"] = np.ascontiguousarray(np.broadcast_to(np.asarray(inputs["moe_router_b"], f)[0][None, :], (128, 8)))
    m["final_g"] = np.ascontiguousarray(np.asarray(inputs["final_g"], f).reshape(KT, 128).T)
    ws = np.asarray(inputs["hy_w_short"], f)
    m["hy_taps"] = np.ascontiguousarray(ws.reshape(DEPTH, 3, 3, 2, 128).transpose(0, 3, 4, 2, 1))
    m["hy_w1"] = np.asarray(inputs["hy_w1"], f)
    m["hy_w2"] = np.asarray(inputs["hy_w2"], f)
    m["hy_w3"] = np.asarray(inputs["hy_w3"], f)
    m["hy_vec"] = np.ascontiguousarray(np.stack([np.asarray(inputs["hy_b1"], f), np.asarray(inputs["hy_b2"], f),
                                                 np.asarray(inputs["hy_freq"], f)], -1))
    m["hy_bias"] = np.ascontiguousarray(np.asarray(inputs["hy_bias"], f).reshape(DEPTH, 2, 128, 1))
    wu = np.asarray(inputs["gla_w_up"], f)
    wup = np.zeros((DEPTH, 2, 32, 2, 128), f)
    for hp in range(2):
        for d in range(2):
            wup[:, hp, 16 * d:16 * d + 16, d, :] = wu[:, d, :, 128 * hp:128 * (hp + 1)]
    m["gla_wup"] = wup
    m["gla_bup"] = np.ascontiguousarray(np.asarray(inputs["gla_b_up"], f).reshape(DEPTH, 2, 2, 128).transpose(0, 2, 3, 1))
    m["rg_lam"] = np.ascontiguousarray(np.asarray(inputs["rg_lam"], f).reshape(DEPTH, 2, 2, 128).transpose(0, 2, 3, 1))
    if rev:
        m["rg_lam"] = m["rg_lam"][..., ::-1]
        m["gla_wup"] = m["gla_wup"][:, :, :, ::-1]
        m["gla_bup"] = m["gla_bup"][..., ::-1]
        m["hy_taps"] = m["hy_taps"][..., ::-1]
        m["hy_w3"] = np.concatenate([m["hy_w3"][..., 256:], m["hy_w3"][..., :256]], -1)
    for k_ in list(m):
        m[k_] = np.ascontiguousarray(m[k_], dtype=f)
    return m


def pos_table():
    rows = NLAT // 64
    q = D // 4
    omega = (1.0 / (10000.0 ** (np.arange(q, dtype=np.float32) / np.float32(q)))).astype(np.float32)
    r = np.arange(rows, dtype=np.float32)[:, None] * omega
    cc = np.arange(64, dtype=np.float32)[:, None] * omega
    er = np.concatenate([np.sin(r), np.cos(r)], -1)
    ec = np.concatenate([np.sin(cc), np.cos(cc)], -1)
    emb = np.concatenate([np.broadcast_to(er[:, None], (rows, 64, D // 2)),
                          np.broadcast_to(ec[None], (rows, 64, D // 2))], -1).reshape(NLAT, D)
    return np.ascontiguousarray(emb.T.astype(np.float32))


def hyena_consts():
    f = np.float32
    out = {}
    for nm, n in (("", NLAT), ("c", NCTX)):
        t = np.arange(n, dtype=f)[:, None]
        bands = np.linspace(1e-4, 15, 16, dtype=f)[None]
        ang = (f(2.0 * math.pi) * bands * t / f(n)).astype(f)
        z = np.concatenate([t / f(n), np.cos(ang), np.sin(ang)], -1).astype(f)
        out["hy_zT" + nm] = np.ascontiguousarray(z.T)
        t01 = (t[:, 0] / f(max(n - 1, 1))).astype(f)
        out["hy_t01" + nm] = np.ascontiguousarray(np.broadcast_to(t01[None, :], (128, n)))
    deltas = np.abs(np.linspace(math.log(1e-2) / 0.3, math.log(1e-2) / 1.5, 256, dtype=f))
    nd = -np.tile(deltas, 2).astype(f)
    out["hy_ndelta"] = np.ascontiguousarray(nd.reshape(4, 128).T)
    return out


def make_program(in_map, stop_after=None, debug_outs=()):
    nc = bass.Bass("TRN2", target_bir_lowering=False)
    A = {}
    for k, v in in_map.items():
        A[k] = nc.dram_tensor(k, list(v.shape), F32, kind="ExternalInput").ap()
    A["out"] = nc.dram_tensor("out", [D, NOUT], F32, kind="ExternalOutput").ap()
    build(nc, A, stop_after, debug_outs)
    return nc


def kernel(**inputs):
    pos = pos_table()
    pos_r = np.ascontiguousarray(pos[:, ::-1])
    hyc = hyena_consts()
    maps = []
    for b in range(4):
        for rev in (False, True):
            m = host_prep(inputs, b, rev)
            m["posT"] = pos_r if rev else pos
            m.update(hyc)
            maps.append(m)
    nc = make_program(maps[0], stop_after=os.environ.get("KSTOP"))
    res = run_bass_kernel_spmd(nc, maps, core_ids=list(range(8)))
    out = np.empty((4, NLAT, D), np.float32)
    for b in range(4):
        out[b, 0:NOUT] = res.results[2 * b]["out"].T
        out[b, NOUT:NLAT] = res.results[2 * b + 1]["out"].T[::-1]
    return out
```

```python
import os
import math
import contextlib
import numpy as np
import concourse.bass as bass
import concourse.mybir as mybir
from concourse.bass_utils import run_bass_kernel_spmd

F32 = mybir.dt.float32
BF16 = mybir.dt.bfloat16
I32 = mybir.dt.int32
AF = mybir.ActivationFunctionType
ALU = mybir.AluOpType
AX = mybir.AxisListType

D = 1024
KT = 8
NCTX = 256
NLAT = 8192
S = NCTX + NLAT + NCTX
NBLK = S // 512
LAT0, LAT1 = NCTX, NCTX + NLAT
DEPTH = 2
IN_COLS = 6688
MIXC = 2592
EPS = 1e-6
D_FF = 2816
D_EXP = 3584
NEXP = 8
NOUT = NLAT // 2

ENGS = ("pe", "act", "dve", "pool", "sp")
EPOCH = 4000
NDMASEM = 8
DEPOCH = 200


class Prog:
    def __init__(self, nc):
        self.nc = nc
        self.ops = []
        self.deps = []
        self.last_w = {}
        self.readers = {}
        self.bar = set()
        self.bar_done = set(ENGS)
        self.last_c = {}
        self.last_d = {e: [] for e in ENGS}

    def barrier(self):
        b = set(self.last_c.values())
        for e in ENGS:
            b.update(self.last_d[e])
        self.bar = b
        self.bar_done = set()

    def op(self, eng, fn, reads=(), writes=(), dma=False):
        i = len(self.ops)
        d = set()
        if eng not in self.bar_done:
            d.update(self.bar)
            self.bar_done.add(eng)
        if dma:
            self.last_d[eng] = (self.last_d[eng] + [i])[-NDMASEM:]
        else:
            self.last_c[eng] = i
        for r in reads:
            if r in self.last_w:
                d.add(self.last_w[r])
        for w in writes:
            if w in self.last_w:
                d.add(self.last_w[w])
            lastr = {}
            for rr in self.readers.get(w, ()):
                if self.ops[rr][2]:
                    d.add(rr)
                else:
                    lastr[self.ops[rr][0]] = rr
            d.update(lastr.values())
        for w in writes:
            self.last_w[w] = i
            self.readers[w] = []
        for r in reads:
            self.readers.setdefault(r, []).append(i)
        d.discard(i)
        self.ops.append((eng, fn, dma))
        self.deps.append(sorted(d))
        return i

    def emit(self, final_wait_ops=()):
        nc = self.nc
        ops, deps = self.ops, self.deps
        n = len(ops)
        waited = [False] * n
        for i in range(n):
            for j in deps[i]:
                if ops[j][0] == "pe" and ops[i][0] == "pe" and not ops[j][2] and not ops[i][2]:
                    continue
                waited[j] = True
        for j in final_wait_ops:
            waited[j] = True
        ms = [None] * n
        cnt = {e: 0 for e in ENGS}
        dcnt = {e: 0 for e in ENGS}
        dnum = [None] * n
        for i in range(n):
            e, _, isd = ops[i]
            if isd:
                dnum[i] = dcnt[e]
                dcnt[e] += 1
            elif waited[i]:
                ms[i] = cnt[e]
                cnt[e] += 1
        stack = contextlib.ExitStack()
        with stack:
            csem = {}
            for e in ENGS:
                for ep in range((cnt[e] + EPOCH - 1) // EPOCH):
                    csem[(e, ep)] = stack.enter_context(nc.semaphore(f"c_{e}_{ep}"))
            dsem = {}
            for e in ENGS:
                if dcnt[e]:
                    nep = (dcnt[e] + NDMASEM * DEPOCH - 1) // (NDMASEM * DEPOCH)
                    for k in range(NDMASEM * nep):
                        dsem[(e, k)] = stack.enter_context(nc.semaphore(f"d_{e}_{k}"))

            def dslot(k):
                ep = k // (NDMASEM * DEPOCH)
                kk = k % (NDMASEM * DEPOCH)
                return ep * NDMASEM + (kk % NDMASEM), kk // NDMASEM

            block = stack.enter_context(nc.Block())
            per_eng = {e: [i for i in range(n) if ops[i][0] == e] for e in ENGS}
            engobj = {"pe": "tensor", "act": "scalar", "dve": "vector", "pool": "gpsimd", "sp": "sync"}

            def make(e):
                def body(eng):
                    seen_c = {}
                    seen_d = set()

                    def wait_for(j):
                        ej, _, isd = ops[j]
                        if isd:
                            if j in seen_d:
                                return
                            seen_d.add(j)
                            sl, rnd = dslot(dnum[j])
                            eng.wait_ge(dsem[(ej, sl)], 16 * (rnd + 1))
                        else:
                            m = ms[j]
                            if m is None:
                                return
                            if seen_c.get(ej, -1) >= m:
                                return
                            seen_c[ej] = m
                            eng.wait_ge(csem[(ej, m // EPOCH)], (m % EPOCH) + 1)

                    for i in per_eng[e]:
                        _, fn, isd = ops[i]
                        for j in deps[i]:
                            if ops[j][0] == "pe" and e == "pe" and not ops[j][2] and not isd:
                                continue
                            wait_for(j)
                        if isd:
                            sl, rnd = dslot(dnum[i])
                            if rnd > 0:
                                eng.wait_ge(dsem[(e, sl)], 16 * rnd)
                            ins = fn(eng)
                            ins.then_inc(dsem[(e, sl)], 16)
                        else:
                            ins = fn(eng)
                            if ms[i] is not None:
                                ins.then_inc(csem[(e, ms[i] // EPOCH)], 1)
                    if e == "sp":
                        for j in final_wait_ops:
                            wait_for(j)
                return body

            for e in ENGS:
                if per_eng[e] or (e == "sp" and final_wait_ops):
                    getattr(block, engobj[e])(make(e))
        return n


class KB:
    def __init__(self, nc, debug_outs=()):
        self.nc = nc
        self.P = Prog(nc)
        self.st = contextlib.ExitStack()
        self.debug_outs = set(debug_outs)
        self.ps_i = 0
        self.psum = []
        self.dq = 0
        self.ev = 0

    def sb(self, name, shape, dt=F32):
        self.uid = getattr(self, "uid", 0) + 1
        return self.st.enter_context(self.nc.sbuf_tensor(f"s{self.uid}_{name}", list(shape), dt))

    @contextlib.contextmanager
    def scope(self):
        old = self.st
        with contextlib.ExitStack() as sst:
            self.st = sst
            try:
                yield
            finally:
                self.st = old
                self.P.barrier()

    def ring(self, name, n, shape, dt=F32):
        return [self.sb(f"{name}{i}", shape, dt) for i in range(n)]

    def dram(self, name, shape, dt=F32):
        kind = "ExternalOutput" if name in self.debug_outs else "Internal"
        return self.nc.dram_tensor(name, list(shape), dt, kind=kind)

    def init_psum(self):
        for i in range(8):
            self.psum.append(self.st.enter_context(self.nc.psum_tensor(f"ps{i}", [128, 512], F32)))

    def ps(self):
        i = self.ps_i % 8
        self.ps_i += 1
        return self.psum[i], f"ps{i}"

    def dma(self, out, in_, r=(), w=(), q=None):
        if q is None:
            q = ("sp", "pool")[self.dq % 2]
            self.dq += 1
        return self.P.op(q, lambda e: e.dma_start(out=out, in_=in_), r, w, dma=True)

    def mm(self, out, lhsT, rhs, start, stop, r, w):
        return self.P.op("pe", lambda e: e.matmul(out, lhsT=lhsT, rhs=rhs, start=start, stop=stop), r, w)

    def tr(self, out, in_, ident, r, w):
        return self.P.op("pe", lambda e: e.transpose(out=out, in_=in_, identity=ident), r, w)

    def act(self, out, in_, func, r, w, bias=None, scale=None):
        kw = {}
        if bias is not None:
            kw["bias"] = bias
        if scale is not None:
            kw["scale"] = scale
        return self.P.op("act", lambda e: e.activation(out=out, in_=in_, func=func, **kw), r, w)

    def tt(self, eng, out, in0, in1, op, r, w):
        return self.P.op(eng, lambda e: e.tensor_tensor(out=out, in0=in0, in1=in1, op=op), r, w)

    def ts(self, eng, out, in0, s1, s2, op0, op1, r, w):
        if s2 is None:
            return self.P.op(eng, lambda e: e.tensor_scalar(out=out, in0=in0, scalar1=s1, scalar2=None, op0=op0), r, w)
        return self.P.op(eng, lambda e: e.tensor_scalar(out=out, in0=in0, scalar1=s1, scalar2=s2, op0=op0, op1=op1), r, w)

    def stt(self, eng, out, in0, scalar, in1, op0, op1, r, w):
        return self.P.op(eng, lambda e: e.scalar_tensor_tensor(out=out, in0=in0, scalar=scalar, in1=in1, op0=op0, op1=op1), r, w)

    def cp(self, eng, out, in_, r, w):
        if eng == "act":
            return self.P.op("act", lambda e: e.copy(out=out, in_=in_), r, w)
        return self.P.op(eng, lambda e: e.tensor_copy(out=out, in_=in_), r, w)

    def evac(self, out, in_, r, w):
        self.ev += 1
        return self.cp(("act", "dve")[self.ev % 2], out, in_, r, w)

    def memset(self, eng, ap, val, w):
        return self.P.op(eng, lambda e: e.memset(ap, val), (), w)

    def scan(self, eng, out, a, b, init, r, w):
        return self.P.op(eng, lambda e: e.tensor_tensor_scan(out=out, data0=a, data1=b, initial=init, op0=ALU.mult, op1=ALU.add), r, w)

    def recip(self, out, in_, r, w):
        return self.P.op("dve", lambda e: e.reciprocal(out=out, in_=in_), r, w)


def segs(t0, n):
    out = []
    for (a, b, c) in ((0, LAT0, 1), (LAT0, LAT1, 0), (LAT1, S, 1)):
        lo, hi = max(a, t0), min(b, t0 + n)
        if hi > lo:
            out.append((lo, hi - lo, c))
    return out


def in_tiles():
    tl = []
    c = 0
    for sz in (256, 256, 256, 256, 32, 256, 768, 256, 256):
        k = 0
        while k < sz:
            w = min(128, sz - k)
            tl.append((c + k, w))
            k += w
        c += sz
    return tl


GELU_C = 2.0 * math.sqrt(2.0 / math.pi)


def gelu_tanh(kb, out, x, tmp, rk, wk, tk):
    kb.act(tmp, x, AF.Square, rk, [tk])
    kb.ts("dve", tmp, tmp, 0.044715, 1.0, ALU.mult, ALU.add, [tk], [tk])
    kb.tt("dve", tmp, tmp, x, ALU.mult, [tk] + list(rk), [tk])
    kb.act(tmp, tmp, AF.Sigmoid, [tk], [tk], scale=GELU_C)
    kb.tt("dve", out, tmp, x, ALU.mult, [tk] + list(rk), wk)


FWD_R = [(t0, min(512, LAT1 - t0)) for t0 in range(0, LAT1, 512)]
BWD_R = [(t0, 512) for t0 in range(S - 512, LAT0, -512)] + [(LAT0, 256)]


def stage_rglru(kb, A, l, pT, brT, tmax=S):
    for ct in range(2):
        with kb.scope():
            xy = kb.sb("rg_xy", [128, S], F32)
            xc = kb.sb("rg_xc", [128, S], F32)
            xcb = kb.sb("rg_xcb", [128, S], BF16)
            taps = kb.sb("rg_taps", [128, 5], F32)
            wbd = kb.sb("rg_wbd", [128, 2, 2, 128], BF16)
            bias = kb.sb("rg_bias", [128, 2, 2], F32)
            lam = kb.sb("rg_lam", [128, 2], F32)
            n8 = kb.sb("rg_n8", [128, 2], F32)
            n16 = kb.sb("rg_n16", [128, 2], F32)
            kb.dma(xy[:], pT.ap()[2080 + 128 * ct:2080 + 128 * (ct + 1), :], r=["pT"], w=["rg_xy"], q="sp")
            kb.dma(taps[:], A["rg_taps"][l, ct], w=["rg_taps"], q="sp")
            kb.dma(wbd[:], A["rg_wbd"][l, ct], w=["rg_wbd"], q="pool")
            kb.dma(bias[:], A["rg_bias"][l, ct], w=["rg_bias"], q="sp")
            kb.dma(lam[:], A["rg_lam"][l, ct], w=["rg_lam"], q="sp")
            kb.act(n8[:], lam[:], AF.Exp, ["rg_lam"], ["rg_n8"], scale=-1.0)
            kb.ts("dve", n8[:], n8[:], 1.0, None, ALU.add, None, ["rg_n8"], ["rg_n8"])
            kb.act(n8[:], n8[:], AF.Ln, ["rg_n8"], ["rg_n8"])
            kb.ts("dve", n16[:], n8[:], -16.0, None, ALU.mult, None, ["rg_n8"], ["rg_n16"])
            kb.ts("dve", n8[:], n8[:], -8.0, None, ALU.mult, None, ["rg_n8", "rg_n16"], ["rg_n8"])
            for (a0, a1) in ((0, LAT0), (LAT0, LAT1), (LAT1, S)):
                kb.ts("dve", xc[:, a0:a1], xy[:, a0:a1], taps[:, 2:3], None, ALU.mult, None,
                      ["rg_xy", "rg_taps"], ["rg_xc"])
                for o in (0, 1, 3, 4):
                    off = o - 2
                    lo, hi = max(a0, a0 - off), min(a1, a1 - off)
                    kb.stt("dve", xc[:, lo:hi], xy[:, lo + off:hi + off], taps[:, o:o + 1], xc[:, lo:hi],
                           ALU.mult, ALU.add, ["rg_xy", "rg_taps", "rg_xc"], ["rg_xc"])
            kb.cp("act", xcb[:], xc[:], ["rg_xc"], ["rg_xcb"])
            y = xy
            kb.memset("pool", y[:, LAT1:S], 0.0, ["rg_xy"])
            tr = kb.ring("rg_r", 2, [128, 512], F32)
            ti = kb.ring("rg_i", 2, [128, 512], F32)
            ta = kb.ring("rg_a", 2, [128, 512], F32)
            tq = kb.ring("rg_q", 2, [128, 512], F32)
            hb = kb.ring("rg_hb", 2, [128, 512], F32)
            it = 0
            for d in range(2):
                prev = None
                for (t0, n) in ([r_ for r_ in FWD_R if r_[0] < tmax] if d == 0 else BWD_R):
                    k = it % 2
                    it += 1
                    r_, i_, a_, q_ = tr[k], ti[k], ta[k], tq[k]
                    rk_, ik_, ak_, qk_ = f"rg_r{k}", f"rg_i{k}", f"rg_a{k}", f"rg_q{k}"
                    p1, pk1 = kb.ps()
                    kb.mm(p1[:, 0:n], wbd[:, d, 0, :], xcb[:, t0:t0 + n], True, True, ["rg_wbd", "rg_xcb"], [pk1])
                    p2, pk2 = kb.ps()
                    kb.mm(p2[:, 0:n], wbd[:, d, 1, :], xcb[:, t0:t0 + n], True, True, ["rg_wbd", "rg_xcb"], [pk2])
                    kb.act(r_[:, 0:n], p1[:, 0:n], AF.Sigmoid, [pk1, "rg_bias"], [rk_], bias=bias[:, d, 0:1])
                    kb.act(i_[:, 0:n], p2[:, 0:n], AF.Sigmoid, [pk2, "rg_bias"], [ik_], bias=bias[:, d, 1:2])
                    kb.act(a_[:, 0:n], r_[:, 0:n], AF.Exp, [rk_, "rg_n8"], [ak_], scale=n8[:, d:d + 1])
                    kb.act(q_[:, 0:n], r_[:, 0:n], AF.Exp, [rk_, "rg_n16"], [qk_], scale=n16[:, d:d + 1])
                    kb.ts("pool", q_[:, 0:n], q_[:, 0:n], -1.0, 1.0, ALU.mult, ALU.add, [qk_], [qk_])
                    kb.act(q_[:, 0:n], q_[:, 0:n], AF.Sqrt, [qk_], [qk_])
                    kb.tt("pool", i_[:, 0:n], i_[:, 0:n], xc[:, t0:t0 + n], ALU.mult, [ik_, "rg_xc"], [ik_])
                    kb.tt("dve", q_[:, 0:n], q_[:, 0:n], i_[:, 0:n], ALU.mult, [qk_, ik_], [qk_])
                    if d == 0:
                        init = 0.0 if prev is None else y[:, t0 - 1:t0]
                        kb.scan("dve", y[:, t0:t0 + n], a_[:, 0:n], q_[:, 0:n], init, [ak_, qk_, "rg_xy"], ["rg_xy"])
                    else:
                        h_, hk_ = hb[k], f"rg_hb{k}"
                        init = 0.0 if prev is None else prev[0][:, 0:1]
                        rr = [ak_, qk_] + ([] if prev is None else [prev[1]])
                        kb.scan("dve", h_[:, 0:n][:, ::-1], a_[:, 0:n][:, ::-1], q_[:, 0:n][:, ::-1], init, rr, [hk_])
                        kb.tt("pool", y[:, t0:t0 + n], y[:, t0:t0 + n], h_[:, 0:n], ALU.add, ["rg_xy", hk_], ["rg_xy"])
                        prev = (h_, hk_)
                    if d == 0:
                        prev = True
            kb.tt("dve", y[:, 0:LAT0], y[:, 0:LAT0], y[:, LAT1:S], ALU.add, ["rg_xy"], ["rg_xy"])
            kb.cp("dve", y[:, LAT1:S], y[:, 0:LAT0], ["rg_xy"], ["rg_xy"])
            gt = kb.ring("rg_g", 2, [128, 512], F32)
            ob = kb.ring("rg_o", 2, [128, 512], BF16)
            for b in range(min(NBLK, tmax // 512)):
                k = b % 2
                t0 = b * 512
                kb.dma(gt[k][:], pT.ap()[2336 + 128 * ct:2336 + 128 * (ct + 1), t0:t0 + 512], r=["pT"], w=[f"rg_g{k}"])
                gelu_tanh(kb, gt[k][:], gt[k][:], tr[k][:], [f"rg_g{k}"], [f"rg_g{k}"], f"rg_r{k}")
                kb.tt("dve", ob[k][:], gt[k][:], y[:, t0:t0 + 512], ALU.mult, [f"rg_g{k}", "rg_xy"], [f"rg_o{k}"])
                kb.dma(brT.ap()[768 + 128 * ct:768 + 128 * (ct + 1), t0:t0 + 512], ob[k][:], r=[f"rg_o{k}"], w=["brT"])


NCH = S // 128


def stage_gla(kb, A, l, pT, brT, C, tmax=S):
    ident, a01 = C["ident"], C["a01"]
    for hp in range(2):
        with kb.scope():
            vt = [kb.sb(f"gl_vtok{h}", [128, NCH, 128], BF16) for h in range(2)]
            oacc = kb.sb("gl_oacc", [128, S], F32)
            wup = kb.sb("gl_wup", [32, 2, 128], F32)
            nb = kb.sb("gl_nb", [128, 2], F32)
            kb.dma(wup[:], A["gla_wup"][l, hp], w=["gl_wup"], q="sp")
            kb.dma(nb[:], A["gla_bup"][l, hp], w=["gl_nb"], q="sp")
            kb.ts("dve", nb[:], nb[:], -1.0, None, ALU.mult, None, ["gl_nb"], ["gl_nb"])
            kb.memset("pool", oacc[:, LAT1:S], 0.0, ["gl_oacc"])
            with kb.scope():
                vb = kb.ring("gl_vb", 2, [128, 512], BF16)
                vfull = kb.ring("gl_vf", 2, [128, 512], BF16)
                for b in range(NBLK):
                    k = b % 2
                    t0 = b * 512
                    kb.dma(vb[k][:], pT.ap()[512 + 128 * hp:512 + 128 * (hp + 1), t0:t0 + 512], r=["pT"], w=[f"gl_vb{k}"], q="pool")
                    pst, pk = kb.ps()
                    pb = pst[:].bitcast(BF16)
                    for j in range(4):
                        kb.tr(pb[:, j * 128:(j + 1) * 128], vb[k][:, j * 128:(j + 1) * 128], ident[:], [f"gl_vb{k}", "ident"], [pk])
                    vf = vfull[b % 2]
                    kb.evac(vf[:], pb[:, 0:512], [pk], [f"gl_vf{b % 2}"])
                    for h in range(2):
                        kb.tt(("pool", "dve")[h], vt[h][:, 4 * b:4 * b + 4, :], vf[:].rearrange("p (c j) -> p c j", j=128),
                              C["hmask"][h][:].unsqueeze(1).to_broadcast([128, 4, 128]), ALU.mult,
                              [f"gl_vf{b % 2}", "hmask"], [f"gl_vtok{h}"])
            for d in range(2):
                with kb.scope():
                    qin = kb.sb("gl_qin", [128, S], BF16)
                    qm = [kb.sb(f"gl_qm{h}", [128, S], BF16) for h in range(2)]
                    kin = kb.sb("gl_kin", [128, S], BF16)
                    kotok = kb.sb("gl_kotok", [128, NCH, 128], BF16)
                    decay = kb.sb("gl_decay", [128, NCH], F32)
                    Sf = kb.sb("gl_S", [128, 128], F32)
                    Sbd = kb.sb("gl_Sb", [128, 128], BF16)
                    kb.memset("pool", Sf[:], 0.0, ["gl_S"])
                    kb.memset("pool", Sbd[:], 0.0, ["gl_Sb"])
                    with kb.scope():
                        qb = kb.ring("gl_qb", 2, [128, 512], BF16)
                        kbf = kb.ring("gl_kb", 2, [128, 512], BF16)
                        gd = kb.ring("gl_gd", 2, [32, 512], F32)
                        cl = kb.ring("gl_cl", 2, [128, 512], F32)
                        e1 = kb.ring("gl_e1", 2, [128, 512], F32)
                        e2 = kb.ring("gl_e2", 2, [128, 512], F32)
                        kob = kb.ring("gl_kob", 2, [128, 512], BF16)
                        for b in range(NBLK if d == 1 else min(NBLK, tmax // 512)):
                            k = b % 2
                            t0 = b * 512
                            kb.dma(qb[k][:], pT.ap()[128 * hp:128 * (hp + 1), t0:t0 + 512], r=["pT"], w=[f"gl_qb{k}"], q="pool")
                            kb.dma(kbf[k][:], pT.ap()[256 + 128 * hp:256 + 128 * (hp + 1), t0:t0 + 512], r=["pT"], w=[f"gl_kb{k}"], q="pool")
                            kb.dma(gd[k][:], pT.ap()[1024:1056, t0:t0 + 512], r=["pT"], w=[f"gl_gd{k}"], q="sp")
                            pst, pk = kb.ps()
                            kb.mm(pst[:], wup[:, d, :], gd[k][:], True, True, ["gl_wup", f"gl_gd{k}"], [pk])
                            c_, ck = cl[k], f"gl_cl{k}"
                            kb.act(c_[:], pst[:], AF.Exp, [pk, "gl_nb"], [ck], bias=nb[:, d:d + 1], scale=-1.0)
                            kb.act(c_[:], c_[:], AF.Ln, [ck, "one_t"], [ck], bias=C["one"][:, 0:1])
                            if d == 0:
                                kb.scan("dve", c_[:], a01[:], c_[:], 0.0, ["a01", ck], [ck])
                                last = c_[:].rearrange("p (c j) -> p c j", j=128)[:, :, 127]
                            else:
                                kb.scan("dve", c_[:, ::-1], a01[:], c_[:, ::-1], 0.0, ["a01", ck], [ck])
                                last = c_[:].rearrange("p (c j) -> p c j", j=128)[:, :, 0]
                            kb.act(e1[k][:], c_[:], AF.Exp, [ck], [f"gl_e1{k}"], scale=-1.0 / 16)
                            kb.act(e2[k][:], c_[:], AF.Exp, [ck], [f"gl_e2{k}"], scale=1.0 / 16)
                            kb.act(decay[:, 4 * b:4 * b + 4], last, AF.Exp, [ck], ["gl_decay"], scale=-1.0 / 16)
                            kb.stt("dve", qin[:, t0:t0 + 512], qb[k][:], 0.125, e1[k][:], ALU.mult, ALU.mult,
                                   [f"gl_qb{k}", f"gl_e1{k}"], ["gl_qin"])
                            for h in range(2):
                                kb.ts("pool", qm[h][:, t0:t0 + 512], qin[:, t0:t0 + 512], C["blk_mask"][:, 64 * h:64 * h + 1], None,
                                      ALU.mult, None, ["gl_qin", "blk_mask"], [f"gl_qm{h}"])
                            kb.tt("pool", kin[:, t0:t0 + 512], kbf[k][:], e2[k][:], ALU.mult, [f"gl_kb{k}", f"gl_e2{k}"], ["gl_kin"])
                            kb.tt("pool", kob[k][:].rearrange("p (c j) -> p c j", j=128),
                                  kin[:, t0:t0 + 512].rearrange("p (c j) -> p c j", j=128),
                                  decay[:, 4 * b:4 * b + 4].unsqueeze(2).to_broadcast([128, 4, 128]), ALU.mult,
                                  ["gl_kin", "gl_decay"], [f"gl_kob{k}"])
                            pst, pk = kb.ps()
                            pb = pst[:].bitcast(BF16)
                            for j in range(4):
                                kb.tr(pb[:, j * 128:(j + 1) * 128], kob[k][:, j * 128:(j + 1) * 128], ident[:], [f"gl_kob{k}", "ident"], [pk])
                            kb.evac(kotok[:, 4 * b:4 * b + 4, :], pb[:, 0:512].rearrange("p (c j) -> p c j", j=128), [pk], ["gl_kotok"])
                    if os.environ.get("GLA_CUT") == "prep":
                        continue
                    with kb.scope():
                        att = kb.ring("gl_att", 2, [128, 2, 128], BF16)
                        mask = C["mask_f"] if d == 0 else C["mask_b"]
                        mkey = "mask_f" if d == 0 else "mask_b"
                        chunks = list(range(0, min(66, tmax // 128))) if d == 0 else list(range(67, 1, -1))
                        need = lambda c_: c_ * 128 < tmax
                        def emit_att(ci):
                            c = chunks[ci]
                            k = ci % 2
                            tok = slice(c * 128, (c + 1) * 128)
                            pa, pka = kb.ps()
                            for h in range(2):
                                kb.mm(pa[:, h * 128:(h + 1) * 128], kin[:, tok], qm[h][:, tok], True, True, ["gl_kin", f"gl_qm{h}"], [pka])
                            kb.tt("dve", att[k][:], pa[:, 0:256].rearrange("p (h i) -> p h i", h=2),
                                  mask[:].unsqueeze(1).to_broadcast([128, 2, 128]), ALU.mult, [pka, mkey], [f"gl_att{k}"])

                        if need(chunks[0]):
                            emit_att(0)
                        for ci, c in enumerate(chunks):
                            k = ci % 2
                            tok = slice(c * 128, (c + 1) * 128)
                            if ci + 1 < len(chunks) and need(chunks[ci + 1]):
                                emit_att(ci + 1)
                            pS, pkS = kb.ps()
                            kb.mm(pS[:, 0:128], kotok[:, c, :], vt[0][:, c, :], True, False, ["gl_kotok", "gl_vtok0"], [pkS])
                            kb.mm(pS[:, 0:128], kotok[:, c, :], vt[1][:, c, :], False, True, ["gl_kotok", "gl_vtok1"], [pkS])
                            if need(c):
                                po, pko = kb.ps()
                                kb.mm(po[:, 0:128], vt[0][:, c, :], att[k][:, 0, :], True, False, ["gl_vtok0", f"gl_att{k}"], [pko])
                                kb.mm(po[:, 0:128], vt[1][:, c, :], att[k][:, 1, :], False, False, ["gl_vtok1", f"gl_att{k}"], [pko])
                                kb.mm(po[:, 0:128], Sbd[:], qin[:, tok], False, True, ["gl_Sb", "gl_qin"], [pko])
                                if d == 0:
                                    kb.cp("act", oacc[:, tok], po[:, 0:128], [pko], ["gl_oacc"])
                                else:
                                    kb.tt("dve", oacc[:, tok], oacc[:, tok], po[:, 0:128], ALU.add, [pko, "gl_oacc"], ["gl_oacc"])
                            kb.stt("dve", Sf[:], Sf[:], decay[:, c:c + 1], pS[:, 0:128], ALU.mult, ALU.add,
                                   ["gl_S", "gl_decay", pkS], ["gl_S"])
                            kb.tt("pool", Sbd[:], Sf[:], C["blk_mask"][:], ALU.mult, ["gl_S", "blk_mask"], ["gl_Sb"])
            kb.tt("dve", oacc[:, 0:LAT0], oacc[:, 0:LAT0], oacc[:, LAT1:S], ALU.add, ["gl_oacc"], ["gl_oacc"])
            kb.cp("dve", oacc[:, LAT1:S], oacc[:, 0:LAT0], ["gl_oacc"], ["gl_oacc"])
            with kb.scope():
                sq = kb.ring("gl_sq", 2, [128, 512], F32)
                og = kb.ring("gl_og", 2, [128, 512], F32)
                ob = kb.ring("gl_ob", 2, [128, 512], BF16)
                for b in range(min(NBLK, tmax // 512)):
                    k = b % 2
                    t0 = b * 512
                    kb.dma(og[k][:], pT.ap()[768 + 128 * hp:768 + 128 * (hp + 1), t0:t0 + 512], r=["pT"], w=[f"gl_og{k}"], q="sp")
                    kb.act(sq[k][:], oacc[:, t0:t0 + 512], AF.Square, ["gl_oacc"], [f"gl_sq{k}"])
                    pst, pk = kb.ps()
                    kb.mm(pst[:], C["blk_ones"][:], sq[k][:], True, True, ["blk_ones", f"gl_sq{k}"], [pk])
                    kb.act(sq[k][:], pst[:], AF.Sqrt, [pk, "eps_t"], [f"gl_sq{k}"], bias=C["eps"][:, 0:1])
                    kb.recip(sq[k][:], sq[k][:], [f"gl_sq{k}"], [f"gl_sq{k}"])
                    kb.act(og[k][:], og[k][:], AF.Silu, [f"gl_og{k}"], [f"gl_og{k}"])
                    kb.tt("pool", sq[k][:], sq[k][:], oacc[:, t0:t0 + 512], ALU.mult, [f"gl_sq{k}", "gl_oacc"], [f"gl_sq{k}"])
                    kb.tt("dve", ob[k][:], sq[k][:], og[k][:], ALU.mult, [f"gl_sq{k}", f"gl_og{k}"], [f"gl_ob{k}"])
                    kb.dma(brT.ap()[128 * hp:128 * (hp + 1), t0:t0 + 512], ob[k][:], r=[f"gl_ob{k}"], w=["brT"])


PI = math.pi
NCS = 66
NS5 = NCS * 128


def stage_s5(kb, A, l, pT, brT, C, tmax=S):
    gsT = kb.dram(f"gsT{l}", [256, S], BF16)
    for ct in range(2):
        with kb.scope():
            yacc = kb.sb("s5_y", [128, S], F32)
            ub = kb.sb("s5_u", [128, S], BF16)
            dsk = kb.sb("s5_d", [128, 1], F32)
            jt = kb.sb("s5_jt", [128, 128], F32)
            kb.dma(dsk[:], A["s5_dskip"][l, ct], w=["s5_d"], q="sp")
            kb.dma(jt[:], A["jtab"][:, :], w=["s5_jt"], q="sp")
            with kb.scope():
                uf = kb.ring("s5_uf", 2, [128, 2176], F32)
                for q4 in range(4):
                    k = q4 % 2
                    sl = slice(2176 * q4, 2176 * (q4 + 1))
                    kb.dma(uf[k][:], pT.ap()[1056 + 128 * ct:1056 + 128 * (ct + 1), sl], r=["pT"], w=[f"s5_uf{k}"], q="sp")
                    kb.cp("act", ub[:, sl], uf[k][:], [f"s5_uf{k}"], ["s5_u"])
                    kb.ts("dve", yacc[:, sl], uf[k][:], dsk[:, 0:1], None, ALU.mult, None, [f"s5_uf{k}", "s5_d"], ["s5_y"])
            kb.memset("dve", yacc[:, LAT1:S], 0.0, ["s5_y"])
            for tl in range(4):
                for d in range(2):
                    s5_tile_dir(kb, A, l, ct, tl, d, yacc, ub, jt, C, tmax)
            kb.tt("dve", yacc[:, 0:LAT0], yacc[:, 0:LAT0], yacc[:, LAT1:S], ALU.add, ["s5_y"], ["s5_y"])
            kb.cp("dve", yacc[:, LAT1:S], yacc[:, 0:LAT0], ["s5_y"], ["s5_y"])
            with kb.scope():
                tmp = kb.ring("s5_gt", 2, [128, 512], F32)
                gb = kb.ring("s5_gb", 2, [128, 512], BF16)
                for b in range(min(NBLK, tmax // 512)):
                    k = b % 2
                    t0 = b * 512
                    gelu_tanh(kb, gb[k][:], yacc[:, t0:t0 + 512], tmp[k][:], ["s5_y"], [f"s5_gb{k}"], f"s5_gt{k}")
                    kb.dma(gsT.ap()[128 * ct:128 * (ct + 1), t0:t0 + 512], gb[k][:], r=[f"s5_gb{k}"], w=["gsT"])
    with kb.scope():
        wg = kb.sb("s5_wg", [128, 2, 256], BF16)
        bg = kb.sb("s5_bg", [128, 2], F32)
        kb.dma(wg[:], A["s5_w_glu"][l].rearrange("(kt p) c -> p kt c", p=128), w=["s5_wg"], q="pool")
        kb.dma(bg[:], A["s5_bglu"][l], w=["s5_bg"], q="sp")
        gin = kb.ring("s5_gin", 2, [128, 2, 512], BF16)
        sg = kb.ring("s5_sg", 2, [128, 512], F32)
        ob = kb.ring("s5_ob", 2, [128, 512], BF16)
        it = 0
        for b in range(min(NBLK, tmax // 512)):
            k = b % 2
            t0 = b * 512
            kb.dma(gin[k][:], gsT.ap()[:, t0:t0 + 512].rearrange("(kt p) t -> p kt t", p=128), r=["gsT"], w=[f"s5_gin{k}"], q="sp")
            for mt in range(2):
                kk = it % 2
                it += 1
                pst, pk = kb.ps()
                for kt in range(2):
                    kb.mm(pst[:], wg[:, kt, 128 * mt:128 * (mt + 1)], gin[k][:, kt, :], kt == 0, kt == 1, ["s5_wg", f"s5_gin{k}"], [pk])
                kb.act(sg[kk][:], pst[:], AF.Sigmoid, [pk, "s5_bg"], [f"s5_sg{kk}"], bias=bg[:, mt:mt + 1])
                kb.tt("dve", ob[kk][:], sg[kk][:], gin[k][:, mt, :], ALU.mult, [f"s5_sg{kk}", f"s5_gin{k}"], [f"s5_ob{kk}"])
                kb.dma(brT.ap()[256 + 128 * mt:256 + 128 * (mt + 1), t0:t0 + 512], ob[kk][:], r=[f"s5_ob{kk}"], w=["brT"])


def s5_tile_dir(kb, A, l, ct, tl, d, yacc, ub, jt, C, tmax=S):
    o0 = 0 if d == 0 else LAT0
    rng_list = [r_ for r_ in FWD_R if r_[0] < tmax] if d == 0 else BWD_R
    out_list = [r_ for r_ in rng_list if r_[0] < tmax]
    NCS = (sum(n_ for (_, n_) in rng_list)) // 128
    NS5 = NCS * 128
    first, last = (0, 127) if d == 0 else (127, 0)
    with kb.scope():
        prm = kb.sb("s5_prm", [128, 3], F32)
        Bz = kb.sb("s5_Bz", [128, 2, 128], BF16)
        Cz = kb.sb("s5_Cz", [128, 2, 128], F32)
        Wz = kb.sb("s5_Wz", [128, 2, 128], BF16)
        kb.dma(prm[:], A["s5_prm"][l, d, ct, tl], w=["s5_prm"], q="sp")
        kb.dma(Bz[:], A["s5_Bz"][l, d, ct, tl], w=["s5_Bz"], q="pool")
        kb.dma(Cz[:], A["s5_Cz"][l, d, ct, tl], w=["s5_Cz"], q="sp")
        sc = kb.sb("s5_sc", [128, 24], F32)
        K_ = "s5_sc"
        col = lambda i: sc[:, i:i + 1]
        kb.act(col(0), prm[:, 2:3], AF.Exp, ["s5_prm"], [K_])
        kb.memset("dve", col(20), -1e-4, [K_])
        kb.act(col(1), prm[:, 0:1], AF.Relu, ["s5_prm", K_], [K_], bias=col(20), scale=-1.0)
        kb.ts("dve", col(1), col(1), -1.0, -1e-4, ALU.mult, ALU.add, [K_], [K_])
        kb.tt("dve", col(2), col(1), col(0), ALU.mult, [K_], [K_])
        kb.tt("dve", col(3), prm[:, 1:2], col(0), ALU.mult, ["s5_prm", K_], [K_])
        ct_ = kb.sb("s5_c", [128, 128], F32)
        st_ = kb.sb("s5_s", [128, 128], F32)
        rp = kb.sb("s5_rp", [128, 128], F32)
        ph = kb.sb("s5_ph", [128, 128], F32)
        kb.ts("dve", ph[:], jt[:], col(3), None, ALU.mult, None, ["s5_jt", K_], ["s5_ph"])
        ti32 = kb.sb("s5_ti", [128, 128], I32)
        for (t_, k_, off) in ((st_, "s5_s", 0.0), (ct_, "s5_c", 0.5 * PI)):
            kb.ts("dve", t_[:], ph[:], off, 1.0 / (2 * PI), ALU.add, ALU.mult, ["s5_ph"], [k_])
            kb.cp("dve", ti32[:], t_[:], [k_], ["s5_ti"])
            kb.cp("dve", t_[:], ti32[:], ["s5_ti"], [k_])
            if off:
                kb.stt("dve", t_[:], t_[:], -2 * PI, ph[:], ALU.mult, ALU.add, [k_, "s5_ph"], [k_])
                kb.ts("dve", t_[:], t_[:], off, None, ALU.add, None, [k_], [k_])
            else:
                kb.stt("dve", t_[:], t_[:], -2 * PI, ph[:], ALU.mult, ALU.add, [k_, "s5_ph"], [k_])
            kb.act(t_[:], t_[:], AF.Sin, [k_], [k_])
        kb.act(rp[:], jt[:], AF.Exp, ["s5_jt", K_], ["s5_rp"], scale=col(2))
        kb.tt("dve", col(4), rp[:, 0:1], ct_[:, 0:1], ALU.mult, ["s5_rp", "s5_c", K_], [K_])
        kb.ts("dve", col(4), col(4), -1.0, None, ALU.add, None, [K_], [K_])
        kb.tt("dve", col(5), rp[:, 0:1], st_[:, 0:1], ALU.mult, ["s5_rp", "s5_s", K_], [K_])
        kb.tt("dve", col(6), col(1), col(1), ALU.mult, [K_], [K_])
        kb.stt("dve", col(6), prm[:, 1:2], prm[:, 1:2], col(6), ALU.mult, ALU.add, ["s5_prm", K_], [K_])
        kb.recip(col(6), col(6), [K_], [K_])
        kb.tt("dve", col(9), col(5), prm[:, 1:2], ALU.mult, [K_, "s5_prm"], [K_])
        kb.stt("dve", col(7), col(4), col(1), col(9), ALU.mult, ALU.add, [K_], [K_])
        kb.tt("dve", col(7), col(7), col(6), ALU.mult, [K_], [K_])
        kb.tt("dve", col(9), col(4), prm[:, 1:2], ALU.mult, [K_, "s5_prm"], [K_])
        kb.stt("dve", col(8), col(5), col(1), col(9), ALU.mult, ALU.subtract, [K_], [K_])
        kb.tt("dve", col(8), col(8), col(6), ALU.mult, [K_], [K_])
        kb.ts("dve", col(10), col(8), -1.0, None, ALU.mult, None, [K_], [K_])
        wt = kb.sb("s5_wt", [128, 128], F32)
        kb.ts("dve", wt[:], Cz[:, 1, :], col(10), None, ALU.mult, None, ["s5_Cz", K_], ["s5_wt"])
        kb.stt("dve", Wz[:, 0, :], Cz[:, 0, :], col(7), wt[:], ALU.mult, ALU.add, ["s5_Cz", K_, "s5_wt"], ["s5_Wz"])
        kb.ts("dve", wt[:], Cz[:, 0, :], col(10), None, ALU.mult, None, ["s5_Cz", K_, "s5_Wz"], ["s5_wt"])
        kb.ts("dve", col(11), col(7), -1.0, None, ALU.mult, None, [K_], [K_])
        kb.stt("dve", Wz[:, 1, :], Cz[:, 1, :], col(11), wt[:], ALU.mult, ALU.add, ["s5_Cz", K_, "s5_wt"], ["s5_Wz"])
        kb.cp("dve", col(12), ct_[:, 127:128], ["s5_c", K_], [K_])
        kb.cp("dve", col(13), st_[:, 127:128], ["s5_s", K_], [K_])
        kb.ts("dve", col(14), col(13), -1.0, None, ALU.mult, None, [K_], [K_])
        if d == 1:
            for (t_, k_) in ((ct_, "s5_c"), (st_, "s5_s"), (rp, "s5_rp")):
                kb.cp("dve", ph[:], t_[:, ::-1], [k_, "s5_ph"], ["s5_ph"])
                kb.cp("dve", t_[:], ph[:], ["s5_ph"], [k_])
        atab = kb.sb("s5_atab", [128, NS5], F32)
        r1c = rp[:, 0:1] if d == 0 else rp[:, 127:128]
        qw = NS5 // 4
        for q4 in range(4):
            kb.act(atab[:, qw * q4:qw * (q4 + 1)], C["one"][:, 0:1].to_broadcast([128, qw]), AF.Identity,
                   ["one_t", "s5_rp"], ["s5_atab"], scale=r1c)
        kb.memset("dve", atab[:].rearrange("p (c j) -> p c j", j=128)[:, :, first:first + 1], 0.0, ["s5_atab"])
        wre = kb.sb("s5_wre", [128, NS5], F32)
        wim = kb.sb("s5_wim", [128, NS5], F32)
        c3 = lambda n: ct_[:].unsqueeze(1).to_broadcast([128, n // 128, 128])
        s3 = lambda n: st_[:].unsqueeze(1).to_broadcast([128, n // 128, 128])
        v3 = lambda ap: ap.rearrange("p (c j) -> p c j", j=128)
        tmps = kb.ring("s5_t", 4, [128, 512], F32)
        ti = [0]

        def tmp():
            i = ti[0] % 4
            ti[0] += 1
            return tmps[i], f"s5_t{i}"
        bu_re = kb.ring("s5_bur", 2, [128, 512], F32)
        bu_im = kb.ring("s5_bui", 2, [128, 512], F32)
        bu_i = [0]
        for (t0, n) in rng_list:
            w0 = t0 - o0
            p1, k1 = kb.ps()
            kb.mm(p1[:, 0:n], Bz[:, 0, :], ub[:, t0:t0 + n], True, True, ["s5_Bz", "s5_u"], [k1])
            p2, k2 = kb.ps()
            kb.mm(p2[:, 0:n], Bz[:, 1, :], ub[:, t0:t0 + n], True, True, ["s5_Bz", "s5_u"], [k2])
            bi_ = bu_i[0] % 2
            bu_i[0] += 1
            b1, b1k = bu_re[bi_], f"s5_bur{bi_}"
            b2, b2k = bu_im[bi_], f"s5_bui{bi_}"
            kb.cp("act", b1[:, 0:n], p1[:, 0:n], [k1], [b1k])
            kb.cp("act", b2[:, 0:n], p2[:, 0:n], [k2], [b2k])
            ta, ka = tmp()
            tb, kbk = tmp()
            kb.tt("dve", v3(ta[:, 0:n]), v3(b1[:, 0:n]), c3(n), ALU.mult, [b1k, "s5_c"], [ka])
            kb.tt("dve", v3(tb[:, 0:n]), v3(b2[:, 0:n]), s3(n), ALU.mult, [b2k, "s5_s"], [kbk])
            kb.tt("pool", wre[:, w0:w0 + n], ta[:, 0:n], tb[:, 0:n], ALU.add, [ka, kbk], ["s5_wre"])
            ta, ka = tmp()
            tb, kbk = tmp()
            kb.tt("dve", v3(ta[:, 0:n]), v3(b2[:, 0:n]), c3(n), ALU.mult, [b2k, "s5_c"], [ka])
            kb.tt("dve", v3(tb[:, 0:n]), v3(b1[:, 0:n]), s3(n), ALU.mult, [b1k, "s5_s"], [kbk])
            kb.tt("pool", wim[:, w0:w0 + n], ta[:, 0:n], tb[:, 0:n], ALU.subtract, [ka, kbk], ["s5_wim"])
        if d == 0:
            kb.scan("dve", wre[:], atab[:], wre[:], 0.0, ["s5_atab", "s5_wre"], ["s5_wre"])
            kb.scan("dve", wim[:], atab[:], wim[:], 0.0, ["s5_atab", "s5_wim"], ["s5_wim"])
        else:
            kb.scan("dve", wre[:, ::-1], atab[:, ::-1], wre[:, ::-1], 0.0, ["s5_atab", "s5_wre"], ["s5_wre"])
            kb.scan("dve", wim[:, ::-1], atab[:, ::-1], wim[:, ::-1], 0.0, ["s5_atab", "s5_wim"], ["s5_wim"])
        Ha = kb.sb("s5_Ha", [128, 2, NCS], F32)
        Hb = kb.sb("s5_Hb", [128, 2, NCS], F32)
        et = kb.sb("s5_et", [128, NCS], F32)
        ger = v3(wre[:])[:, :, last]
        gei = v3(wim[:])[:, :, last]
        kb.ts("dve", et[:], gei, col(14), None, ALU.mult, None, ["s5_wim", K_], ["s5_et"])
        kb.stt("dve", Ha[:, 0, :], ger, col(12), et[:], ALU.mult, ALU.add, ["s5_wre", K_, "s5_et"], ["s5_Ha"])
        kb.ts("dve", et[:], ger, col(13), None, ALU.mult, None, ["s5_wre", K_, "s5_Ha"], ["s5_et"])
        kb.stt("dve", Ha[:, 1, :], gei, col(12), et[:], ALU.mult, ALU.add, ["s5_wim", K_, "s5_et"], ["s5_Ha"])
        r128 = rp[:, 127:128] if d == 0 else rp[:, 0:1]
        kb.tt("dve", col(15), r128, col(12), ALU.mult, ["s5_rp", K_], [K_])
        kb.tt("dve", col(16), r128, col(13), ALU.mult, ["s5_rp", K_], [K_])
        cur, ck, nxt, nk = Ha, "s5_Ha", Hb, "s5_Hb"
        sft = 1
        while sft < NCS:
            kb.ts("dve", col(17), col(16), -1.0, None, ALU.mult, None, [K_], [K_])
            m = NCS - sft
            if d == 0:
                dst, src, keep = slice(sft, NCS), slice(0, m), slice(0, sft)
            else:
                dst, src, keep = slice(0, m), slice(sft, NCS), slice(m, NCS)
            kb.cp("dve", nxt[:, :, keep], cur[:, :, keep], [ck], [nk])
            kb.stt("dve", nxt[:, 0, dst], cur[:, 0, src], col(15), cur[:, 0, dst], ALU.mult, ALU.add, [ck, K_, nk], [nk])
            kb.stt("dve", nxt[:, 0, dst], cur[:, 1, src], col(17), nxt[:, 0, dst], ALU.mult, ALU.add, [ck, K_, nk], [nk])
            kb.stt("dve", nxt[:, 1, dst], cur[:, 1, src], col(15), cur[:, 1, dst], ALU.mult, ALU.add, [ck, K_, nk], [nk])
            kb.stt("dve", nxt[:, 1, dst], cur[:, 0, src], col(16), nxt[:, 1, dst], ALU.mult, ALU.add, [ck, K_, nk], [nk])
            kb.tt("dve", col(18), col(15), col(15), ALU.mult, [K_], [K_])
            kb.stt("dve", col(18), col(16), col(17), col(18), ALU.mult, ALU.add, [K_], [K_])
            kb.stt("dve", col(19), col(15), 2.0, col(16), ALU.mult, ALU.mult, [K_], [K_])
            kb.cp("dve", col(15), col(18), [K_], [K_])
            kb.cp("dve", col(16), col(19), [K_], [K_])
            cur, ck, nxt, nk = nxt, nk, cur, ck
            sft *= 2
        Hp, hpk = nxt, nk
        if d == 0:
            kb.memset("dve", Hp[:, :, 0:1], 0.0, [hpk])
            kb.cp("dve", Hp[:, :, 1:NCS], cur[:, :, 0:NCS - 1], [ck, hpk], [hpk])
        else:
            kb.memset("dve", Hp[:, :, NCS - 1:NCS], 0.0, [hpk])
            kb.cp("dve", Hp[:, :, 0:NCS - 1], cur[:, :, 1:NCS], [ck, hpk], [hpk])
        hb_re = kb.ring("s5_hre", 2, [128, 512], BF16)
        hb_im = kb.ring("s5_him", 2, [128, 512], BF16)
        for bi, (t0, n) in enumerate(out_list):
            w0 = t0 - o0
            c0, nc_ = w0 // 128, n // 128
            rp3 = rp[:].unsqueeze(1).to_broadcast([128, nc_, 128])
            for (wbuf, wk, comp) in ((wre, "s5_wre", 0), (wim, "s5_wim", 1)):
                ta, ka = tmp()
                kb.tt("dve", v3(ta[:, 0:n]), rp3, Hp[:, comp, c0:c0 + nc_].unsqueeze(2).to_broadcast([128, nc_, 128]),
                      ALU.mult, ["s5_rp", hpk], [ka])
                kb.tt("pool", wbuf[:, w0:w0 + n], wbuf[:, w0:w0 + n], ta[:, 0:n], ALU.add, [wk, ka], [wk])
            k = bi % 2
            gr, gi = v3(wre[:, w0:w0 + n]), v3(wim[:, w0:w0 + n])
            ta, ka = tmp()
            tb, kbk = tmp()
            kb.tt("dve", v3(ta[:, 0:n]), gr, c3(n), ALU.mult, ["s5_wre", "s5_c"], [ka])
            kb.tt("dve", v3(tb[:, 0:n]), gi, s3(n), ALU.mult, ["s5_wim", "s5_s"], [kbk])
            kb.tt("pool", hb_re[k][:, 0:n], ta[:, 0:n], tb[:, 0:n], ALU.subtract, [ka, kbk], [f"s5_hre{k}"])
            ta, ka = tmp()
            tb, kbk = tmp()
            kb.tt("dve", v3(ta[:, 0:n]), gr, s3(n), ALU.mult, ["s5_wre", "s5_s"], [ka])
            kb.tt("dve", v3(tb[:, 0:n]), gi, c3(n), ALU.mult, ["s5_wim", "s5_c"], [kbk])
            kb.tt("pool", hb_im[k][:, 0:n], ta[:, 0:n], tb[:, 0:n], ALU.add, [ka, kbk], [f"s5_him{k}"])
            py, ky = kb.ps()
            kb.mm(py[:, 0:n], Wz[:, 0, :], hb_re[k][:, 0:n], True, False, ["s5_Wz", f"s5_hre{k}"], [ky])
            kb.mm(py[:, 0:n], Wz[:, 1, :], hb_im[k][:, 0:n], False, True, ["s5_Wz", f"s5_him{k}"], [ky])
            kb.tt("dve", yacc[:, t0:t0 + n], yacc[:, t0:t0 + n], py[:, 0:n], ALU.add, ["s5_y", ky], ["s5_y"])


FROW = NLAT + 128


def sin_rr(kb, t, k, ti32, tik, shape_ok=True):
    kb.ts("dve", ti32, t, 1.0 / (2 * PI), None, ALU.mult, None, [k], [tik])
    return ti32


def hy_filters(kb, A, l, n_tok, zT_ap, t01_ap, frow_t, brow_t, rowlen):
    nb = max(1, n_tok // 512)
    bw = min(512, n_tok)
    with kb.scope():
        w1 = kb.sb("hf_w1", [33, 64], F32)
        w2 = kb.sb("hf_w2", [64, 64], F32)
        w3 = kb.sb("hf_w3", [64, 512], F32)
        vec = kb.sb("hf_vec", [64, 5], F32)
        ndl = kb.sb("hf_ndl", [128, 4], F32)
        hyb = kb.sb("hf_eps", [128, 1], F32)
        kb.dma(w1[:], A["hy_w1"][l], w=["hf_w1"], q="sp")
        kb.dma(w2[:], A["hy_w2"][l], w=["hf_w2"], q="sp")
        kb.dma(w3[:], A["hy_w3"][l], w=["hf_w3"], q="sp")
        kb.dma(vec[:, 0:3], A["hy_vec"][l], w=["hf_vec"], q="sp")
        kb.dma(ndl[:], A["hy_ndelta"][:, :], w=["hf_ndl"], q="sp")
        kb.tt("dve", vec[:, 3:4], vec[:, 0:1], vec[:, 2:3], ALU.mult, ["hf_vec"], ["hf_vec"])
        kb.tt("dve", vec[:, 4:5], vec[:, 1:2], vec[:, 2:3], ALU.mult, ["hf_vec"], ["hf_vec"])
        h2 = kb.sb("hf_h2", [64, n_tok], F32)
        zb = kb.ring("hf_z", 2, [33, bw], F32)
        h1 = kb.ring("hf_h1", 2, [64, bw], F32)
        tf = kb.ring("hf_tf", 2, [64, bw], F32)
        ti = kb.ring("hf_ti", 2, [64, bw], I32)

        def sin_inplace(t, k, kk):
            kb.ts("dve", tf[kk][:], t, 1.0 / (2 * PI), None, ALU.mult, None, [k], [f"hf_tf{kk}"])
            kb.cp("dve", ti[kk][:], tf[kk][:], [f"hf_tf{kk}"], [f"hf_ti{kk}"])
            kb.cp("dve", tf[kk][:], ti[kk][:], [f"hf_ti{kk}"], [f"hf_tf{kk}"])
            kb.stt("dve", t, tf[kk][:], -2 * PI, t, ALU.mult, ALU.add, [f"hf_tf{kk}", k], [k])
            kb.act(t, t, AF.Sin, [k], [k])

        for b in range(nb):
            k = b % 2
            sl = slice(b * bw, (b + 1) * bw)
            kb.dma(zb[k][:], zT_ap[:, sl], w=[f"hf_z{k}"], q="sp")
            p1, k1 = kb.ps()
            kb.mm(p1[0:64, 0:bw], w1[:], zb[k][:], True, True, ["hf_w1", f"hf_z{k}"], [k1])
            kb.act(h1[k][:], p1[0:64, 0:bw], AF.Identity, [k1, "hf_vec"], [f"hf_h1{k}"], bias=vec[:, 3:4], scale=vec[:, 2:3])
            sin_inplace(h1[k][:], f"hf_h1{k}", k)
            p2, k2 = kb.ps()
            kb.mm(p2[0:64, 0:bw], w2[:], h1[k][:], True, True, ["hf_w2", f"hf_h1{k}"], [k2])
            kb.act(h2[:, sl], p2[0:64, 0:bw], AF.Identity, [k2, "hf_vec"], ["hf_h2"], bias=vec[:, 4:5], scale=vec[:, 2:3])
            sin_inplace(h2[:, sl], "hf_h2", k)
        filt = kb.sb("hf_filt", [128, n_tok], F32)
        row = kb.sb("hf_row", [128, rowlen], BF16)
        t01 = kb.ring("hf_t01", 2, [128, bw], F32)
        ab = kb.ring("hf_ab", 2, [128, bw], F32)
        sums = kb.sb("hf_sums", [128, nb + 2], F32)
        for ft in range(4):
            for b in range(nb):
                k = b % 2
                sl = slice(b * bw, (b + 1) * bw)
                kb.dma(t01[k][:], t01_ap[:, sl], w=[f"hf_t01{k}"], q="sp")
                kb.act(t01[k][:], t01[k][:], AF.Exp, [f"hf_t01{k}", "hf_ndl"], [f"hf_t01{k}"], scale=ndl[:, ft:ft + 1])
                p3, k3 = kb.ps()
                kb.mm(p3[:, 0:bw], w3[:, 128 * ft:128 * (ft + 1)], h2[:, sl], True, True, ["hf_w3", "hf_h2"], [k3])
                kb.tt("dve", filt[:, sl], p3[:, 0:bw], t01[k][:], ALU.mult, [k3, f"hf_t01{k}"], ["hf_filt"])
                kb.act(ab[k][:], filt[:, sl], AF.Abs, ["hf_filt"], [f"hf_ab{k}"])
                kb.P.op("dve", lambda e, o=sums[:, b:b + 1], i=ab[k][:]: e.reduce_sum(out=o, in_=i, axis=AX.X),
                        [f"hf_ab{k}"], ["hf_sums"])
            kb.P.op("dve", lambda e, o=sums[:, nb:nb + 1], i=sums[:, 0:nb]: e.reduce_sum(out=o, in_=i, axis=AX.X),
                    ["hf_sums"], ["hf_sums"])
            kb.ts("dve", sums[:, nb + 1:nb + 2], sums[:, nb:nb + 1], EPS, None, ALU.add, None, ["hf_sums"], ["hf_sums"])
            kb.recip(sums[:, nb + 1:nb + 2], sums[:, nb + 1:nb + 2], ["hf_sums"], ["hf_sums"])
            for q in range(0, rowlen, 2080):
                kb.memset("dve", row[:, q:min(rowlen, q + 2080)], 0.0, ["hf_row"])
            for b in range(nb):
                sl = slice(b * bw, (b + 1) * bw)
                if ft < 2:
                    dst = row[:, n_tok - (b + 1) * bw:n_tok - b * bw]
                    kb.ts("dve", dst, filt[:, sl][:, ::-1], sums[:, nb + 1:nb + 2], None, ALU.mult, None,
                          ["hf_filt", "hf_sums"], ["hf_row"])
                else:
                    dst = row[:, 127 + b * bw:127 + (b + 1) * bw]
                    kb.ts("dve", dst, filt[:, sl], sums[:, nb + 1:nb + 2], None, ALU.mult, None,
                          ["hf_filt", "hf_sums"], ["hf_row"])
            tgt = frow_t if ft < 2 else brow_t
            ctl = ft % 2
            kb.dma(tgt.ap()[128 * ctl:128 * (ctl + 1), :], row[:], r=["hf_row"], w=["hy_rows"], q="sp")


def hy_conv(kb, nblk, Zall, zkey, ctl, frow_t, brow_t, rowlen, convT, C, nout=None):
    n_tok = nblk * 128
    nout = nblk if nout is None else nout
    nkf = nout
    f0 = n_tok - 128 * nkf
    with kb.scope():
        G = kb.ring("hc_G", 8, [128, n_tok], BF16)
        Zp = kb.ring("hc_Zp", 3, [128, 3 * nblk - 2], BF16)
        ob = kb.ring("hc_ob", 2, [64, 8, 128], F32)
        for i in range(3):
            kb.memset("dve", Zp[i][:], 0.0, [f"hc_Zp{i}"])
        qs = ("sp", "act", "sp")
        for c in range(128):
            ch = 128 * ctl + c
            gi = (2 * c) % 8
            Gf, gfk = G[gi], f"hc_G{gi}"
            Gb, gbk = G[gi + 1], f"hc_G{gi + 1}"
            kb.dma(Gf[:, f0:n_tok], bass.AP(frow_t, ch * rowlen + f0, [[1, 128], [1, n_tok - f0]]), r=["hy_rows"], w=[gfk], q=qs[(2 * c) % 3])
            kb.dma(Gb[:], bass.AP(brow_t, ch * rowlen, [[1, 128], [1, n_tok]]), r=["hy_rows"], w=[gbk], q=qs[(2 * c + 1) % 3])
            zp, zpk = Zp[c % 3], f"hc_Zp{c % 3}"
            kb.cp("pool", zp[:, nblk - 1:2 * nblk - 1], Zall[:, :, c], [zkey, zpk], [zpk])
            po, pk = kb.ps()
            nm = nkf + nblk
            mi = 0
            for k in range(nkf):
                kb.mm(po[0:nout, 0:128], zp[:, nblk - 1 - k:nblk - 1 - k + nout],
                      Gf[:, n_tok - 128 * (k + 1):n_tok - 128 * k][:, ::-1], mi == 0, mi == nm - 1, [zpk, gfk], [pk])
                mi += 1
            for k in range(nblk):
                kb.mm(po[0:nout, 0:128], zp[:, nblk - 1 + k:nblk - 1 + k + nout],
                      Gb[:, 128 * k:128 * (k + 1)][:, ::-1], mi == 0, mi == nm - 1, [zpk, gbk], [pk])
                mi += 1
            o_, ok_ = ob[(c // 8) % 2], f"hc_ob{(c // 8) % 2}"
            kb.evac(o_[0:nout, c % 8, :], po[0:nout, 0:128], [pk], [ok_])
            if c % 8 == 7:
                c0 = ch - 7
                kb.dma(convT.ap()[c0:c0 + 8, 0:128 * nout].rearrange("c (I i) -> I c i", i=128), o_[0:nout, :, :], r=[ok_], w=["hy_conv"], q="sp")


def stage_hyena(kb, A, l, pT, brT, C, do_ctx, half=False):
    hzT = kb.dram(f"hzT{l}", [256, S], F32)
    hx0T = kb.dram(f"hx0T{l}", [256, S], F32)
    convT = kb.dram(f"hconvT{l}", [256, NLAT], F32)
    convTc = kb.dram(f"hconvTc{l}", [256, NCTX], F32)
    frow = kb.dram(f"hfrow{l}", [256, FROW], BF16)
    brow = kb.dram(f"hbrow{l}", [256, FROW], BF16)
    frowc = kb.dram(f"hfrowc{l}", [256, NCTX + 128], BF16)
    browc = kb.dram(f"hbrowc{l}", [256, NCTX + 128], BF16)
    hy_filters(kb, A, l, NLAT, A["hy_zT"], A["hy_t01"], frow, brow, FROW)
    if do_ctx:
        hy_filters(kb, A, l, NCTX, A["hy_zTc"], A["hy_t01c"], frowc, browc, NCTX + 128)
    for ct in range(2):
        with kb.scope():
            Zall = kb.sb("hy_Zall", [128, 64, 128], BF16)
            Zc = kb.sb("hy_Zc", [128, 2, 128], BF16)
            with kb.scope():
                taps = kb.sb("hy_taps", [128, 3, 3], F32)
                kb.dma(taps[:], A["hy_taps"][l, ct], w=["hy_taps"], q="sp")
                raw = kb.sb("hy_raw", [128, S], F32)
                cv = [kb.sb(f"hy_cv{i}", [128, S], F32) for i in range(3)]
                for wi in range(3):
                    r0 = 1312 + 256 * wi + 128 * ct
                    kb.dma(raw[:], pT.ap()[r0:r0 + 128, :], r=["pT"], w=["hy_raw"], q="sp")
                    for (a0, a1) in ((0, LAT0), (LAT0, LAT1), (LAT1, S)):
                        kb.ts("dve", cv[wi][:, a0:a1], raw[:, a0:a1], taps[:, wi, 1:2], None, ALU.mult, None,
                              ["hy_raw", "hy_taps"], [f"hy_cv{wi}"])
                        for o in (0, 2):
                            off = o - 1
                            lo, hi = max(a0, a0 - off), min(a1, a1 - off)
                            kb.stt("dve", cv[wi][:, lo:hi], raw[:, lo + off:hi + off], taps[:, wi, o:o + 1], cv[wi][:, lo:hi],
                                   ALU.mult, ALU.add, ["hy_raw", "hy_taps", f"hy_cv{wi}"], [f"hy_cv{wi}"])
                kb.tt("dve", cv[2][:], cv[2][:], cv[0][:], ALU.mult, ["hy_cv2", "hy_cv0"], ["hy_cv2"])
                kb.dma(hzT.ap()[128 * ct:128 * (ct + 1), :], cv[2][:], r=["hy_cv2"], w=["hzT"], q="sp")
                kb.dma(hx0T.ap()[128 * ct:128 * (ct + 1), :], cv[1][:], r=["hy_cv1"], w=["hx0T"], q="sp")
                zb16 = kb.sb("hy_zb", [128, S], BF16)
                kb.cp("act", zb16[:], cv[2][:], ["hy_cv2"], ["hy_zb"])
                for blk in range(0, 66):
                    tok = slice(128 * blk, 128 * (blk + 1))
                    if blk % 4 == 0:
                        pst, pk = kb.ps()
                        pb = pst[:].bitcast(BF16)
                    j = blk % 4
                    kb.tr(pb[:, j * 128:(j + 1) * 128], zb16[:, tok], C["ident"][:], ["hy_zb", "ident"], [pk])
                    if blk == 1:
                        kb.evac(Zc[:, 0:2, :], pb[:, 0:256].rearrange("p (c j) -> p c j", j=128), [pk], ["hy_Zc"])
                    if blk >= 2 and (blk % 4 == 3 or blk == 65):
                        b0 = (blk // 4) * 4
                        lo = max(b0, 2)
                        kb.evac(Zall[:, lo - 2:blk - 1, :],
                                pb[:, (lo - b0) * 128:(blk - b0 + 1) * 128].rearrange("p (c j) -> p c j", j=128), [pk], ["hy_Zall"])
            hy_conv(kb, 64, Zall, "hy_Zall", ct, frow, brow, FROW, convT, C, nout=(32 if half else 64))
            if do_ctx:
                hy_conv(kb, 2, Zc, "hy_Zc", ct, frowc, browc, NCTX + 128, convTc, C)
            with kb.scope():
                hb_ = kb.sb("hy_bias", [128, 1], F32)
                kb.dma(hb_[:], A["hy_bias"][l, ct], w=["hy_bias"], q="sp")
                zz = kb.ring("hy_zz", 2, [128, 512], F32)
                x0 = kb.ring("hy_x0", 2, [128, 512], F32)
                cc = kb.ring("hy_cc", 2, [128, 512], F32)
                ob = kb.ring("hy_ob", 2, [128, 512], BF16)
                rows = slice(128 * ct, 128 * (ct + 1))
                it = 0
                for (t0, n, isc) in [(t0, 512, 0) for t0 in range(LAT0, LAT0 + (NOUT if half else NLAT), 512)] + ([(0, 256, 1)] if do_ctx else []):
                    k = it % 2
                    it += 1
                    kb.dma(zz[k][:, 0:n], hzT.ap()[rows, t0:t0 + n], r=["hzT"], w=[f"hy_zz{k}"], q="sp")
                    kb.dma(x0[k][:, 0:n], hx0T.ap()[rows, t0:t0 + n], r=["hx0T"], w=[f"hy_x0{k}"], q="sp")
                    src = convTc.ap()[rows, 0:n] if isc else convT.ap()[rows, t0 - LAT0:t0 - LAT0 + n]
                    kb.dma(cc[k][:, 0:n], src, r=["hy_conv"], w=[f"hy_cc{k}"], q="sp")
                    kb.stt("dve", cc[k][:, 0:n], zz[k][:, 0:n], hb_[:, 0:1], cc[k][:, 0:n], ALU.mult, ALU.add,
                           [f"hy_zz{k}", "hy_bias", f"hy_cc{k}"], [f"hy_cc{k}"])
                    kb.tt("dve", ob[k][:, 0:n], cc[k][:, 0:n], x0[k][:, 0:n], ALU.mult, [f"hy_cc{k}", f"hy_x0{k}"], [f"hy_ob{k}"])
                    kb.dma(brT.ap()[512 + 128 * ct:512 + 128 * (ct + 1), t0:t0 + n], ob[k][:, 0:n], r=[f"hy_ob{k}"], w=["brT"], q="sp")
                    if isc:
                        kb.dma(brT.ap()[512 + 128 * ct:512 + 128 * (ct + 1), LAT1:S], ob[k][:, 0:n], r=[f"hy_ob{k}"], w=["brT"], q="sp")


def rms_rstd(kb, C, sq, sqk, x, xk, rstd, rk):
    kb.act(sq, x, AF.Square, [xk], [sqk])
    pst, pk = kb.ps()
    n = rstd.shape[-1]
    for kt in range(KT):
        kb.mm(pst[:, 0:n], C["ones_f"][:], sq[:, kt, :], kt == 0, kt == KT - 1, ["ones_f", sqk], [pk])
    kb.act(rstd, pst[:, 0:n], AF.Sqrt, [pk, "eps_t"], [rk], bias=C["eps"][:, 0:1])
    kb.recip(rstd, rstd, [rk], [rk])


def stage_f1(kb, A, l, xres_v, brT, sgT, h2T, gT, C, A1, A2, MV, tok_lo, tok_hi, moe):
    with kb.scope():
        wbr = kb.sb("f1_wbr", [128, 8, D], BF16)
        wo = kb.sb("f1_wo", [128, 8, D], BF16)
        for k4 in range(4):
            kb.dma(wbr[:, 2 * k4:2 * k4 + 2, :], A["w_branch"][l, k4].rearrange("(kt p) c -> p kt c", p=128), w=["f1_wbr"], q="pool")
        for kt in range(0, KT, 2):
            kb.dma(wo[:, kt:kt + 2, :], A["w_out"][l].rearrange("(kt p) c -> p kt c", p=128)[:, kt:kt + 2, :], w=["f1_wo"], q="pool")
        if moe:
            rw = kb.sb("f1_rw", [128, KT, 8], F32)
            rb = kb.sb("f1_rb", [128, 8], F32)
            kb.dma(rw[:], A["moe_router"][0].rearrange("(kt p) e -> p kt e", p=128), w=["f1_rw"], q="sp")
            kb.dma(rb[:], A["moe_rb"][:, :], w=["f1_rb"], q="sp")
            lg = kb.ring("f1_lg", 2, [128, 8], F32)
            e1 = kb.ring("f1_e1", 2, [128, 8], F32)
            e2 = kb.ring("f1_e2", 2, [128, 8], F32)
            mm_ = kb.ring("f1_mm", 2, [128, 4], F32)
            gts = kb.sb("f1_gts", [8, 512], F32)
        br = kb.ring("f1_br", 2, [128, 8, 512], BF16)
        sg = kb.ring("f1_sg", 2, [128, 8, 512], BF16)
        ym = kb.sb("f1_ym", [128, 8, 512], F32)
        ymb = kb.sb("f1_ymb", [128, 8, 512], BF16)
        xs = kb.ring("f1_x", 2, [128, 8, 512], F32)
        sq = kb.sb("f1_sq", [128, 8, 512], F32)
        h2b = kb.ring("f1_h2b", 2, [128, 8, 512], BF16)
        rstd = kb.sb("f1_rstd", [128, 512], F32)
        tmp = kb.ring("f1_tmp", 3, [128, 512], F32)
        ti = 0
        sgi = 0
        for bi, t0 in enumerate(range(tok_lo, tok_hi, 512)):
            kk = bi % 2
            brb, brk = br[kk], f"f1_br{kk}"
            x_, xk = xs[kk], f"f1_x{kk}"
            kb.dma(brb[:], brT.ap()[:, t0:t0 + 512].rearrange("(kt p) t -> p kt t", p=128), r=["brT"], w=[brk], q="sp")
            kb.dma(x_[:], xres_v[:, :, t0:t0 + 512], r=["xres"], w=[xk], q="sp")
            for k4 in range(4):
                sgb, sgk = sg[sgi % 2], f"f1_sg{sgi % 2}"
                sgi += 1
                kb.dma(sgb[:], sgT.ap()[1024 * k4:1024 * (k4 + 1), t0:t0 + 512].rearrange("(kt p) t -> p kt t", p=128),
                       r=["sgT"], w=[sgk], q="sp")
                for mt in range(KT):
                    pst, pk = kb.ps()
                    for kt in range(2):
                        kb.mm(pst[:], wbr[:, 2 * k4 + kt, 128 * mt:128 * (mt + 1)], brb[:, 2 * k4 + kt, :], kt == 0, kt == 1,
                              ["f1_wbr", brk], [pk])
                    if k4 == 0:
                        kb.tt("dve", ym[:, mt, :], pst[:], sgb[:, mt, :], ALU.mult, [pk, sgk], ["f1_ym"])
                    else:
                        t_, tk = tmp[ti % 3], f"f1_tmp{ti % 3}"
                        ti += 1
                        kb.tt("dve", t_[:], pst[:], sgb[:, mt, :], ALU.mult, [pk, sgk], [tk])
                        kb.tt("pool", ym[:, mt, :], ym[:, mt, :], t_[:], ALU.add, ["f1_ym", tk], ["f1_ym"])
            kb.cp("act", ymb[:], ym[:], ["f1_ym"], ["f1_ymb"])
            for mt in range(KT):
                pst, pk = kb.ps()
                for kt in range(KT):
                    kb.mm(pst[:], wo[:, kt, 128 * mt:128 * (mt + 1)], ymb[:, kt, :], kt == 0, kt == KT - 1, ["f1_wo", "f1_ymb"], [pk])
                for (s0, n, isc) in segs(t0, 512):
                    o0 = s0 - t0
                    kb.stt("dve", x_[:, mt, o0:o0 + n], pst[:, o0:o0 + n], MV[:, 2, mt, isc:isc + 1], x_[:, mt, o0:o0 + n],
                           ALU.mult, ALU.add, [pk, f"MV{l}", xk], [xk])
            kb.dma(xres_v[:, :, t0:t0 + 512], x_[:], r=[xk], w=["xres"], q="sp")
            rms_rstd(kb, C, sq[:], "f1_sq", x_[:], xk, rstd[:], "f1_rstd")
            kb.tt("dve", sq[:], x_[:], rstd[:].unsqueeze(1).to_broadcast([128, KT, 512]), ALU.mult, [xk, "f1_rstd"], ["f1_sq"])
            hb, hk = h2b[kk], f"f1_h2b{kk}"
            for (s0, n, isc) in segs(t0, 512):
                o0 = s0 - t0
                for kt in range(KT):
                    if moe:
                        kb.ts("pool", sq[:, kt, o0:o0 + n], sq[:, kt, o0:o0 + n], A2[:, kt, isc:isc + 1], MV[:, 3, kt, isc:isc + 1],
                              ALU.mult, ALU.add, ["f1_sq", f"A2{l}", f"MV{l}"], ["f1_sq"])
                    else:
                        kb.ts("pool", hb[:, kt, o0:o0 + n], sq[:, kt, o0:o0 + n], A2[:, kt, isc:isc + 1], MV[:, 3, kt, isc:isc + 1],
                              ALU.mult, ALU.add, ["f1_sq", f"A2{l}", f"MV{l}"], [hk])
            if moe:
                kb.cp("act", hb[:], sq[:], ["f1_sq"], [hk])
                for g4 in range(4):
                    k2 = g4 % 2
                    tk128 = slice(128 * g4, 128 * (g4 + 1))
                    pl, pkl = kb.ps()
                    for kt in range(KT):
                        kb.mm(pl[:, 0:8], sq[:, kt, tk128], rw[:, kt, :], kt == 0, kt == KT - 1, ["f1_sq", "f1_rw"], [pkl])
                    L, Lk = lg[k2], f"f1_lg{k2}"
                    E1, E1k = e1[k2], f"f1_e1{k2}"
                    E2, E2k = e2[k2], f"f1_e2{k2}"
                    M, Mk = mm_[k2], f"f1_mm{k2}"
                    kb.tt("dve", L[:], pl[:, 0:8], rb[:], ALU.add, [pkl, "f1_rb"], [Lk])
                    kb.P.op("dve", lambda e, o=M[:, 0:1], i=L[:]: e.reduce_max(out=o, in_=i, axis=AX.X), [Lk], [Mk])
                    kb.ts("dve", E1[:], L[:], M[:, 0:1], None, ALU.is_equal, None, [Lk, Mk], [E1k])
                    kb.stt("dve", E2[:], E1[:], -1e30, L[:], ALU.mult, ALU.add, [E1k, Lk], [E2k])
                    kb.P.op("dve", lambda e, o=M[:, 1:2], i=E2[:]: e.reduce_max(out=o, in_=i, axis=AX.X), [E2k], [Mk])
                    kb.ts("dve", E2[:], E2[:], M[:, 1:2], None, ALU.is_equal, None, [E2k, Mk], [E2k])
                    kb.ts("dve", M[:, 2:3], M[:, 0:1], -1.0, None, ALU.mult, None, [Mk], [Mk])
                    kb.act(M[:, 3:4], M[:, 1:2], AF.Sigmoid, [Mk], [Mk], bias=M[:, 2:3])
                    kb.ts("dve", M[:, 2:3], M[:, 3:4], -1.0, 1.0, ALU.mult, ALU.add, [Mk], [Mk])
                    kb.ts("dve", E1[:], E1[:], M[:, 2:3], None, ALU.mult, None, [E1k, Mk], [E1k])
                    kb.stt("dve", E1[:], E2[:], M[:, 3:4], E1[:], ALU.mult, ALU.add, [E2k, Mk, E1k], [E1k])
                    pt_, pkt = kb.ps()
                    kb.tr(pt_[0:8, 0:128], E1[:], C["identf"][:], [E1k, "identf"], [pkt])
                    kb.cp("act", gts[:, tk128], pt_[0:8, 0:128], [pkt], ["f1_gts"])
                kb.dma(gT.ap()[:, t0:t0 + 512], gts[:], r=["f1_gts"], w=["gT"], q="sp")
            kb.dma(h2T.ap()[:, t0:t0 + 512].rearrange("(kt p) t -> p kt t", p=128), hb[:], r=[hk], w=["h2T"], q="sp")


def stage_f2(kb, A, l, xres_v, h2T, gT, C, MV, tok_lo, tok_hi, moe):
    if moe:
        n_exp, n_ht, wgu_of, wd_of, d_h = NEXP, D_EXP // 128, (lambda e: A["moe_w_gu"][0, e]), (lambda e: A["moe_w_down"][0, e]), D_EXP
    else:
        n_exp, n_ht, wgu_of, wd_of, d_h = 1, D_FF // 128, (lambda e: A["ffn_w_gu"][0]), (lambda e: A["ffn_w_down"][0]), D_FF
    CH = 4 if n_ht % 4 == 0 else 2
    NW = 3
    sblocks = [(t0, min(2048, tok_hi - t0)) for t0 in range(tok_lo, tok_hi, 2048)]
    with kb.scope():
        h2b = kb.sb("f2_h2", [128, KT, 2048], BF16)
        yacc = kb.sb("f2_y", [128, KT, 2048], F32)
        wgu = kb.ring("f2_wgu", 2, [128, KT, 2, 128 * CH], BF16)
        wd = kb.ring("f2_wd", NW, [128, CH, D], BF16)
        actc = kb.ring("f2_act", 3, [128, CH, 512], BF16)
        sl_ = kb.ring("f2_sl", 2, [128, 512], F32)
        xb = kb.ring("f2_x", 2, [128, 512], F32)
        if moe:
            gts = kb.sb("f2_gts", [8, 2048], F32)
            sel = kb.sb("f2_sel", [8, 8, 128], F32)
            kb.cp("dve", sel[:], C["identf"][0:8, 0:8].unsqueeze(2).to_broadcast([8, 8, 128]), ["identf"], ["f2_sel"])
            Ge = kb.ring("f2_Ge", 2, [128, 2048], BF16)
            mt_ = kb.ring("f2_mt", 2, [128, 512], F32)
        visits = [(e, h0) for e in range(n_exp) for h0 in range(0, n_ht, CH)]
        wcount = [0]

        def load_w(vi):
            e, h0 = visits[vi]
            k = wcount[0] % NW
            kg = wcount[0] % 2
            wcount[0] += 1
            gu_v = wgu_of(e).rearrange("(kt p) c -> p kt c", p=128)
            wd_v = wd_of(e).rearrange("(ht p) c -> p ht c", p=128)
            for kt in range(0, KT, 4):
                kb.dma(wgu[kg][:, kt:kt + 4, 0, :], gu_v[:, kt:kt + 4, 128 * h0:128 * (h0 + CH)], w=[f"f2_wgu{kg}"], q="pool")
                kb.dma(wgu[kg][:, kt:kt + 4, 1, :], gu_v[:, kt:kt + 4, d_h + 128 * h0:d_h + 128 * (h0 + CH)], w=[f"f2_wgu{kg}"], q="pool")
            kb.dma(wd[k][:], wd_v[:, h0:h0 + CH, :], w=[f"f2_wd{k}"], q="pool")
            return (kg, k)

        for (t0, nt) in sblocks:
            nsb = nt // 512
            for kt in range(KT):
                kb.dma(h2b[:, kt, 0:nt], h2T.ap()[128 * kt:128 * (kt + 1), t0:t0 + nt], r=["h2T"], w=["f2_h2"], q="sp")
            if moe:
                kb.dma(gts[:, 0:nt], gT.ap()[:, t0:t0 + nt], r=["gT"], w=["f2_gts"], q="sp")
            wslot = {0: load_w(0)}
            pend = None
            si = 0

            def emit_down(p):
                ac, ack, wd_, wdk, tsl, first = p
                for mt in range(KT):
                    pd, pkd = kb.ps()
                    for hh in range(CH):
                        kb.mm(pd[:], wd_[:, hh, 128 * mt:128 * (mt + 1)], ac[:, hh, :], hh == 0, hh == CH - 1, [wdk, ack], [pkd])
                    if first:
                        kb.cp("act", yacc[:, mt, tsl], pd[:], [pkd], ["f2_y"])
                    else:
                        kb.tt("dve", yacc[:, mt, tsl], yacc[:, mt, tsl], pd[:], ALU.add, [pkd, "f2_y"], ["f2_y"])

            for vi, (e, h0) in enumerate(visits):
                if vi + 1 < len(visits):
                    wslot[vi + 1] = load_w(vi + 1)
                kg, k = wslot[vi]
                wg_, wgk, wd_, wdk = wgu[kg], f"f2_wgu{kg}", wd[k], f"f2_wd{k}"
                if moe and h0 == 0:
                    G_, Gk = Ge[e % 2], f"f2_Ge{e % 2}"
                    for sb_ in range(nsb):
                        pg, pkg = kb.ps()
                        kb.mm(pg[:], sel[:, e, :], gts[:, 512 * sb_:512 * (sb_ + 1)], True, True, ["f2_sel", "f2_gts"], [pkg])
                        kb.cp("act", G_[:, 512 * sb_:512 * (sb_ + 1)], pg[:], [pkg], [Gk])
                for sb_ in range(nsb):
                    tsl = slice(512 * sb_, 512 * (sb_ + 1))
                    ac, ack = actc[si % 3], f"f2_act{si % 3}"
                    si += 1
                    for hh in range(CH):
                        pgt, pkg = kb.ps()
                        for kt in range(KT):
                            kb.mm(pgt[:], wg_[:, kt, 0, 128 * hh:128 * (hh + 1)], h2b[:, kt, tsl], kt == 0, kt == KT - 1, [wgk, "f2_h2"], [pkg])
                        put, pku = kb.ps()
                        for kt in range(KT):
                            kb.mm(put[:], wg_[:, kt, 1, 128 * hh:128 * (hh + 1)], h2b[:, kt, tsl], kt == 0, kt == KT - 1, [wgk, "f2_h2"], [pku])
                        s_, sk = sl_[hh % 2], f"f2_sl{hh % 2}"
                        kb.act(s_[:], pgt[:], AF.Silu, [pkg], [sk])
                        if moe:
                            m_, mk = mt_[hh % 2], f"f2_mt{hh % 2}"
                            kb.tt("dve", m_[:], s_[:], put[:], ALU.mult, [sk, pku], [mk])
                            kb.tt("pool", ac[:, hh, :], m_[:], G_[:, tsl], ALU.mult, [mk, Gk], [ack])
                        else:
                            kb.tt("dve", ac[:, hh, :], s_[:], put[:], ALU.mult, [sk, pku], [ack])
                    if pend is not None:
                        emit_down(pend)
                    pend = (ac, ack, wd_, wdk, tsl, vi == 0)
            emit_down(pend)
            xi = 0
            for sb_ in range(nsb):
                tb = t0 + 512 * sb_
                for mt in range(KT):
                    x_, xk = xb[xi % 2], f"f2_x{xi % 2}"
                    xi += 1
                    dst = xres_v[:, mt, tb:tb + 512]
                    kb.dma(x_[:], dst, r=["xres"], w=[xk], q="sp")
                    for (s0, n, isc) in segs(tb, 512):
                        o0 = s0 - tb
                        kb.stt("dve", x_[:, o0:o0 + n], yacc[:, mt, 512 * sb_ + o0:512 * sb_ + o0 + n], MV[:, 5, mt, isc:isc + 1],
                               x_[:, o0:o0 + n], ALU.mult, ALU.add, ["f2_y", f"MV{l}", xk], [xk])
                    kb.dma(dst, x_[:], r=[xk], w=["xres"], q="sp")


def stage_final(kb, A, xres_v, C):
    with kb.scope():
        fg = kb.sb("fn_g", [128, KT], F32)
        kb.dma(fg[:], A["final_g"][:, :], w=["fn_g"], q="sp")
        xs = kb.ring("fn_x", 2, [128, KT, 512], F32)
        sq = kb.ring("fn_sq", 2, [128, KT, 512], F32)
        rs = kb.ring("fn_rs", 2, [128, 512], F32)
        out_v = A["out"].rearrange("(kt p) t -> p kt t", p=128)
        for bi, t0 in enumerate(range(LAT0, LAT0 + NOUT, 512)):
            k = bi % 2
            kb.dma(xs[k][:], xres_v[:, :, t0:t0 + 512], r=["xres"], w=[f"fn_x{k}"], q="sp")
            rms_rstd(kb, C, sq[k][:], f"fn_sq{k}", xs[k][:], f"fn_x{k}", rs[k][:], f"fn_rs{k}")
            kb.tt("dve", sq[k][:], xs[k][:], rs[k][:].unsqueeze(1).to_broadcast([128, KT, 512]), ALU.mult,
                  [f"fn_x{k}", f"fn_rs{k}"], [f"fn_sq{k}"])
            kb.tt("pool", sq[k][:], sq[k][:], fg[:].unsqueeze(2).to_broadcast([128, KT, 512]), ALU.mult,
                  [f"fn_sq{k}", "fn_g"], [f"fn_sq{k}"])
            kb.dma(out_v[:, :, t0 - LAT0:t0 - LAT0 + 512], sq[k][:], r=[f"fn_sq{k}"], w=["out"], q="sp")


def build(nc, A, stop_after=None, debug_outs=()):
    kb = KB(nc, debug_outs)
    P = kb.P
    final_ops = []
    with kb.st:
        kb.init_psum()
        xres = kb.dram("xres", [D, S], F32)
        pT = kb.dram("pT", [MIXC, S], F32)
        sgT = kb.dram("sgT", [4 * D, S], BF16)
        brT = kb.dram("brT", [D, S], BF16)
        h2T = kb.dram("h2T", [D, S], BF16)
        gT = kb.dram("gT", [8, S], F32)
        xres_v = xres.ap().rearrange("(kt p) t -> p kt t", p=128)

        ones_f = kb.sb("ones_f", [128, 128], F32)
        kb.memset("pool", ones_f[:], 1.0 / D, ["ones_f"])
        eps_t = kb.sb("eps_t", [128, 1], F32)
        kb.memset("pool", eps_t[:], EPS, ["eps_t"])
        C = {}
        identf = kb.sb("identf", [128, 128], F32)
        kb.memset("pool", identf[:], 0.0, ["identf"])
        P.op("pool", lambda e: e.affine_select(out=identf[:], in_=identf[:], pattern=[[-1, 128]], compare_op=ALU.not_equal,
                                               fill=1.0, base=0, channel_multiplier=1), ["identf"], ["identf"])
        C["ident"] = kb.sb("ident", [128, 128], BF16)
        kb.cp("dve", C["ident"][:], identf[:], ["identf"], ["ident"])
        C["identf"] = identf
        for nm, sg in (("mask_f", -1), ("mask_b", 1)):
            mk = kb.sb(nm, [128, 128], F32)
            kb.memset("pool", mk[:], 1.0, [nm])
            P.op("pool", lambda e, mk=mk, sg=sg: e.affine_select(out=mk[:], in_=mk[:], pattern=[[-sg, 128]], compare_op=ALU.is_ge,
                                                                fill=0.0, base=0, channel_multiplier=sg), [nm], [nm])
            C[nm] = mk
        bo = kb.sb("blk_ones", [128, 128], F32)
        kb.memset("pool", bo[:], 0.0, ["blk_ones"])
        kb.memset("pool", bo[0:64, 0:64], 1.0 / 64, ["blk_ones"])
        kb.memset("pool", bo[64:128, 64:128], 1.0 / 64, ["blk_ones"])
        C["blk_ones"] = bo
        bm = kb.sb("blk_mask", [128, 128], F32)
        kb.ts("dve", bm[:], bo[:], 64.0, None, ALU.mult, None, ["blk_ones"], ["blk_mask"])
        C["blk_mask"] = bm
        C["hmask"] = []
        for h in range(2):
            hm = kb.sb(f"hmask{h}", [128, 128], BF16)
            kb.memset("dve", hm[:], 0.0, ["hmask"])
            kb.memset("dve", hm[:, 64 * h:64 * h + 64], 1.0, ["hmask"])
            C["hmask"].append(hm)
        a01 = kb.sb("a01", [128, 512], F32)
        kb.memset("pool", a01[:], 1.0, ["a01"])
        kb.memset("pool", a01[:].rearrange("p (c j) -> p c j", j=128)[:, :, 0:1], 0.0, ["a01"])
        C["a01"] = a01
        C["eps"] = eps_t
        C["ones_f"] = ones_f
        one_t = kb.sb("one_t", [128, 1], F32)
        kb.memset("pool", one_t[:], 1.0, ["one_t"])
        C["one"] = one_t
        c2 = kb.sb("c2", [128, KT, 2], F32)
        sc2 = kb.sb("sc2", [128, KT, 2], F32)
        kb.dma(c2[:], A["c2"][:, :, :], w=["c2"])
        kb.act(sc2[:], c2[:], AF.Silu, ["c2"], ["sc2"])

        xin_v = A["xT"].rearrange("(kt p) t -> p kt t", p=128)
        pos_v = A["posT"].rearrange("(kt p) t -> p kt t", p=128)
        with kb.scope():
            xr = kb.ring("px", 2, [128, KT, 512], F32)
            pr = kb.ring("pp", 2, [128, KT, 512], F32)
            for b in range(NBLK):
                t0 = b * 512
                xs, xk = xr[b % 2], f"px{b % 2}"
                ps_, pk = pr[b % 2], f"pp{b % 2}"
                kb.dma(xs[:], xin_v[:, :, t0:t0 + 512], w=[xk])
                for (s0, n, isc) in segs(t0, 512):
                    if isc:
                        continue
                    kb.dma(ps_[:, :, s0 - t0:s0 - t0 + n], pos_v[:, :, s0 - LAT0:s0 - LAT0 + n], w=[pk])
                    kb.tt("dve", xs[:, :, s0 - t0:s0 - t0 + n], xs[:, :, s0 - t0:s0 - t0 + n],
                          ps_[:, :, s0 - t0:s0 - t0 + n], ALU.add, [xk, pk], [xk])
                kb.dma(xres_v[:, :, t0:t0 + 512], xs[:], r=[xk], w=["xres"])

        A1s = [kb.sb(f"A1_{l}", [128, KT, 2], F32) for l in range(DEPTH)]
        A2s = [kb.sb(f"A2_{l}", [128, KT, 2], F32) for l in range(DEPTH)]
        MVs = [kb.sb(f"MV_{l}", [128, 6, KT, 2], F32) for l in range(DEPTH)]
        for l in range(DEPTH):
            with kb.scope():
                modv = kb.sb(f"modv{l}", [128, 48, 2], F32)
                modb = kb.sb(f"modb{l}", [128, 48], F32)
                g12 = kb.sb(f"g12{l}", [128, 2, KT], F32)
                kb.dma(modb[:], A["mod_b"][l], w=[f"modb{l}"])
                kb.dma(g12[:, 0, :], A["norm1_g"][l], w=[f"g12{l}"])
                kb.dma(g12[:, 1, :], A["norm2_g"][l], w=[f"g12{l}"])
                wm = kb.ring("wmod", 2, [128, KT, 512], F32)
                mw_v = A["mod_w"][l].rearrange("(kt p) c -> p kt c", p=128)
                for j in range(12):
                    wt, wk = wm[j % 2], f"wmod{j % 2}"
                    kb.dma(wt[:], mw_v[:, :, j * 512:(j + 1) * 512], w=[wk])
                    for sub in range(4):
                        ct = j * 4 + sub
                        pst, pk = kb.ps()
                        for kt in range(KT):
                            kb.mm(pst[:, 0:2], wt[:, kt, sub * 128:(sub + 1) * 128], sc2[:, kt, :],
                                  kt == 0, kt == KT - 1, [wk, "sc2"], [pk])
                        kb.ts("dve", modv[:, ct, :], pst[:, 0:2], modb[:, ct:ct + 1], None, ALU.add, None,
                              [pk, f"modb{l}"], [f"modv{l}"])
                A1, A2, MV = A1s[l], A2s[l], MVs[l]
                kb.cp("dve", MV[:].rearrange("p a k s -> p (a k) s"), modv[:], [f"modv{l}"], [f"MV{l}"])
                kb.stt("dve", A1[:], MV[:, 1, :, :], 1.0, g12[:, 0, :].unsqueeze(2).to_broadcast([128, KT, 2]),
                       ALU.add, ALU.mult, [f"MV{l}", f"g12{l}"], [f"A1{l}"])
                kb.stt("dve", A2[:], MV[:, 4, :, :], 1.0, g12[:, 1, :].unsqueeze(2).to_broadcast([128, KT, 2]),
                       ALU.add, ALU.mult, [f"MV{l}", f"g12{l}"], [f"A2{l}"])

            with kb.scope():
                win = kb.sb("win", [128, KT, IN_COLS], BF16)
                wi_v = A["w_in"][l].rearrange("(kt p) c -> p kt c", p=128)
                WQ = 8
                wq_w = IN_COLS // WQ
                for ch in range(WQ):
                    c0, c1 = ch * wq_w, (ch + 1) * wq_w
                    for kt in range(0, KT, 4):
                        kb.dma(win[:, kt:kt + 4, c0:c1], wi_v[:, kt:kt + 4, c0:c1], w=[f"win{ch}"], q="pool")

                def wkeys(c0, w):
                    return [f"win{q}" for q in range(c0 // wq_w, (c0 + w - 1) // wq_w + 1)]
                xr = kb.ring("ax", 2, [128, KT, 512], F32)
                sr = kb.ring("asq", 1, [128, KT, 512], F32)
                hr = kb.ring("ahb", 2, [128, KT, 512], BF16)
                rs = kb.ring("arstd", 2, [128, 512], F32)
                so = kb.ring("aso", 4, [128, 512], F32)
                go = kb.ring("ago", 2, [128, 4, 512], BF16)
                tiles = in_tiles()
                soi = 0
                for b in range(NBLK):
                    t0 = b * 512
                    xs, xk = xr[b % 2], f"ax{b % 2}"
                    sq, sk = sr[0], "asq0"
                    hb, hk = hr[b % 2], f"ahb{b % 2}"
                    rstd, rk = rs[b % 2], f"arstd{b % 2}"
                    kb.dma(xs[:], xres_v[:, :, t0:t0 + 512], r=["xres"], w=[xk], q="sp")
                    kb.act(sq[:], xs[:], AF.Square, [xk], [sk])
                    pst, pk = kb.ps()
                    for kt in range(KT):
                        kb.mm(pst[:], ones_f[:], sq[:, kt, :], kt == 0, kt == KT - 1, ["ones_f", sk], [pk])
                    kb.act(rstd[:], pst[:], AF.Sqrt, [pk, "eps_t"], [rk], bias=eps_t[:, 0:1])
                    kb.recip(rstd[:], rstd[:], [rk], [rk])
                    kb.tt("dve", sq[:], xs[:], rstd[:].unsqueeze(1).to_broadcast([128, KT, 512]), ALU.mult,
                          [xk, rk], [sk])
                    for (s0, n, isc) in segs(t0, 512):
                        o0 = s0 - t0
                        for kt in range(KT):
                            kb.ts("pool", hb[:, kt, o0:o0 + n], sq[:, kt, o0:o0 + n], A1[:, kt, isc:isc + 1],
                                  MV[:, 0, kt, isc:isc + 1], ALU.mult, ALU.add, [sk, f"A1{l}", f"MV{l}"], [hk])
                    if "hdbg" in kb.debug_outs:
                        if b == 0:
                            hdbg = kb.dram("hdbg", [D, S], BF16)
                            mvdbg = kb.dram("mvdbg", [128, 96], F32)
                            final_ops.append(kb.dma(mvdbg.ap()[:, :], MV[:].rearrange("p a k s -> p (a k s)"), r=[f"MV{l}"], w=["mvdbg"]))
                        final_ops.append(kb.dma(hdbg.ap().rearrange("(kt p) t -> p kt t", p=128)[:, :, t0:t0 + 512], hb[:], r=[hk], w=["hdbg"]))
                    for (c0, w) in tiles:
                        pst, pk = kb.ps()
                        for kt in range(KT):
                            kb.mm(pst[0:w, :], win[:, kt, c0:c0 + w], hb[:, kt, :], kt == 0, kt == KT - 1,
                                  wkeys(c0, w) + [hk], [pk])
                        sbuf, sbk = so[soi % 4], f"aso{soi % 4}"
                        soi += 1
                        kb.evac(sbuf[0:w, :], pst[0:w, :], [pk], [sbk])
                        kb.dma(pT.ap()[c0:c0 + w, t0:t0 + 512], sbuf[0:w, :], r=[sbk], w=["pT"])
                    for g4 in range(8 if (l < DEPTH - 1 or t0 < 4608) else 0):
                        gbuf, gk = go[g4 % 2], f"ago{g4 % 2}"
                        for j in range(4):
                            c0 = MIXC + (g4 * 4 + j) * 128
                            pst, pk = kb.ps()
                            for kt in range(KT):
                                kb.mm(pst[:], win[:, kt, c0:c0 + 128], hb[:, kt, :], kt == 0, kt == KT - 1,
                                      wkeys(c0, 128) + [hk], [pk])
                            kb.act(gbuf[:, j, :], pst[:], AF.Sigmoid, [pk], [gk])
                        r0 = g4 * 512
                        kb.dma(sgT.ap()[r0:r0 + 512, t0:t0 + 512].rearrange("(j p) t -> p j t", p=128), gbuf[:],
                               r=[gk], w=["sgT"])
            if stop_after == f"A{l}":
                break
            stage_rglru(kb, A, l, pT, brT, 4608 if l == DEPTH - 1 else S)
            if stop_after == f"D{l}":
                break
            tmax = 4608 if l == DEPTH - 1 else S
            stage_gla(kb, A, l, pT, brT, C, tmax)
            if stop_after == f"B{l}":
                break
            stage_s5(kb, A, l, pT, brT, C, tmax)
            if stop_after == f"C{l}":
                break
            stage_hyena(kb, A, l, pT, brT, C, do_ctx=(l < DEPTH - 1), half=(l == DEPTH - 1))
            if stop_after == f"E{l}":
                break
            last = (l == DEPTH - 1)
            tok_lo, tok_hi = (LAT0, LAT0 + NOUT) if last else (0, S)
            stage_f1(kb, A, l, xres_v, brT, sgT, h2T, gT, C, A1s[l], A2s[l], MVs[l], tok_lo, tok_hi, moe=(l % 2 == 1))
            if stop_after == f"F{l}":
                break
            stage_f2(kb, A, l, xres_v, h2T, gT, C, MVs[l], tok_lo, tok_hi, moe=(l % 2 == 1))
            if stop_after == f"G{l}":
                break
            continue
            if stop_after == f"E{l}":
                break
            if stop_after == f"C{l}":
                break
            if stop_after == f"B{l}":
                break
            if stop_after == f"D{l}":
                break

        if stop_after is None:
            stage_final(kb, A, xres_v, C)
        fin = []
        for key in ("pT", "sgT", "xres", "brT", "out", "hy_conv", "hy_rows", "h2T", "gT"):
            if key in P.last_w:
                fin.append(P.last_w[key])
        P.emit(final_wait_ops=sorted(set(fin + final_ops)))
    return nc


def host_prep(inputs, b, rev=False):
    f = np.float32
    m = {}
    x = np.asarray(inputs["x"][b], f)
    ctx = np.asarray(inputs["ctx"][b], f)
    if rev:
        x = x[::-1]
        ctx = ctx[::-1]
    xT = np.empty((D, S), f)
    xT[:, 0:LAT0] = ctx.T
    xT[:, LAT0:LAT1] = x.T
    xT[:, LAT1:S] = ctx.T
    m["xT"] = xT
    c2 = np.stack([np.asarray(inputs["c"][b], f), np.asarray(inputs["c_ctx"], f)], -1)
    m["c2"] = np.ascontiguousarray(c2.reshape(KT, 128, 2).transpose(1, 0, 2))
    m["mod_w"] = np.asarray(inputs["mod_w"], f)
    m["mod_b"] = np.ascontiguousarray(np.asarray(inputs["mod_b"], f).reshape(DEPTH, 48, 128).transpose(0, 2, 1))
    m["norm1_g"] = np.ascontiguousarray(np.asarray(inputs["norm1_g"], f).reshape(DEPTH, KT, 128).transpose(0, 2, 1))
    m["norm2_g"] = np.ascontiguousarray(np.asarray(inputs["norm2_g"], f).reshape(DEPTH, KT, 128).transpose(0, 2, 1))
    m["w_in"] = np.asarray(inputs["w_in"], f)
    wc = np.asarray(inputs["rg_w_conv"], f)
    taps = np.zeros((DEPTH, 2, 128, 5), f)
    for ct in range(2):
        taps[:, ct, :, 0:4] = wc[:, :, ct * 128:(ct + 1) * 128].transpose(0, 2, 1)
    m["rg_taps"] = taps
    wbd = np.zeros((DEPTH, 2, 128, 2, 2, 128), f)
    wa = np.asarray(inputs["rg_w_a"], f)
    wx = np.asarray(inputs["rg_w_x"], f)
    for ct in range(2):
        for bb in range(2):
            sl = slice(64 * bb, 64 * bb + 64)
            for d in range(2):
                wbd[:, ct, sl, d, 0, sl] = wa[:, d, 2 * ct + bb]
                wbd[:, ct, sl, d, 1, sl] = wx[:, d, 2 * ct + bb]
    m["rg_wbd"] = wbd
    ba = np.asarray(inputs["rg_b_a"], f).reshape(DEPTH, 2, 2, 128)
    bx = np.asarray(inputs["rg_b_x"], f).reshape(DEPTH, 2, 2, 128)
    m["rg_bias"] = np.ascontiguousarray(np.stack([ba, bx], -1).transpose(0, 2, 3, 1, 4))
    lre = np.asarray(inputs["s5_lam_re"], f); lim = np.asarray(inputs["s5_lam_im"], f); ldt = np.asarray(inputs["s5_log_dt"], f)
    bre = np.asarray(inputs["s5_b_re"], f); bim = np.asarray(inputs["s5_b_im"], f)
    cre = np.asarray(inputs["s5_c_re"], f); cim = np.asarray(inputs["s5_c_im"], f)
    prm = np.zeros((DEPTH, 2, 2, 4, 128, 3), f)
    Bz = np.zeros((DEPTH, 2, 2, 4, 128, 2, 128), f)
    Cz = np.zeros((DEPTH, 2, 2, 4, 128, 2, 128), f)
    for ct in range(2):
        for tl in range(4):
            for gl in range(2):
                g = ct * 8 + tl * 2 + gl
                ps_ = slice(64 * gl, 64 * gl + 64)
                cs_ = slice(32 * tl + 16 * gl, 32 * tl + 16 * gl + 16)
                prm[:, :, ct, tl, ps_, 0] = lre[:, :, g, :]
                prm[:, :, ct, tl, ps_, 1] = lim[:, :, g, :]
                prm[:, :, ct, tl, ps_, 2] = ldt[:, :, g, None]
                Bz[:, :, ct, tl, cs_, 0, ps_] = bre[:, :, g].transpose(0, 1, 3, 2)
                Bz[:, :, ct, tl, cs_, 1, ps_] = bim[:, :, g].transpose(0, 1, 3, 2)
                Cz[:, :, ct, tl, ps_, 0, cs_] = cre[:, :, g].transpose(0, 1, 3, 2)
                Cz[:, :, ct, tl, ps_, 1, cs_] = cim[:, :, g].transpose(0, 1, 3, 2)
    m["s5_prm"], m["s5_Bz"], m["s5_Cz"] = prm, Bz, Cz
    m["s5_dskip"] = np.ascontiguousarray(np.asarray(inputs["s5_d"], f).reshape(DEPTH, 2, 128, 1))
    m["s5_w_glu"] = np.asarray(inputs["s5_w_glu"], f)
    m["s5_bglu"] = np.ascontiguousarray(np.asarray(inputs["s5_b_glu"], f).reshape(DEPTH, 2, 128).transpose(0, 2, 1))
    if rev:
        m["rg_taps"] = m["rg_taps"][..., ::-1]
        m["rg_wbd"] = m["rg_wbd"][:, :, :, ::-1]
        m["rg_bias"] = m["rg_bias"][:, :, :, ::-1]
        m["s5_prm"], m["s5_Bz"], m["s5_Cz"] = m["s5_prm"][:, ::-1], m["s5_Bz"][:, ::-1], m["s5_Cz"][:, ::-1]
    m["jtab"] = np.ascontiguousarray(np.broadcast_to(np.arange(1, 129, dtype=f)[None, :], (128, 128)))
    for k_ in ("w_branch", "w_out", "ffn_w_gu", "ffn_w_down", "moe_router", "moe_w_gu", "moe_w_down"):
        m[k_] = np.asarray(inputs[k_], f)
    m["moe_rb"] = np.ascontiguousarray(np.broadcast_to(np.asarray(inputs["moe_router_b"], f)[0][None, :], (128, 8)))
    m["final_g"] = np.ascontiguousarray(np.asarray(inputs["final_g"], f).reshape(KT, 128).T)
    ws = np.asarray(inputs["hy_w_short"], f)
    m["hy_taps"] = np.ascontiguousarray(ws.reshape(DEPTH, 3, 3, 2, 128).transpose(0, 3, 4, 2, 1))
    m["hy_w1"] = np.asarray(inputs["hy_w1"], f)
    m["hy_w2"] = np.asarray(inputs["hy_w2"], f)
    m["hy_w3"] = np.asarray(inputs["hy_w3"], f)
    m["hy_vec"] = np.ascontiguousarray(np.stack([np.asarray(inputs["hy_b1"], f), np.asarray(inputs["hy_b2"], f),
                                                 np.asarray(inputs["hy_freq"], f)], -1))
    m["hy_bias"] = np.ascontiguousarray(np.asarray(inputs["hy_bias"], f).reshape(DEPTH, 2, 128, 1))
    wu = np.asarray(inputs["gla_w_up"], f)
    wup = np.zeros((DEPTH, 2, 32, 2, 128), f)
    for hp in range(2):
        for d in range(2):
            wup[:, hp, 16 * d:16 * d + 16, d, :] = wu[:, d, :, 128 * hp:128 * (hp + 1)]
    m["gla_wup"] = wup
    m["gla_bup"] = np.ascontiguousarray(np.asarray(inputs["gla_b_up"], f).reshape(DEPTH, 2, 2, 128).transpose(0, 2, 3, 1))
    m["rg_lam"] = np.ascontiguousarray(np.asarray(inputs["rg_lam"], f).reshape(DEPTH, 2, 2, 128).transpose(0, 2, 3, 1))
    if rev:
        m["rg_lam"] = m["rg_lam"][..., ::-1]
        m["gla_wup"] = m["gla_wup"][:, :, :, ::-1]
        m["gla_bup"] = m["gla_bup"][..., ::-1]
        m["hy_taps"] = m["hy_taps"][..., ::-1]
        m["hy_w3"] = np.concatenate([m["hy_w3"][..., 256:], m["hy_w3"][..., :256]], -1)
    for k_ in list(m):
        m[k_] = np.ascontiguousarray(m[k_], dtype=f)
    return m


def pos_table():
    rows = NLAT // 64
    q = D // 4
    omega = (1.0 / (10000.0 ** (np.arange(q, dtype=np.float32) / np.float32(q)))).astype(np.float32)
    r = np.arange(rows, dtype=np.float32)[:, None] * omega
    cc = np.arange(64, dtype=np.float32)[:, None] * omega
    er = np.concatenate([np.sin(r), np.cos(r)], -1)
    ec = np.concatenate([np.sin(cc), np.cos(cc)], -1)
    emb = np.concatenate([np.broadcast_to(er[:, None], (rows, 64, D // 2)),
                          np.broadcast_to(ec[None], (rows, 64, D // 2))], -1).reshape(NLAT, D)
    return np.ascontiguousarray(emb.T.astype(np.float32))


def hyena_consts():
    f = np.float32
    out = {}
    for nm, n in (("", NLAT), ("c", NCTX)):
        t = np.arange(n, dtype=f)[:, None]
        bands = np.linspace(1e-4, 15, 16, dtype=f)[None]
        ang = (f(2.0 * math.pi) * bands * t / f(n)).astype(f)
        z = np.concatenate([t / f(n), np.cos(ang), np.sin(ang)], -1).astype(f)
        out["hy_zT" + nm] = np.ascontiguousarray(z.T)
        t01 = (t[:, 0] / f(max(n - 1, 1))).astype(f)
        out["hy_t01" + nm] = np.ascontiguousarray(np.broadcast_to(t01[None, :], (128, n)))
    deltas = np.abs(np.linspace(math.log(1e-2) / 0.3, math.log(1e-2) / 1.5, 256, dtype=f))
    nd = -np.tile(deltas, 2).astype(f)
    out["hy_ndelta"] = np.ascontiguousarray(nd.reshape(4, 128).T)
    return out


def make_program(in_map, stop_after=None, debug_outs=()):
    nc = bass.Bass("TRN2", target_bir_lowering=False)
    A = {}
    for k, v in in_map.items():
        A[k] = nc.dram_tensor(k, list(v.shape), F32, kind="ExternalInput").ap()
    A["out"] = nc.dram_tensor("out", [D, NOUT], F32, kind="ExternalOutput").ap()
    build(nc, A, stop_after, debug_outs)
    return nc


def kernel(**inputs):
    pos = pos_table()
    pos_r = np.ascontiguousarray(pos[:, ::-1])
    hyc = hyena_consts()
    maps = []
    for b in range(4):
        for rev in (False, True):
            m = host_prep(inputs, b, rev)
            m["posT"] = pos_r if rev else pos
            m.update(hyc)
            maps.append(m)
    nc = make_program(maps[0], stop_after=os.environ.get("KSTOP"))
    res = run_bass_kernel_spmd(nc, maps, core_ids=list(range(8)))
    out = np.empty((4, NLAT, D), np.float32)
    for b in range(4):
        out[b, 0:NOUT] = res.results[2 * b]["out"].T
        out[b, NOUT:NLAT] = res.results[2 * b + 1]["out"].T[::-1]
    return out
```
